# Optimizing a Trainium2 kernel written in Bass

```python
import math
import jax, jax.numpy as jnp
from jax import lax
import numpy as np

D_MODEL = 1024
BATCH = 8
SEQ = 2048
DEPTH = 2

HEAD_DIM = 64
ATTN_HEADS = 8
ATTN_KV_HEADS = 2
ATTN_WIDTH = ATTN_HEADS * HEAD_DIM
KV_WIDTH = ATTN_KV_HEADS * HEAD_DIM
WINDOW = 128
BLOCK = 128
ROPE_THETA = 10000.0
HYENA_WIDTH = D_MODEL - ATTN_WIDTH
HYENA_ORDER = 2
HYENA_SHORT = 3
FILTER_EMB = 33
FILTER_HIDDEN = 64
FAST_DECAY_PCT = 0.3
SLOW_DECAY_PCT = 1.5
DECAY_TARGET = 1e-2
IN_WIDTH = ATTN_WIDTH + 2 * KV_WIDTH + (HYENA_ORDER + 1) * HYENA_WIDTH
MIX_WIDTH = ATTN_WIDTH + HYENA_WIDTH
CONV_KERNEL = 31
CONV_WIDTH = D_MODEL
FF_DENSE = 2816
N_EXPERTS = 8
TOP_K = 2
FF_EXPERT = 3584
EPS = 1e-6

kernel_name = "hybrid_swa_hyena_conformer_moe_encoder"


def rmsnorm(x, g):
    xf = x.astype(jnp.float32)
    y = xf * lax.rsqrt(jnp.mean(xf * xf, axis=-1, keepdims=True) + EPS)
    return (y * g.astype(jnp.float32)).astype(x.dtype)


def layernorm(x, g, b):
    xf = x.astype(jnp.float32)
    mu = jnp.mean(xf, axis=-1, keepdims=True)
    var = jnp.mean(jnp.square(xf - mu), axis=-1, keepdims=True)
    y = (xf - mu) * lax.rsqrt(var + EPS)
    return (y * g.astype(jnp.float32) + b.astype(jnp.float32)).astype(x.dtype)


def rope(t, positions):
    half = HEAD_DIM // 2
    inv = ROPE_THETA ** (-jnp.arange(half, dtype=jnp.float32) / half)
    ang = positions[:, None] * inv[None, :]
    cos = jnp.cos(ang)[None, :, None, :]
    sin = jnp.sin(ang)[None, :, None, :]
    tf = t.astype(jnp.float32)
    t1, t2 = tf[..., :half], tf[..., half:]
    return jnp.concatenate([t1 * cos - t2 * sin, t2 * cos + t1 * sin], axis=-1).astype(t.dtype)


def windowed_gqa_sink(q, k, v, sink):
    B, L, H, Dh = q.shape
    KV = k.shape[2]
    G = H // KV
    nb = L // BLOCK
    n_side = WINDOW // BLOCK
    n_slices = 2 * n_side + 1
    padw = n_side * BLOCK
    kp = jnp.pad(k, ((0, 0), (padw, padw), (0, 0), (0, 0)))
    vp = jnp.pad(v, ((0, 0), (padw, padw), (0, 0), (0, 0)))

    def band(t):
        return jnp.concatenate(
            [t[:, i * BLOCK:i * BLOCK + L].reshape(B, nb, BLOCK, KV, Dh) for i in range(n_slices)],
            axis=2)

    kb = band(kp).astype(jnp.float32)
    vb = band(vp).astype(jnp.float32)
    qb = q.reshape(B, nb, BLOCK, KV, G, Dh).astype(jnp.float32)
    s = jnp.einsum('bnqkgd,bnskd->bnkgqs', qb, kb) / math.sqrt(Dh)
    r = jnp.arange(BLOCK)[:, None]
    j = jnp.arange(n_slices * BLOCK)[None, :]
    rel = j - padw - r
    kpos = jnp.arange(nb)[:, None, None] * BLOCK + (j - padw)[None]
    mask = (jnp.abs(rel) <= WINDOW)[None] & (kpos >= 0) & (kpos < L)
    s = jnp.where(mask[None, :, None, None], s, -jnp.inf)
    sk = sink.astype(jnp.float32).reshape(1, 1, KV, G, 1, 1)
    m = jnp.maximum(jnp.max(s, axis=-1, keepdims=True), sk)
    p = jnp.exp(s - m)
    p = p / (jnp.sum(p, axis=-1, keepdims=True) + jnp.exp(sk - m))
    o = jnp.einsum('bnkgqs,bnskd->bnqkgd', p, vb)
    return o.reshape(B, L, H * Dh).astype(q.dtype)


def hyena_filter_fft(L, w1, b1, w2, b2, w3, b3, freq):
    f32 = jnp.float32
    C = HYENA_WIDTH
    bands = (FILTER_EMB - 1) // 2
    t = jnp.linspace(0.0, 1.0, L, dtype=f32)
    w = 2.0 * math.pi * jnp.arange(L, dtype=f32) / L
    fb = jnp.linspace(1e-4, bands - 1, bands, dtype=f32)
    zw = fb[None, :] * w[:, None]
    z = jnp.concatenate([t[:, None], jnp.cos(zw), -jnp.sin(zw)], axis=-1)
    fq = freq.astype(f32)
    h = jnp.sin(fq[0] * (z @ w1.astype(f32) + b1.astype(f32)))
    h = jnp.sin(fq[1] * (h @ w2.astype(f32) + b2.astype(f32)))
    h = (h @ w3.astype(f32) + b3.astype(f32)).reshape(L, HYENA_ORDER, 2, C)
    min_decay = math.log(DECAY_TARGET) / SLOW_DECAY_PCT
    max_decay = math.log(DECAY_TARGET) / FAST_DECAY_PCT
    deltas = jnp.abs(jnp.linspace(min_decay, max_decay, C, dtype=f32))
    decay = jnp.exp(-t[:, None] * deltas[None, :])
    h = h * decay[:, None, None, :]
    fwd, bwd = h[:, :, 0], h[:, :, 1]
    kern = jnp.concatenate([fwd, jnp.zeros((1, HYENA_ORDER, C), f32), bwd[1:][::-1]], axis=0)
    return jnp.fft.rfft(kern, axis=0)


def hyena_mix(u, short_w, short_b, kf, dskip):
    B, L, _ = u.shape
    C = HYENA_WIDTH
    pad = HYENA_SHORT // 2
    uc = lax.conv_general_dilated(u, short_w[:, None, :].astype(u.dtype), (1,), [(pad, pad)],
                                  dimension_numbers=('NWC', 'WIO', 'NWC'),
                                  feature_group_count=u.shape[-1]) + short_b.astype(u.dtype)
    gates = [uc[..., 0:C], uc[..., C:2 * C]]
    z = uc[..., 2 * C:].astype(jnp.float32)
    ds = dskip.astype(jnp.float32)
    for n in range(HYENA_ORDER):
        zf = jnp.fft.rfft(z, n=2 * L, axis=1)
        y = jnp.fft.irfft(zf * kf[None, :, n, :], n=2 * L, axis=1)[:, :L]
        z = gates[n].astype(jnp.float32) * (y + z * ds[n])
    return z.astype(u.dtype)


def swiglu(h, wg, wu, wd):
    return (jax.nn.silu(h @ wg) * (h @ wu)) @ wd


def conformer_conv(h, pw1_w, pw1_b, dw_w, dw_b, ln_g, ln_b, pw2_w, pw2_b):
    u = h @ pw1_w + pw1_b
    u = u[..., :CONV_WIDTH] * jax.nn.sigmoid(u[..., CONV_WIDTH:])
    pad = CONV_KERNEL // 2
    u = lax.conv_general_dilated(u, dw_w[:, None, :].astype(u.dtype), (1,), [(pad, pad)],
                                 dimension_numbers=('NWC', 'WIO', 'NWC'),
                                 feature_group_count=CONV_WIDTH) + dw_b
    u = jax.nn.silu(layernorm(u, ln_g, ln_b))
    return u @ pw2_w + pw2_b


def moe_swiglu(h, router, wg, wu, wd):
    B, L, D = h.shape
    hf = h.reshape(B * L, D)
    logits = hf.astype(jnp.float32) @ router.astype(jnp.float32)
    top_v, top_i = lax.top_k(logits, TOP_K)
    gates = jax.nn.softmax(top_v, axis=-1)
    combine = jnp.sum(jax.nn.one_hot(top_i, N_EXPERTS, dtype=jnp.float32) * gates[..., None], axis=1)
    out = jnp.zeros_like(hf)
    for e in range(N_EXPERTS):
        out = out + combine[:, e:e + 1].astype(hf.dtype) * swiglu(hf, wg[e], wu[e], wd[e])
    return out.reshape(B, L, D)


def setup_inputs(seed: int = 0) -> dict:
    key = jax.random.key(seed)
    ks = iter(jax.random.split(key, 40))
    ne = (DEPTH + 1) // 2
    no = DEPTH // 2
    D = D_MODEL
    C = HYENA_WIDTH
    f32 = jnp.float32

    def nrm(shape, scale):
        return jax.random.normal(next(ks), shape, f32) * scale

    def gain(shape):
        return 1.0 + nrm(shape, 0.02)

    return {
        "x": nrm((BATCH, SEQ, D), 1.0),
        "ev_norm_mix": gain((ne, D)),
        "ev_w_in": nrm((ne, D, IN_WIDTH), D ** -0.5),
        "ev_sink": nrm((ne, ATTN_HEADS), 0.5),
        "ev_short_w": nrm((ne, HYENA_SHORT, 3 * C), HYENA_SHORT ** -0.5),
        "ev_short_b": nrm((ne, 3 * C), 0.02),
        "ev_filt_w1": nrm((ne, FILTER_EMB, FILTER_HIDDEN), FILTER_EMB ** -0.5),
        "ev_filt_b1": nrm((ne, FILTER_HIDDEN), 0.1),
        "ev_filt_w2": nrm((ne, FILTER_HIDDEN, FILTER_HIDDEN), FILTER_HIDDEN ** -0.5),
        "ev_filt_b2": nrm((ne, FILTER_HIDDEN), 0.1),
        "ev_filt_w3": nrm((ne, FILTER_HIDDEN, HYENA_ORDER * 2 * C), 0.03 * FILTER_HIDDEN ** -0.5),
        "ev_filt_b3": nrm((ne, HYENA_ORDER * 2 * C), 0.005),
        "ev_filt_freq": 1.0 + nrm((ne, 2, FILTER_HIDDEN), 0.05),
        "ev_dskip": nrm((ne, HYENA_ORDER, C), 0.5),
        "ev_w_out": nrm((ne, MIX_WIDTH, D), MIX_WIDTH ** -0.5),
        "ev_norm_ffn": gain((ne, D)),
        "ev_ffn_wg": nrm((ne, D, FF_DENSE), D ** -0.5),
        "ev_ffn_wu": nrm((ne, D, FF_DENSE), D ** -0.5),
        "ev_ffn_wd": nrm((ne, FF_DENSE, D), FF_DENSE ** -0.5),
        "od_norm_mix": gain((no, D)),
        "od_pw1_w": nrm((no, D, 2 * CONV_WIDTH), D ** -0.5),
        "od_pw1_b": nrm((no, 2 * CONV_WIDTH), 0.02),
        "od_dw_w": nrm((no, CONV_KERNEL, CONV_WIDTH), CONV_KERNEL ** -0.5),
        "od_dw_b": nrm((no, CONV_WIDTH), 0.02),
        "od_ln_g": gain((no, CONV_WIDTH)),
        "od_ln_b": nrm((no, CONV_WIDTH), 0.02),
        "od_pw2_w": nrm((no, CONV_WIDTH, D), CONV_WIDTH ** -0.5),
        "od_pw2_b": nrm((no, D), 0.02),
        "od_norm_ffn": gain((no, D)),
        "od_router": nrm((no, D, N_EXPERTS), D ** -0.5),
        "od_moe_wg": nrm((no, N_EXPERTS, D, FF_EXPERT), D ** -0.5),
        "od_moe_wu": nrm((no, N_EXPERTS, D, FF_EXPERT), D ** -0.5),
        "od_moe_wd": nrm((no, N_EXPERTS, FF_EXPERT, D), FF_EXPERT ** -0.5),
        "final_norm": gain((D,)),
    }


def reference(x, ev_norm_mix, ev_w_in, ev_sink, ev_short_w, ev_short_b, ev_filt_w1, ev_filt_b1,
              ev_filt_w2, ev_filt_b2, ev_filt_w3, ev_filt_b3, ev_filt_freq, ev_dskip, ev_w_out,
              ev_norm_ffn, ev_ffn_wg, ev_ffn_wu, ev_ffn_wd,
              od_norm_mix, od_pw1_w, od_pw1_b, od_dw_w, od_dw_b, od_ln_g, od_ln_b, od_pw2_w, od_pw2_b,
              od_norm_ffn, od_router, od_moe_wg, od_moe_wu, od_moe_wd, final_norm):
    B, L, _ = x.shape
    positions = jnp.arange(L, dtype=jnp.float32)
    q_end = ATTN_WIDTH
    k_end = q_end + KV_WIDTH
    v_end = k_end + KV_WIDTH
    for layer in range(DEPTH):
        i = layer // 2
        if layer % 2 == 0:
            h = rmsnorm(x, ev_norm_mix[i])
            proj = h @ ev_w_in[i]
            q = rope(proj[..., :q_end].reshape(B, L, ATTN_HEADS, HEAD_DIM), positions)
            k = rope(proj[..., q_end:k_end].reshape(B, L, ATTN_KV_HEADS, HEAD_DIM), positions)
            v = proj[..., k_end:v_end].reshape(B, L, ATTN_KV_HEADS, HEAD_DIM)
            a_out = windowed_gqa_sink(q, k, v, ev_sink[i])
            kf = hyena_filter_fft(L, ev_filt_w1[i], ev_filt_b1[i], ev_filt_w2[i], ev_filt_b2[i],
                                  ev_filt_w3[i], ev_filt_b3[i], ev_filt_freq[i])
            b_out = hyena_mix(proj[..., v_end:], ev_short_w[i], ev_short_b[i], kf, ev_dskip[i])
            x = x + jnp.concatenate([a_out, b_out], axis=-1) @ ev_w_out[i]
            h = rmsnorm(x, ev_norm_ffn[i])
            x = x + swiglu(h, ev_ffn_wg[i], ev_ffn_wu[i], ev_ffn_wd[i])
        else:
            h = rmsnorm(x, od_norm_mix[i])
            x = x + conformer_conv(h, od_pw1_w[i], od_pw1_b[i], od_dw_w[i], od_dw_b[i],
                                   od_ln_g[i], od_ln_b[i], od_pw2_w[i], od_pw2_b[i])
            h = rmsnorm(x, od_norm_ffn[i])
            x = x + moe_swiglu(h, od_router[i], od_moe_wg[i], od_moe_wu[i], od_moe_wd[i])
    return rmsnorm(x, final_norm)
```

```python
import math
from contextlib import ExitStack
import numpy as np
import ml_dtypes
import concourse.bass as bass
import concourse.mybir as mybir
from concourse.bass_utils import run_bass_kernel_spmd

F32 = mybir.dt.float32
BF16 = mybir.dt.bfloat16
ALU = mybir.AluOpType
AF = mybir.ActivationFunctionType
AX = mybir.AxisListType

ENGS = ('pe', 'act', 'dve', 'pool', 'sp')
EPOCH = 24000

D = 1024
L = 2048
NB = 16
NT = 4
FF = 2816
FFE = 3584
NE = 8
EPS = 1e-6


class Res:
    __slots__ = ('name', 'w', 'r')

    def __init__(self, name):
        self.name = name
        self.w = None
        self.r = {}


class Tok:
    __slots__ = ('sem', 'val', 'eng')

    def __init__(self, sem, val, eng):
        self.sem = sem
        self.val = val
        self.eng = eng


class Prog:
    def __init__(self, nc, es, same_engine_raw=True):
        self.nc = nc
        self.es = es
        self.ops = {e: [] for e in ENGS}
        self.count = {e: 0 for e in ENGS}
        self.epoch = {e: 0 for e in ENGS}
        self.csem = {}
        for e in ('pe', 'act', 'dve', 'pool'):
            self.csem[e] = [es.enter_context(nc.semaphore('c_%s_0' % e))]
        self.waited = {e: {} for e in ENGS}
        self.dsem = {}
        self.same_engine_raw = same_engine_raw
        self.all_toks = {e: None for e in ENGS}

    def dma_sem(self, name):
        if name not in self.dsem:
            self.dsem[name] = [self.es.enter_context(self.nc.semaphore('d_' + name)), 0]
        return self.dsem[name]

    def _need(self, eng, tok, waits, raw):
        if tok is None:
            return
        if tok.eng == eng:
            if not (raw and self.same_engine_raw and eng in ('act', 'dve', 'pool')):
                return
        k = id(tok.sem)
        if self.waited[eng].get(k, 0) >= tok.val:
            return
        if k in waits:
            if waits[k][1] < tok.val:
                waits[k] = (tok.sem, tok.val)
        else:
            waits[k] = (tok.sem, tok.val)

    def _hazards(self, eng, reads, writes):
        waits = {}
        for r in reads:
            self._need(eng, r.w, waits, True)
        for w in writes:
            self._need(eng, w.w, waits, False)
            for t in w.r.values():
                self._need(eng, t, waits, False)
        for k, (s, v) in waits.items():
            self.waited[eng][k] = v
        return list(waits.values())

    def op(self, eng, fn, reads=(), writes=()):
        waits = self._hazards(eng, reads, writes)
        self.count[eng] += 1
        if self.count[eng] > EPOCH:
            self.epoch[eng] += 1
            self.csem[eng].append(self.es.enter_context(
                self.nc.semaphore('c_%s_%d' % (eng, self.epoch[eng]))))
            self.count[eng] = 1
        sem = self.csem[eng][-1]
        tok = Tok(sem, self.count[eng], eng)
        for r in reads:
            r.r[eng] = tok
        for w in writes:
            w.w = tok
            w.r = {}
        self.ops[eng].append((waits, fn, (sem, 1)))
        self.all_toks[eng] = tok
        return tok

    def dma(self, q, out, in_, reads=(), writes=(), sem='dma', **kw):
        waits = self._hazards(q, reads, writes)
        s = self.dma_sem(sem)
        s[1] += 16
        tok = Tok(s[0], s[1], None)
        key = 'dma_' + sem
        for r in reads:
            r.r[key] = tok
        for w in writes:
            w.w = tok
            w.r = {}

        def fn(e, out=out, in_=in_, kw=kw):
            return e.dma_start(out=out, in_=in_, **kw)
        self.ops[q].append((waits, fn, (s[0], 16)))
        return tok

    def barrier(self):
        toks = [t for t in self.all_toks.values() if t is not None]
        for name, (s, v) in self.dsem.items():
            if v > 0:
                toks.append(Tok(s, v, None))
        for e in ENGS:
            waits = {}
            for t in toks:
                if t.eng == e:
                    continue
                self._need(e, t, waits, True)
            for k, (s, v) in waits.items():
                self.waited[e][k] = v
            if waits:
                self.ops[e].append((list(waits.values()), None, None))

    def emit(self):
        nc = self.nc
        with nc.Block() as block:
            def mk(ename):
                def body(e):
                    for waits, fn, inc in self.ops[ename]:
                        for (s, v) in waits:
                            e.wait_ge(s, v)
                        if fn is not None:
                            ins = fn(e)
                            if inc is not None:
                                ins.then_inc(inc[0], inc[1])
                return body
            block.tensor(mk('pe'))
            block.scalar(mk('act'))
            block.vector(mk('dve'))
            block.gpsimd(mk('pool'))
            block.sync(mk('sp'))


class Arena:
    def __init__(self, nc, base=17408, limit=224 * 1024 - 256):
        self.nc = nc
        self.top = base
        self.limit = limit
        self.n = 0
        self.peak = 0
        self.prog = None

    def alloc(self, name, shape, dtype):
        free = int(np.prod(shape[1:]))
        nbytes = free * (4 if dtype == F32 else 2)
        off = (self.top + 63) // 64 * 64
        self.n += 1
        t = self.nc.alloc_sbuf_tensor_at('%s_%d' % (name, self.n), list(shape), dtype, offset=off)
        self.last_off = off
        self.top = off + nbytes
        self.peak = max(self.peak, self.top)
        assert self.top <= self.limit, ('SBUF overflow', name, self.top)
        return t

    def mark(self):
        return self.top

    def release(self, m):
        if self.prog is not None:
            self.prog.barrier()
        self.top = m


class Rot:
    def __init__(self, items):
        self.items = items
        self.i = 0

    def next(self):
        it = self.items[self.i % len(self.items)]
        self.i += 1
        return it


def build_program(stop='full'):
    import os
    SKIP = set(os.environ.get('KSKIP', '').split(','))
    nc = bass.Bass("TRN2", target_bir_lowering=False)

    def din(name, shape, dt=F32):
        return nc.dram_tensor(name, list(shape), dt, kind="ExternalInput").ap()

    x_d = din('x', [L, D])
    out_d = nc.dram_tensor('out', [L, D], F32, kind="ExternalOutput").ap()
    ev_norm_mix = din('ev_norm_mix', [D])
    ev_w_in = din('ev_w_in', [D, 2304])
    ev_sink = din('ev_sink', [8])
    ev_short_w = din('ev_short_w', [3, 1536])
    ev_short_b = din('ev_short_b', [1536])
    ev_filt_w1 = din('ev_filt_w1', [33, 64])
    ev_filt_b1 = din('ev_filt_b1', [64])
    ev_filt_w2 = din('ev_filt_w2', [64, 64])
    ev_filt_b2 = din('ev_filt_b2', [64])
    ev_filt_w3 = din('ev_filt_w3', [64, 2048])
    ev_filt_b3 = din('ev_filt_b3', [2048])
    ev_filt_freq = din('ev_filt_freq', [2, 64])
    ev_dskip = din('ev_dskip', [2, 512])
    ev_w_out = din('ev_w_out', [D, D])
    ev_norm_ffn = din('ev_norm_ffn', [D])
    ev_ffn_wg = din('ev_ffn_wg', [D, FF])
    ev_ffn_wu = din('ev_ffn_wu', [D, FF])
    ev_ffn_wd = din('ev_ffn_wd', [FF, D])
    od_norm_mix = din('od_norm_mix', [D])
    od_pw1_w = din('od_pw1_w', [D, 2048])
    od_pw1_b = din('od_pw1_b', [2048])
    od_dw_w = din('od_dw_w', [31, D])
    od_dw_b = din('od_dw_b', [D])
    od_ln_g = din('od_ln_g', [D])
    od_ln_b = din('od_ln_b', [D])
    od_pw2_w = din('od_pw2_w', [D, D])
    od_pw2_b = din('od_pw2_b', [D])
    od_norm_ffn = din('od_norm_ffn', [D])
    od_router = din('od_router', [D, NE])
    od_moe_wg = din('od_moe_wg', [NE, D, FFE])
    od_moe_wu = din('od_moe_wu', [NE, D, FFE])
    od_moe_wd = din('od_moe_wd', [NE, FFE, D])
    final_norm = din('final_norm', [D])
    c_ident = din('c_ident', [128, 128])
    c_cos = din('c_cos', [128, L])
    c_sin = din('c_sin', [128, L])
    c_mask = din('c_mask', [128, 384])
    c_zf = din('c_zf', [33, L + 1])
    c_dec = din('c_dec', [L, 512])
    c_decsh = din('c_decsh', [L, 512])
    c_cq = din('c_cq', [L, L], BF16)
    c_sq = din('c_sq', [L, L], BF16)
    c_cqf = din('c_cqf', [16, 128, 16 * 128], BF16)
    c_sqf = din('c_sqf', [16, 128, 16 * 128], BF16)
    c_cfsf = din('c_cfsf', [128, 32])
    c_sel = din('c_sel', [8, 8 * 128])
    kspec = nc.dram_tensor('kspec', [16, 128, 2 * 2 * 512], BF16, kind="Internal").ap()

    es = ExitStack()
    with es:
        P = Prog(nc, es)
        A = Arena(nc)
        A.prog = P
        NCD = dict(allow_slow_non_contiguous=True)

        PS = []
        for i in range(8):
            t = es.enter_context(nc.psum_tensor('ps%d' % i, [128, 512], F32))
            PS.append((t, Res('ps%d' % i)))

        def ps_bf(t):
            return t[:].bitcast(BF16)

        ident = A.alloc('ident', [128, 128], F32)
        identb = A.alloc('identb', [128, 128], BF16)
        onesb = A.alloc('onesb', [128, 128], BF16)
        onesf = A.alloc('onesf', [128, 128], F32)
        negpi = A.alloc('negpi', [128, 1], F32)
        epsD = A.alloc('epsD', [128, 1], F32)
        epsT = A.alloc('epsT', [128, 1], F32)
        r_const = Res('const')
        P.dma('sp', ident[:], c_ident, writes=[r_const], sem='cid')
        P.op('dve', lambda e: e.tensor_copy(out=identb[:], in_=ident[:]), reads=[r_const], writes=[r_const])
        P.op('dve', lambda e: e.memset(onesb[:], 1.0), writes=[r_const])
        P.op('dve', lambda e: e.memset(onesf[:], 1.0), writes=[r_const])
        P.op('dve', lambda e: e.memset(negpi[:], -math.pi), writes=[r_const])
        P.op('dve', lambda e: e.memset(epsD[:], float(D * EPS)), writes=[r_const])
        P.op('dve', lambda e: e.memset(epsT[:], float(EPS)), writes=[r_const])

        vecs = A.alloc('vecs', [128, 160], F32)
        r_vec = Res('vecs')
        vofs = {}
        vo = [0]

        def load_vec(name, ap, n):
            nch = n // 128
            vofs[name] = vo[0]
            if 'vecs' in SKIP:
                vo[0] += nch
                return
            P.dma('act', vecs[:, vo[0]:vo[0] + nch], ap.rearrange('(c p) -> p c', p=128),
                  writes=[r_vec], sem='cv', **NCD)
            vo[0] += nch

        load_vec('g_mix0', ev_norm_mix, D)
        load_vec('g_ffn0', ev_norm_ffn, D)
        load_vec('g_mix1', od_norm_mix, D)
        load_vec('g_ffn1', od_norm_ffn, D)
        load_vec('g_fin', final_norm, D)
        load_vec('short_b', ev_short_b, 1536)
        load_vec('sw0', ev_short_w[0], 1536)
        load_vec('sw1', ev_short_w[1], 1536)
        load_vec('sw2', ev_short_w[2], 1536)
        load_vec('ds0', ev_dskip[0], 512)
        load_vec('ds1', ev_dskip[1], 512)
        load_vec('pw1_b', od_pw1_b, 2048)
        load_vec('dw_b', od_dw_b, D)
        load_vec('ln_g', od_ln_g, D)
        load_vec('ln_b', od_ln_b, D)
        load_vec('pw2_b', od_pw2_b, D)
        assert vo[0] <= 160

        def V(name, c):
            o = vofs[name] + c
            return vecs[:, o:o + 1]

        sinkb = A.alloc('sinkb', [128, 8], F32)
        nsinkb = A.alloc('nsinkb', [128, 8], F32)
        cfsf = A.alloc('cfsf', [128, 32], F32)
        r_sink = Res('sink')
        r_cfsf = Res('cfsf')
        if 'sink' not in SKIP:
            P.dma('sp', sinkb[:], ev_sink.partition_broadcast(128), writes=[r_sink], sem='csk', **NCD)
        P.dma('sp', cfsf[:], c_cfsf, writes=[r_cfsf], sem='ccf')
        P.op('dve', lambda e: e.tensor_scalar(out=nsinkb[:], in0=sinkb[:], scalar1=-1.0, scalar2=None,
                                              op0=ALU.mult), reads=[r_sink], writes=[r_sink])
        P.op('dve', lambda e: e.tensor_scalar(out=vecs[:, 0:40], in0=vecs[:, 0:40], scalar1=float(math.sqrt(D)),
                                              scalar2=None, op0=ALU.mult), reads=[r_vec], writes=[r_vec])

        persist_mark = A.mark()
        if stop == 'const':
            P.barrier()
            P.emit()
            return nc

        def MM(ps_ap, lhsT, rhs, start, stop, R, W):
            P.op('pe', lambda e: e.matmul(ps_ap, lhsT=lhsT, rhs=rhs, start=start, stop=stop),
                 reads=R, writes=W)

        def TR(ps_ap, in_ap, id_ap, R, W):
            P.op('pe', lambda e: e.transpose(ps_ap, in_ap, id_ap), reads=R, writes=W)

        psrot = Rot(PS)

        def rmsnorm_block(xTb, r_x, gname, hT_ap, r_h, hf_ap=None, tmp=None):
            m = A.mark()
            if tmp is None:
                sq = A.alloc('sq', [128, 8, 512], BF16)
                rs = A.alloc('rs', [128, 512], F32)
                r_sq, r_rs = Res('sq'), Res('rs')
            else:
                sq, rs, r_sq, r_rs = tmp
            P.op('act', lambda e: e.activation(out=sq[:], in_=xTb, func=AF.Square), reads=[r_x], writes=[r_sq])
            pst, r_ps = psrot.next()
            for c in range(8):
                MM(pst[:], onesb[:], sq[:, c, :], c == 0, c == 7, [r_sq, r_const], [r_ps])
            P.op('act', lambda e: e.activation(out=rs[:], in_=pst[:], func=AF.Sqrt, bias=epsD[:], scale=1.0),
                 reads=[r_ps, r_const], writes=[r_rs])
            P.op('dve', lambda e: e.reciprocal(out=rs[:], in_=rs[:]), reads=[r_rs], writes=[r_rs])
            for c in range(8):
                P.op('dve', lambda e, c=c: e.scalar_tensor_tensor(
                    out=hT_ap[:, c, :], in0=xTb[:, c, :], scalar=V(gname, c), in1=rs[:],
                    op0=ALU.mult, op1=ALU.mult), reads=[r_x, r_rs, r_vec], writes=[r_h])
                if hf_ap is not None:
                    P.op('dve', lambda e, c=c: e.scalar_tensor_tensor(
                        out=hf_ap[:, c, :], in0=xTb[:, c, :], scalar=V(gname, c), in1=rs[:],
                        op0=ALU.mult, op1=ALU.mult), reads=[r_x, r_rs, r_vec], writes=[r_h])
            if tmp is None:
                A.release(m)

        def load_x_block(tb, xTb, r_xTb, stage=None):
            m = A.mark()
            if stage is None:
                xtok = A.alloc('xtok', [128, 4, D], F32)
                r_xtok = Res('xtok')
                semn = 'xin'
            else:
                xtok, r_xtok, semn = stage
            P.dma('sp', xtok[:], x_d[tb * 512:(tb + 1) * 512, :].rearrange('(j p) d -> p j d', p=128),
                  writes=[r_xtok], sem=semn)
            for c in range(8):
                pst, r_ps = psrot.next()
                for j in range(4):
                    TR(pst[:, j * 128:(j + 1) * 128], xtok[:, j, c * 128:(c + 1) * 128], ident[:],
                       [r_xtok, r_const], [r_ps])
                eng = 'act' if c % 2 == 0 else 'dve'
                if eng == 'act':
                    P.op('act', lambda e, c=c, pst=pst: e.copy(out=xTb[:, c, :], in_=pst[:]),
                         reads=[r_ps], writes=[r_xTb])
                else:
                    P.op('dve', lambda e, c=c, pst=pst: e.tensor_copy(out=xTb[:, c, :], in_=pst[:]),
                         reads=[r_ps], writes=[r_xTb])
            if stage is None:
                A.release(m)

        def store_block(tb, oT, r_oT, stage=None):
            m = A.mark()
            if stage is None:
                otok = A.alloc('otok', [128, 4, D], F32)
                r_otok = Res('otok')
                semn = 'xout'
            else:
                otok, r_otok, semn = stage
            for j in range(4):
                for half in range(2):
                    pst, r_ps = psrot.next()
                    for cc in range(4):
                        c = half * 4 + cc
                        TR(pst[:, cc * 128:(cc + 1) * 128], oT[:, c, j * 128:(j + 1) * 128], ident[:],
                           [r_oT, r_const], [r_ps])
                    if (j + half) % 2 == 0:
                        P.op('act', lambda e, j=j, half=half, pst=pst: e.copy(
                            out=otok[:, j, half * 512:(half + 1) * 512], in_=pst[:]),
                            reads=[r_ps], writes=[r_otok])
                    else:
                        P.op('dve', lambda e, j=j, half=half, pst=pst: e.tensor_copy(
                            out=otok[:, j, half * 512:(half + 1) * 512], in_=pst[:]),
                            reads=[r_ps], writes=[r_otok])
            t = P.dma('sp', out_d[tb * 512:(tb + 1) * 512, :].rearrange('(j p) d -> p j d', p=128), otok[:],
                      reads=[r_otok], sem=semn)
            if stage is None:
                P.barrier()
                A.release(m)
            return t

        def finish(xT_blocks_fn):
            for tb in range(NT):
                oT, r = xT_blocks_fn(tb)
                store_block(tb, oT, r)
            P.barrier()
            P.emit()

        def phase_filter():
            m = A.mark()
            ktc = A.alloc('ktc', [128, NB, 2, 512], BF16)
            kts = A.alloc('kts', [128, NB, 2, 512], BF16)
            m_mlp = A.mark()
            zf = A.alloc('zf', [33, L + 1], F32)
            w1 = A.alloc('fw1', [33, 64], F32)
            w2 = A.alloc('fw2', [64, 64], F32)
            w3a = A.alloc('fw3a', [65, 2048], F32)
            fv = A.alloc('fv', [64, 4], F32)
            h1 = A.alloc('fh1', [64, L + 1], F32)
            h2a = A.alloc('fh2a', [65, L + 1], F32)
            r_f = Res('filt_in')
            r_h1, r_h2, r_kt = Res('h1'), Res('h2'), Res('kt')
            P.dma('sp', zf[:], c_zf, writes=[r_f], sem='c')
            P.dma('sp', w1[:], ev_filt_w1, writes=[r_f], sem='c')
            P.dma('sp', w2[:], ev_filt_w2, writes=[r_f], sem='c')
            P.dma('sp', w3a[0:64, :], ev_filt_w3, writes=[r_f], sem='c')
            P.dma('sp', w3a[64:65, :], ev_filt_b3.rearrange('(o n) -> o n', o=1), writes=[r_f], sem='c')
            P.dma('sp', fv[:, 0:1], ev_filt_b1.rearrange('(p o) -> p o', o=1), writes=[r_f], sem='c', **NCD)
            P.dma('sp', fv[:, 1:2], ev_filt_b2.rearrange('(p o) -> p o', o=1), writes=[r_f], sem='c', **NCD)
            P.dma('sp', fv[:, 2:4], ev_filt_freq.rearrange('t p -> p t'), writes=[r_f], sem='c', **NCD)
            P.op('dve', lambda e: e.memset(h2a[0:64, :], 0.0), writes=[r_h2])
            P.op('dve', lambda e: e.memset(h2a[64:65, :], 1.0), writes=[r_h2])
            P.op('dve', lambda e: e.memset(h1[:], 0.0), writes=[r_h1])

            fargs = [A.alloc('farg%d' % i, [64, 512], F32) for i in range(2)]
            fkfs = [A.alloc('fkf%d' % i, [64, 512], F32) for i in range(2)]
            r_fargs = [Res('farg0'), Res('farg1')]
            lcnt = [0]

            def layer(wt, K, src, r_src, dst, r_dst, bcol, fcol):
                for tb in range(NT):
                    pst, r_ps = psrot.next()
                    sl = slice(tb * 512, (tb + 1) * 512)
                    MM(pst[0:64, :], wt[0:K, :], src[0:K, sl], True, True, [r_f, r_src], [r_ps])
                    bb = lcnt[0] % 2
                    lcnt[0] += 1
                    arg, kf, r_arg = fargs[bb], fkfs[bb], r_fargs[bb]
                    P.op('dve', lambda e, pst=pst, arg=arg: e.tensor_scalar(
                        out=arg[:], in0=pst[0:64, :], scalar1=fv[:, bcol:bcol + 1], scalar2=fv[:, fcol:fcol + 1],
                        op0=ALU.add, op1=ALU.mult), reads=[r_ps, r_f], writes=[r_arg])
                    P.op('dve', lambda e, arg=arg: e.tensor_scalar(
                        out=arg[:], in0=arg[:], scalar1=float(1.0 / (2 * math.pi)), scalar2=64.0,
                        op0=ALU.mult, op1=ALU.add), reads=[r_arg], writes=[r_arg])
                    P.op('dve', lambda e, arg=arg, kf=kf: e.tensor_scalar(
                        out=kf[:], in0=arg[:], scalar1=8388608.0, scalar2=8388608.0,
                        op0=ALU.add, op1=ALU.subtract), reads=[r_arg], writes=[r_arg])
                    P.op('dve', lambda e, arg=arg, kf=kf: e.tensor_tensor(
                        out=arg[:], in0=arg[:], in1=kf[:], op=ALU.subtract), reads=[r_arg], writes=[r_arg])
                    P.op('act', lambda e, arg=arg, sl=sl: e.activation(
                        out=dst[0:64, sl], in_=arg[:], func=AF.Sin, scale=6.28318),
                        reads=[r_arg, r_const], writes=[r_dst])

            layer(w1, 33, zf, r_f, h1, r_h1, 0, 2)
            layer(w2, 64, h1, r_h1, h2a, r_h2, 1, 3)

            mk_ = A.mark()
            decb = [A.alloc('dec%d' % i, [128, 512], F32) for i in range(2)]
            decshb = [A.alloc('decsh%d' % i, [128, 512], F32) for i in range(2)]
            r_decb = [Res('dec0'), Res('dec1')]
            fdb = [A.alloc('fd%d' % i, [128, 512], F32) for i in range(2)]
            bdb = [A.alloc('bd%d' % i, [128, 512], F32) for i in range(2)]
            r_fdb = [Res('fd0'), Res('fd1')]
            r_bdb = [Res('bd0'), Res('bd1')]
            ki = 0
            for blk in range(NB):
                db_ = blk % 2
                dec, decsh, r_dec = decb[db_], decshb[db_], r_decb[db_]
                P.dma('sp', dec[:], c_dec[blk * 128:(blk + 1) * 128, :], writes=[r_dec], sem='dec%d' % db_)
                P.dma('sp', decsh[:], c_decsh[blk * 128:(blk + 1) * 128, :], writes=[r_dec], sem='dec%d' % db_)
                for n in range(2):
                    psf, r_psf = psrot.next()
                    psb, r_psb = psrot.next()
                    MM(psf[:], h2a[:, blk * 128:(blk + 1) * 128], w3a[:, n * 1024:n * 1024 + 512], True, True,
                       [r_h2, r_f], [r_psf])
                    MM(psb[:], h2a[:, blk * 128 + 1:(blk + 1) * 128 + 1], w3a[:, n * 1024 + 512:n * 1024 + 1024],
                       True, True, [r_h2, r_f], [r_psb])
                    fd, bd, r_fd, r_bd = fdb[ki % 2], bdb[ki % 2], r_fdb[ki % 2], r_bdb[ki % 2]
                    ki += 1
                    P.op('dve', lambda e, psf=psf, fd=fd, dec=dec: e.tensor_tensor(out=fd[:], in0=psf[:], in1=dec[:], op=ALU.mult),
                         reads=[r_psf, r_dec], writes=[r_fd])
                    P.op('dve', lambda e, psb=psb, bd=bd, decsh=decsh: e.tensor_tensor(out=bd[:], in0=psb[:], in1=decsh[:], op=ALU.mult),
                         reads=[r_psb, r_dec], writes=[r_bd])
                    P.op('pool', lambda e, fd=fd, bd=bd, n=n, blk=blk: e.tensor_tensor(
                        out=ktc[:, blk, n, :], in0=fd[:], in1=bd[:], op=ALU.add), reads=[r_fd, r_bd], writes=[r_kt])
                    P.op('dve', lambda e, fd=fd, bd=bd, n=n, blk=blk: e.tensor_tensor(
                        out=kts[:, blk, n, :], in0=bd[:], in1=fd[:], op=ALU.subtract), reads=[r_fd, r_bd], writes=[r_kt])
            A.release(m_mlp)

            cqs = [A.alloc('cqf%d' % i, [128, 16, 128], BF16) for i in range(3)]
            sqs = [A.alloc('sqf%d' % i, [128, 16, 128], BF16) for i in range(3)]
            r_m = [Res('mf0'), Res('mf1'), Res('mf2')]
            kst = [A.alloc('kst%d' % i, [128, 2, 2, 512], BF16) for i in range(3)]
            r_kst = [Res('kst0'), Res('kst1'), Res('kst2')]
            t1s = [A.alloc('kt1_%d' % i, [128, 512], F32) for i in range(4)]
            r_t1s = [Res('kt1_%d' % i) for i in range(4)]
            ti = 0
            def load_f(j):
                b = j % 3
                P.dma('sp', cqs[b][:], c_cqf[j].rearrange('p (i q) -> p i q', q=128), writes=[r_m[b]], sem='mf%d' % b)
                P.dma('sp', sqs[b][:], c_sqf[j].rearrange('p (i q) -> p i q', q=128), writes=[r_m[b]], sem='mf%d' % b)

            load_f(0)
            load_f(1)
            for j in range(16):
                b = j % 3
                if j + 2 < 16:
                    load_f(j + 2)
                for n in range(2):
                    psc, r_psc = psrot.next()
                    pss, r_pss = psrot.next()
                    for i in range(16):
                        MM(psc[:], cqs[b][:, i, :], ktc[:, i, n, :], i == 0, i == 15, [r_m[b], r_kt], [r_psc])
                    for i in range(16):
                        MM(pss[:], sqs[b][:, i, :], kts[:, i, n, :], i == 0, i == 15, [r_m[b], r_kt], [r_pss])
                    cf = cfsf[:, j:j + 1]
                    sf = cfsf[:, 16 + j:17 + j]
                    ta, r_ta = t1s[ti % 4], r_t1s[ti % 4]
                    tb_, r_tb = t1s[(ti + 1) % 4], r_t1s[(ti + 1) % 4]
                    ti += 2
                    P.op('act', lambda e, pss=pss, ta=ta, sf=sf: e.activation(out=ta[:], in_=pss[:], func=AF.Identity, scale=sf),
                         reads=[r_pss, r_cfsf], writes=[r_ta])
                    P.op('act', lambda e, pss=pss, tb_=tb_, cf=cf: e.activation(out=tb_[:], in_=pss[:], func=AF.Identity, scale=cf),
                         reads=[r_pss, r_cfsf], writes=[r_tb])
                    P.op('dve', lambda e, psc=psc, ta=ta, cf=cf, b=b, n=n: e.scalar_tensor_tensor(
                        out=kst[b][:, n, 0, :], in0=psc[:], scalar=cf, in1=ta[:], op0=ALU.mult, op1=ALU.subtract),
                        reads=[r_psc, r_ta, r_cfsf], writes=[r_kst[b]])
                    P.op('dve', lambda e, psc=psc, tb_=tb_, sf=sf, b=b, n=n: e.scalar_tensor_tensor(
                        out=kst[b][:, n, 1, :], in0=psc[:], scalar=sf, in1=tb_[:], op0=ALU.mult, op1=ALU.add),
                        reads=[r_psc, r_tb, r_cfsf], writes=[r_kst[b]])
                P.dma('sp', kspec[j].rearrange('p (a b c) -> p a b c', a=2, b=2), kst[b][:], reads=[r_kst[b]],
                      sem='kst%d' % b)
            P.barrier()
            A.release(m)

        def cast_load(dst_ap, src_ap, r_dst, sem):
            return P.dma('pool', dst_ap, src_ap, writes=[r_dst], sem=sem)

        def phase_layer0_mixer(xT, r_xT):
            m0 = A.mark()
            mixT = A.alloc('mixT', [128, 8, L], BF16)
            r_mix = [[Res('mix%d_%d' % (c, tb)) for tb in range(NT)] for c in range(8)]
            m_h = A.mark()
            hT = A.alloc('hT', [128, 8, L], BF16)
            r_hT = [Res('hT%d' % tb) for tb in range(NT)]
            AXr = Arena(nc, base=xT_off, limit=xT_off + 65536)
            AXr.prog = P
            mm_ = A.mark()
            xtk = [A.alloc('xtok%d' % i, [128, 4, D], F32) for i in range(2)]
            r_xtk = [Res('xtok0'), Res('xtok1')]
            xTbs = [AW.alloc('xTb%d' % i, [128, 8, 512], F32) for i in range(2)]
            r_xTbs = [Res('xTb0'), Res('xTb1')]
            sq1 = [A.alloc('sq%d' % i, [128, 8, 512], BF16) for i in range(2)]
            rs1 = [A.alloc('rs%d' % i, [128, 512], F32) for i in range(2)]
            r_sq1 = [Res('sq0'), Res('sq1')]
            r_rs1 = [Res('rs0'), Res('rs1')]
            for tb in range(NT):
                b = tb % 2
                load_x_block(tb, xTbs[b], r_xTbs[b], stage=(xtk[b], r_xtk[b], 'xin%d' % b))
                rmsnorm_block(xTbs[b][:], r_xTbs[b], 'g_mix0', hT[:, :, tb * 512:(tb + 1) * 512], r_hT[tb],
                              tmp=(sq1[b], rs1[b], r_sq1[b], r_rs1[b]))
            A.release(mm_)

            m2 = A.mark()
            qT = A.alloc('qT', [128, 4, L], BF16)
            kT = A.alloc('kT', [128, L], BF16)
            Vt = A.alloc('Vt', [128, NB, 128], BF16)
            cosT = A.alloc('cosT', [128, L], F32)
            sinT = A.alloc('sinT', [128, L], F32)
            maskt = A.alloc('mask', [128, 384], F32)
            r_tab = Res('tabs')
            r_q, r_k, r_v = Res('qT'), Res('kT'), Res('Vt')
            P.dma('sp', cosT[:], c_cos, writes=[r_tab], sem='c')
            P.dma('sp', sinT[:], c_sin, writes=[r_tab], sem='c')
            P.dma('sp', maskt[:], c_mask, writes=[r_tab], sem='c')
            w_in_v = ev_w_in.rearrange('(kc p) n -> p kc n', p=128)
            rope_t = [A.alloc('rope_t%d' % i, [128, 512], F32) for i in range(4)]
            r_rope = [Res('rope_t%d' % i) for i in range(4)]
            ri = 0
            for ci in range(5):
                for tb in range(NT):
                    sl = slice(tb * 512, (tb + 1) * 512)
                    ps1, r_ps1 = psrot.next()
                    ps2, r_ps2 = psrot.next()
                    for kc in range(8):
                        MM(ps1[:], wq_all[ci][:, kc, :], hT[:, kc, sl], kc == 0, kc == 7, [r_wpre, r_hT[tb]], [r_ps1])
                    for kc in range(8):
                        MM(ps2[:], wr_all[ci][:, kc, :], hT[:, kc, sl], kc == 0, kc == 7, [r_wpre, r_hT[tb]], [r_ps2])
                    t1, r_t1 = rope_t[ri % 4], r_rope[ri % 4]
                    t2, r_t2 = rope_t[(ri + 1) % 4], r_rope[(ri + 1) % 4]
                    ri += 2
                    P.op('dve', lambda e, ps1=ps1, t1=t1, sl=sl: e.tensor_tensor(out=t1[:], in0=ps1[:], in1=cosT[:, sl], op=ALU.mult),
                         reads=[r_ps1, r_tab], writes=[r_t1])
                    P.op('dve', lambda e, ps2=ps2, t2=t2, sl=sl: e.tensor_tensor(out=t2[:], in0=ps2[:], in1=sinT[:, sl], op=ALU.mult),
                         reads=[r_ps2, r_tab], writes=[r_t2])
                    dst = qT[:, ci, sl] if ci < 4 else kT[:, sl]
                    P.op('pool', lambda e, t1=t1, t2=t2, dst=dst: e.tensor_tensor(out=dst, in0=t1[:], in1=t2[:], op=ALU.add),
                         reads=[r_t1, r_t2], writes=[r_q if ci < 4 else r_k])
            wv = wv_pre
            r_wv = r_wpre
            for blk in range(NB):
                pst, r_ps = psrot.next()
                for kc in range(8):
                    MM(pst[:, 0:128], hT[:, kc, blk * 128:(blk + 1) * 128], wv[:, kc, :], kc == 0, kc == 7,
                       [r_hT[blk // 4], r_wv], [r_ps])
                P.op('act', lambda e, pst=pst, blk=blk: e.copy(out=Vt[:, blk, :], in_=pst[:, 0:128]),
                     reads=[r_ps], writes=[r_v])
            P.barrier()

            LEAD = 3
            NR = 6
            sm_b = [A.alloc('sm%d' % i, [128, 384], F32) for i in range(NR)]
            p_b = [A.alloc('pb%d' % i, [128, 384], BF16) for i in range(NR)]
            pt_b = [A.alloc('ptb%d' % i, [128, 384], BF16) for i in range(NR)]
            st_b = [A.alloc('st%d' % i, [128, 8], F32) for i in range(NR)]
            r_sm = [Res('sm%d' % i) for i in range(NR)]
            r_p = [Res('p%d' % i) for i in range(NR)]
            r_pt = [Res('pt%d' % i) for i in range(NR)]
            r_st = [Res('st%d' % i) for i in range(NR)]
            r_st2 = [Res('st2_%d' % i) for i in range(NR)]
            atok = [A.alloc('atok%d' % i, [128, 512], BF16) for i in range(2)]
            r_atok = [[Res('atok%d_%d' % (i, h)) for h in range(8)] for i in range(2)]
            S_ps = Rot(PS[0:3])
            T_ps = Rot(PS[3:5])
            O_ps = [PS[5], PS[6]]
            r_O = [[Res('O%d_%d' % (i, h)) for h in range(8)] for i in range(2)]
            r_Ob = [Res('Obank0'), Res('Obank1')]
            items = [(qb, h) for qb in range(NB) for h in range(8)]

            def geom(qb):
                kbs = [kb for kb in (qb - 1, qb, qb + 1) if 0 <= kb < NB]
                nk = len(kbs)
                return kbs, nk, (kbs[0] - (qb - 1)) * 128, nk * 128, kbs[0] * 128

            def stage_a(i):
                qb, h = items[i]
                kbs, nk, mcol0, W, k0 = geom(qb)
                c, half = h % 4, h // 4
                pr = slice(half * 64, half * 64 + 64)
                bi = i % NR
                sps, r_sps = S_ps.next()
                MM(sps[:, 0:W], qT[pr, c, qb * 128:(qb + 1) * 128], kT[pr, k0:k0 + W], True, True,
                   [r_q, r_k], [r_sps])
                sm, p_, st = sm_b[bi], p_b[bi], st_b[bi]
                P.op('dve', lambda e: e.tensor_tensor(
                    out=sm[:, 0:W], in0=sps[:, 0:W], in1=maskt[:, mcol0:mcol0 + W], op=ALU.add),
                    reads=[r_sps, r_tab], writes=[r_sm[bi]])
                P.op('dve', lambda e: e.tensor_reduce(
                    out=st[:, 0:1], in_=sm[:, 0:W], axis=AX.X, op=ALU.max),
                    reads=[r_sm[bi]], writes=[r_st[bi]])
                P.op('dve', lambda e: e.tensor_scalar(
                    out=st[:, 1:2], in0=st[:, 0:1], scalar1=-0.125, scalar2=nsinkb[:, h:h + 1],
                    op0=ALU.mult, op1=ALU.min), reads=[r_st[bi], r_sink], writes=[r_st[bi]])
                P.op('act', lambda e: e.activation(
                    out=p_[:, 0:W], in_=sm[:, 0:W], func=AF.Exp, bias=st[:, 1:2], scale=0.125,
                    accum_out=st[:, 2:3]), reads=[r_sm[bi], r_st[bi]], writes=[r_p[bi], r_st2[bi]])
                P.op('act', lambda e: e.activation(
                    out=st[:, 3:4], in_=st[:, 1:2], func=AF.Exp, bias=sinkb[:, h:h + 1], scale=1.0),
                    reads=[r_st[bi], r_sink], writes=[r_st2[bi]])

            def stage_b(i):
                qb, h = items[i]
                kbs, nk, mcol0, W, k0 = geom(qb)
                c, half = h % 4, h // 4
                bi = i % NR
                ob = qb % 2
                ops_t, _ = O_ps[ob]
                p_, pt, st = p_b[bi], pt_b[bi], st_b[bi]
                P.op('dve', lambda e: e.tensor_tensor(out=st[:, 4:5], in0=st[:, 2:3], in1=st[:, 3:4], op=ALU.add),
                     reads=[r_st2[bi]], writes=[r_st2[bi]])
                P.op('dve', lambda e: e.reciprocal(out=st[:, 5:6], in_=st[:, 4:5]),
                     reads=[r_st2[bi]], writes=[r_st2[bi]])
                tps, r_tps = T_ps.next()
                tpb = ps_bf(tps)
                for k in range(nk):
                    TR(tpb[:, k * 128:(k + 1) * 128], p_[:, k * 128:(k + 1) * 128], identb[:],
                       [r_p[bi], r_const], [r_tps])
                if h % 2 == 0:
                    P.op('act', lambda e: e.copy(out=pt[:, 0:W], in_=tpb[:, 0:W]),
                         reads=[r_tps], writes=[r_pt[bi]])
                else:
                    P.op('dve', lambda e: e.tensor_copy(out=pt[:, 0:W], in_=tpb[:, 0:W]),
                         reads=[r_tps], writes=[r_pt[bi]])

            def stage_b2(i):
                qb, h = items[i]
                kbs, nk, mcol0, W, k0 = geom(qb)
                c, half = h % 4, h // 4
                bi = i % NR
                ob = qb % 2
                ops_t, _ = O_ps[i % 2]
                p_, pt, st = p_b[bi], pt_b[bi], st_b[bi]
                for k in range(nk):
                    MM(ops_t[:, h * 64:(h + 1) * 64], pt[:, k * 128:(k + 1) * 128],
                       Vt[:, kbs[k], half * 64:half * 64 + 64], k == 0, k == nk - 1,
                       [r_pt[bi], r_v], [r_Ob[i % 2]])
                P.op('act', lambda e: e.activation(
                    out=atok[ob][:, h * 64:(h + 1) * 64], in_=ops_t[:, h * 64:(h + 1) * 64],
                    func=AF.Identity, scale=st[:, 5:6]),
                    reads=[r_Ob[i % 2], r_st2[bi]], writes=[r_atok[ob][h]])
                if h == 7:
                    ps7, r_ps7 = PS[7]
                    p7b = ps_bf(ps7)
                    for cc in range(4):
                        TR(p7b[:, cc * 128:(cc + 1) * 128], atok[ob][:, cc * 128:(cc + 1) * 128], identb[:],
                           [r_atok[ob][2 * cc], r_atok[ob][2 * cc + 1], r_const], [r_ps7])
                    P.op('dve', lambda e: e.tensor_copy(
                        out=mixT[:, 0:4, qb * 128:(qb + 1) * 128],
                        in_=p7b[:, 0:512].rearrange('p (c t) -> p c t', c=4)),
                        reads=[r_ps7], writes=[r_mix[cc_][qb // 4] for cc_ in range(4)])

            n_it = len(items)
            for i in range(min(LEAD, n_it)):
                stage_a(i)
            for i in range(n_it + 1):
                if i + LEAD < n_it:
                    stage_a(i + LEAD)
                if i < n_it:
                    stage_b(i)
                if i >= 1:
                    stage_b2(i - 1)
            P.barrier()
            A.release(m2)
            if stop == 'attn':
                return mixT, r_mix

            g0T = AXr.alloc('g0T', [128, 4, L], BF16)
            g1T = AXr.alloc('g1T', [128, 4, L], BF16)
            zT = AXr.alloc('zT', [128, 4, L], BF16)
            r_g0 = [[Res('g0_%d_%d' % (c, tb)) for tb in range(NT)] for c in range(4)]
            r_g1 = [[Res('g1_%d_%d' % (c, tb)) for tb in range(NT)] for c in range(4)]
            r_z = [[Res('z_%d_%d' % (c, tb)) for tb in range(NT)] for c in range(4)]
            m3 = A.mark()
            wu_ = [A.alloc('wu%d' % i, [128, 8, 128], BF16) for i in range(2)]
            r_wu = [Res('wu0'), Res('wu1')]
            upad = [A.alloc('upad%d' % i, [128, L + 2], F32) for i in range(2)]
            r_up = [Res('upad0'), Res('upad1')]
            t0b = [A.alloc('t0b%d' % i, [128, L], F32) for i in range(2)]
            r_t0 = [Res('t0b0'), Res('t0b1')]
            for i in range(2):
                P.op('dve', lambda e, i=i: e.memset(upad[i][:, 0:1], 0.0), writes=[r_up[i]])
                P.op('dve', lambda e, i=i: e.memset(upad[i][:, L + 1:L + 2], 0.0), writes=[r_up[i]])
            dsts = [(g0T, r_g0), (g1T, r_g1), (zT, r_z)]
            for uc in range(12):
                b = uc % 2
                cast_load(wu_[b][:], w_in_v[:, :, 768 + uc * 128:768 + (uc + 1) * 128], r_wu[b], 'wu%d' % b)
                for tb in range(NT):
                    pst, r_ps = psrot.next()
                    sl = slice(tb * 512, (tb + 1) * 512)
                    for kc in range(8):
                        MM(pst[:], wu_[b][:, kc, :], hT[:, kc, sl], kc == 0, kc == 7, [r_wu[b], r_hT[tb]], [r_ps])
                    P.op('act', lambda e, pst=pst, b=b, tb=tb: e.copy(out=upad[b][:, 1 + tb * 512:1 + (tb + 1) * 512], in_=pst[:]),
                         reads=[r_ps], writes=[r_up[b]])
                dt_, rr = dsts[uc // 4]
                cc = uc % 4
                P.op('act', lambda e, b=b, uc=uc: e.activation(
                    out=t0b[b][:], in_=upad[b][:, 1:L + 1], func=AF.Identity, bias=V('short_b', uc), scale=V('sw1', uc)),
                    reads=[r_up[b], r_vec], writes=[r_t0[b]])
                P.op('dve', lambda e, b=b, uc=uc: e.scalar_tensor_tensor(
                    out=t0b[b][:], in0=upad[b][:, 0:L], scalar=V('sw0', uc), in1=t0b[b][:], op0=ALU.mult, op1=ALU.add),
                    reads=[r_up[b], r_t0[b], r_vec], writes=[r_t0[b]])
                P.op('dve', lambda e, b=b, uc=uc, dt_=dt_, cc=cc: e.scalar_tensor_tensor(
                    out=dt_[:, cc, :], in0=upad[b][:, 2:L + 2], scalar=V('sw2', uc), in1=t0b[b][:], op0=ALU.mult, op1=ALU.add),
                    reads=[r_up[b], r_t0[b], r_vec], writes=rr[cc])
            P.barrier()
            A.release(m_h)

            wo = A.alloc('wo', [128, 8, D], BF16)
            r_wo = Res('wo')
            for kc in range(8):
                cast_load(wo[:, kc, :], ev_w_out[kc * 128:(kc + 1) * 128, :], r_wo, 'wo')
            m_p4 = A.mark()
            ztok = A.alloc('ztok', [128, NB, 512], BF16)
            r_ztok = Res('ztok')
            Yb = A.alloc('Yb', [128, 16, 2, 512], BF16)
            r_Y = Res('Yb')
            for n in range(2):
                for blk in range(NB):
                    pst, r_ps = psrot.next()
                    pb = ps_bf(pst)
                    for cc in range(4):
                        TR(pb[:, cc * 128:(cc + 1) * 128], zT[:, cc, blk * 128:(blk + 1) * 128], identb[:],
                           [r_z[cc][blk // 4], r_const], [r_ps])
                    if blk % 2 == 0:
                        P.op('act', lambda e, pb=pb, blk=blk: e.copy(out=ztok[:, blk, :], in_=pb[:, 0:512]),
                             reads=[r_ps], writes=[r_ztok])
                    else:
                        P.op('dve', lambda e, pb=pb, blk=blk: e.tensor_copy(out=ztok[:, blk, :], in_=pb[:, 0:512]),
                             reads=[r_ps], writes=[r_ztok])
                m4 = A.mark()
                cqs = [A.alloc('cqf%d' % i, [128, 16, 128], BF16) for i in range(3)]
                sqs = [A.alloc('sqf%d' % i, [128, 16, 128], BF16) for i in range(3)]
                ksb = [A.alloc('ksb%d' % i, [128, 2, 512], BF16) for i in range(3)]
                r_m = [Res('mf0'), Res('mf1'), Res('mf2')]
                mt = [A.alloc('mt%d' % i, [128, 512], F32) for i in range(4)]
                r_mt = [Res('mt%d' % i) for i in range(4)]
                for j in range(16):
                    b = j % 3
                    P.dma('sp', cqs[b][:], c_cqf[j].rearrange('p (i q) -> p i q', q=128), writes=[r_m[b]], sem='mf%d' % b)
                    P.dma('sp', sqs[b][:], c_sqf[j].rearrange('p (i q) -> p i q', q=128), writes=[r_m[b]], sem='mf%d' % b)
                    P.dma('sp', ksb[b][:], kspec[j].rearrange('p (a b c) -> p a b c', a=2, b=2)[:, n, :, :],
                          writes=[r_m[b]], sem='mf%d' % b)
                    psa, r_psa = psrot.next()
                    psb, r_psb = psrot.next()
                    for i in range(16):
                        MM(psa[:], cqs[b][:, i, :], ztok[:, i, :], i == 0, i == 15, [r_m[b], r_ztok], [r_psa])
                    for i in range(16):
                        MM(psb[:], sqs[b][:, i, :], ztok[:, i, :], i == 0, i == 15, [r_m[b], r_ztok], [r_psb])
                    kr, ki = ksb[b][:, 0, :], ksb[b][:, 1, :]
                    P.op('dve', lambda e, psa=psa, kr=kr: e.tensor_tensor(out=mt[0][:], in0=psa[:], in1=kr, op=ALU.mult),
                         reads=[r_psa, r_m[b]], writes=[r_mt[0]])
                    P.op('dve', lambda e, psb=psb, ki=ki: e.tensor_tensor(out=mt[1][:], in0=psb[:], in1=ki, op=ALU.mult),
                         reads=[r_psb, r_m[b]], writes=[r_mt[1]])
                    P.op('pool', lambda e, j=j: e.tensor_tensor(out=Yb[:, j, 0, :], in0=mt[0][:], in1=mt[1][:], op=ALU.add),
                         reads=[r_mt[0], r_mt[1]], writes=[r_Y])
                    P.op('dve', lambda e, psb=psb, kr=kr: e.tensor_tensor(out=mt[2][:], in0=psb[:], in1=kr, op=ALU.mult),
                         reads=[r_psb, r_m[b]], writes=[r_mt[2]])
                    P.op('dve', lambda e, psa=psa, ki=ki: e.tensor_tensor(out=mt[3][:], in0=psa[:], in1=ki, op=ALU.mult),
                         reads=[r_psa, r_m[b]], writes=[r_mt[3]])
                    P.op('pool', lambda e, j=j: e.tensor_tensor(out=Yb[:, j, 1, :], in0=mt[2][:], in1=mt[3][:], op=ALU.subtract),
                         reads=[r_mt[2], r_mt[3]], writes=[r_Y])
                P.barrier()
                A.release(m4)
                m5 = A.mark()
                cqh = [A.alloc('cqh%d' % i, [128, 8, 512], BF16) for i in range(2)]
                sqh = [A.alloc('sqh%d' % i, [128, 8, 512], BF16) for i in range(2)]
                r_mh = [Res('mh0'), Res('mh1')]
                yt = [A.alloc('yt%d' % i, [128, 512], F32) for i in range(2)]
                r_yt = [Res('yt0'), Res('yt1')]
                dsn = 'ds%d' % n
                cq_v = c_cq.rearrange('(j p) t -> p j t', p=128)
                sq_v = c_sq.rearrange('(j p) t -> p j t', p=128)

                def load_half(tb, hf_):
                    sl = slice(tb * 512, (tb + 1) * 512)
                    P.dma('sp', cqh[hf_][:], cq_v[:, hf_ * 8:(hf_ + 1) * 8, sl], writes=[r_mh[hf_]], sem='mh%d' % hf_)
                    P.dma('sp', sqh[hf_][:], sq_v[:, hf_ * 8:(hf_ + 1) * 8, sl], writes=[r_mh[hf_]], sem='mh%d' % hf_)

                load_half(0, 0)
                load_half(0, 1)
                for tb in range(NT):
                    sl = slice(tb * 512, (tb + 1) * 512)
                    banks = PS[0:4] if tb % 2 == 0 else PS[4:8]
                    for hf_ in range(2):
                        for cc in range(4):
                            pst, r_ps = banks[cc]
                            for jj in range(8):
                                j = hf_ * 8 + jj
                                MM(pst[:], Yb[:, j, 0, cc * 128:(cc + 1) * 128], cqh[hf_][:, jj, :],
                                   hf_ == 0 and jj == 0, False, [r_Y, r_mh[hf_]], [r_ps])
                            for jj in range(8):
                                j = hf_ * 8 + jj
                                MM(pst[:], Yb[:, j, 1, cc * 128:(cc + 1) * 128], sqh[hf_][:, jj, :],
                                   False, hf_ == 1 and jj == 7, [r_Y, r_mh[hf_]], [r_ps])
                        if tb + 1 < NT:
                            load_half(tb + 1, hf_)
                    for cc in range(4):
                        pst, r_ps = banks[cc]
                        y_, r_y = yt[cc % 2], r_yt[cc % 2]
                        P.op('dve', lambda e, pst=pst, y_=y_, cc=cc, sl=sl, dsn=dsn: e.scalar_tensor_tensor(
                            out=y_[:], in0=zT[:, cc, sl], scalar=V(dsn, cc), in1=pst[:], op0=ALU.mult, op1=ALU.add),
                            reads=[r_ps, r_z[cc][tb], r_vec], writes=[r_y])
                        if n == 0:
                            P.op('pool', lambda e, y_=y_, cc=cc, sl=sl: e.tensor_tensor(
                                out=zT[:, cc, sl], in0=y_[:], in1=g0T[:, cc, sl], op=ALU.mult),
                                reads=[r_y, r_g0[cc][tb]], writes=[r_z[cc][tb]])
                        else:
                            P.op('pool', lambda e, y_=y_, cc=cc, sl=sl: e.tensor_tensor(
                                out=mixT[:, 4 + cc, sl], in0=y_[:], in1=g1T[:, cc, sl], op=ALU.mult),
                                reads=[r_y, r_g1[cc][tb]], writes=[r_mix[4 + cc][tb]])
                P.barrier()
                A.release(m5)
            if stop == 'mix':
                return mixT, r_mix

            A.release(m_p4)
            m6 = A.mark()
            xtk = [A.alloc('xtok%d' % i, [128, 4, D], F32) for i in range(2)]
            r_xtk = [Res('xtok0'), Res('xtok1')]
            xTbs = [A.alloc('xTb%d' % i, [128, 8, 512], F32) for i in range(2)]
            r_xTbs = [Res('xTb0'), Res('xTb1')]
            for tb in range(NT):
                b = tb % 2
                xTb, r_xTb = xTbs[b], r_xTbs[b]
                load_x_block(tb, xTb, r_xTb, stage=(xtk[b], r_xtk[b], 'xin%d' % b))
                sl = slice(tb * 512, (tb + 1) * 512)
                for oc in range(8):
                    pst, r_ps = psrot.next()
                    for kc in range(8):
                        MM(pst[:], wo[:, kc, oc * 128:(oc + 1) * 128], mixT[:, kc, sl], kc == 0, kc == 7,
                           [r_wo, r_mix[kc][tb]], [r_ps])
                    P.op('dve', lambda e, pst=pst, oc=oc, sl=sl, xTb=xTb: e.tensor_tensor(
                        out=xT[:, oc, sl], in0=pst[:], in1=xTb[:, oc, :], op=ALU.add),
                        reads=[r_ps, r_xTb], writes=[r_xT[oc][tb]])
            A.release(m0)
            return None, None

        def norm_all(xT, r_xT, gname, hT, r_hT, hf_cb=None):
            m = A.mark()
            sqs_ = [A.alloc('sq%d' % i, [128, 8, 512], BF16) for i in range(2)]
            rss_ = [A.alloc('rs%d' % i, [128, 512], F32) for i in range(2)]
            r_sqs = [Res('sq0'), Res('sq1')]
            r_rss = [Res('rs0'), Res('rs1')]
            hfs, r_hfs = None, None
            if hf_cb is not None:
                hfs = [A.alloc('hf%d' % i, [128, 8, 512], F32) for i in range(2)]
                r_hfs = [Res('hf0'), Res('hf1')]
            for tb in range(NT):
                sl = slice(tb * 512, (tb + 1) * 512)
                b = tb % 2
                sq, rs, r_sq, r_rs = sqs_[b], rss_[b], r_sqs[b], r_rss[b]
                rx_all = [r_xT[c][tb] for c in range(8)]
                P.op('act', lambda e, sl=sl, sq=sq: e.activation(out=sq[:], in_=xT[:, :, sl], func=AF.Square),
                     reads=rx_all, writes=[r_sq])
                pst, r_ps = psrot.next()
                for c in range(8):
                    MM(pst[:], onesb[:], sq[:, c, :], c == 0, c == 7, [r_sq, r_const], [r_ps])
                P.op('act', lambda e, pst=pst, rs=rs: e.activation(out=rs[:], in_=pst[:], func=AF.Sqrt, bias=epsD[:], scale=1.0),
                     reads=[r_ps, r_const], writes=[r_rs])
                P.op('dve', lambda e, rs=rs: e.reciprocal(out=rs[:], in_=rs[:]), reads=[r_rs], writes=[r_rs])
                for c in range(8):
                    P.op('dve', lambda e, c=c, sl=sl, rs=rs: e.scalar_tensor_tensor(
                        out=hT[:, c, sl], in0=xT[:, c, sl], scalar=V(gname, c), in1=rs[:],
                        op0=ALU.mult, op1=ALU.mult), reads=[r_xT[c][tb], r_rs, r_vec], writes=[r_hT[tb]])
                    if hfs is not None:
                        P.op('dve', lambda e, c=c, sl=sl, rs=rs, hf=hfs[b]: e.scalar_tensor_tensor(
                            out=hf[:, c, :], in0=xT[:, c, sl], scalar=V(gname, c), in1=rs[:],
                            op0=ALU.mult, op1=ALU.mult), reads=[r_xT[c][tb], r_rs, r_vec], writes=[r_hfs[b]])
                if hfs is not None:
                    hf_cb(tb, hfs[b], r_hfs[b])
            A.release(m)

        def swiglu_multi(xT, r_xT, hT, r_hT, experts, G, tag='f', cw_prep=None, before_compute=None):
            m = A.mark()
            NW = 2
            wgb = [A.alloc('wgb%d' % i, [128, 8, G * 128], BF16) for i in range(NW)]
            wub = [A.alloc('wub%d' % i, [128, 8, G * 128], BF16) for i in range(NW)]
            wdb = [A.alloc('wdb%d' % i, [128, G, D], BF16) for i in range(NW)]
            r_w = [Res('w%d' % i) for i in range(NW)]
            r_wd = [Res('wd%d' % i) for i in range(NW)]
            NA = 3
            sg = [A.alloc('sg%d' % i, [128, 512], F32) for i in range(NA)]
            r_sg = [Res('sg%d' % i) for i in range(NA)]
            a1 = [A.alloc('a1_%d' % i, [128, 512], F32) for i in range(NA)]
            r_a1 = [Res('a1_%d' % i) for i in range(NA)]
            NACT = 3
            actb = [A.alloc('actb%d' % i, [128, G, 512], BF16) for i in range(NACT)]
            r_act = [[Res('act%d_%d' % (i, f)) for f in range(G)] for i in range(NACT)]
            GU = Rot(PS[0:4])
            DN = Rot(PS[4:8])
            work = []
            for xi, (wg_d, wu_d, wd_d, nff) in enumerate(experts):
                nch = nff // 128
                assert nch % G == 0
                for g in range(nch // G):
                    work.append((xi, g))
            views = [(wg_d.rearrange('(kc p) f -> p kc f', p=128), wu_d.rearrange('(kc p) f -> p kc f', p=128),
                      wd_d.rearrange('(fc p) d -> p fc d', p=128)) for (wg_d, wu_d, wd_d, nff) in experts]

            def issue_gu(w):
                xi, g = work[w]
                b = w % NW
                wg_v, wu_v, wd_v = views[xi]
                fs = slice(g * G * 128, (g + 1) * G * 128)
                P.dma('pool', wgb[b][:], wg_v[:, :, fs], writes=[r_w[b]], sem='%sw%d' % (tag, b))
                P.dma('pool', wub[b][:], wu_v[:, :, fs], writes=[r_w[b]], sem='%sw%d' % (tag, b))

            def issue_d(w):
                xi, g = work[w]
                b = w % NW
                wg_v, wu_v, wd_v = views[xi]
                P.dma('pool', wdb[b][:], wd_v[:, g * G:(g + 1) * G, :], writes=[r_wd[b]], sem='%sd%d' % (tag, b))

            def issue(w):
                issue_gu(w)
                issue_d(w)

            steps = [(w, tb) for w in range(len(work)) for tb in range(NT)]
            cw_cur = {}
            kcnt = [0]

            def emit_gu(si):
                w, tb = steps[si]
                xi, g = work[w]
                b = w % NW
                if tb == 0:
                    if cw_prep is not None and g == 0:
                        cw_cur[xi] = cw_prep(xi)
                cwb, r_cwb = cw_cur[xi] if cw_prep is not None else (None, None)
                sl = slice(tb * 512, (tb + 1) * 512)
                ab = si % NACT
                for f in range(G):
                    psg, r_psg = GU.next()
                    psu, r_psu = GU.next()
                    for kc in range(8):
                        MM(psg[:], wgb[b][:, kc, f * 128:(f + 1) * 128], hT[:, kc, sl], kc == 0, kc == 7,
                           [r_w[b], r_hT[tb]], [r_psg])
                    for kc in range(8):
                        MM(psu[:], wub[b][:, kc, f * 128:(f + 1) * 128], hT[:, kc, sl], kc == 0, kc == 7,
                           [r_w[b], r_hT[tb]], [r_psu])
                    k = kcnt[0]
                    kcnt[0] += 1
                    s_, r_s = sg[k % NA], r_sg[k % NA]
                    a_, r_a = a1[k % NA], r_a1[k % NA]
                    P.op('act', lambda e, psg=psg, s_=s_: e.activation(out=s_[:], in_=psg[:], func=AF.Silu),
                         reads=[r_psg], writes=[r_s])
                    if cwb is None:
                        P.op('dve', lambda e, psu=psu, s_=s_, ab=ab, f=f: e.tensor_tensor(
                            out=actb[ab][:, f, :], in0=psu[:], in1=s_[:], op=ALU.mult),
                            reads=[r_psu, r_s], writes=[r_act[ab][f]])
                    else:
                        P.op('dve', lambda e, psu=psu, s_=s_, a_=a_: e.tensor_tensor(
                            out=a_[:], in0=psu[:], in1=s_[:], op=ALU.mult),
                            reads=[r_psu, r_s], writes=[r_a])
                        P.op('pool', lambda e, a_=a_, ab=ab, f=f, sl=sl, cwb=cwb: e.tensor_tensor(
                            out=actb[ab][:, f, :], in0=a_[:], in1=cwb[:, sl], op=ALU.mult),
                            reads=[r_a, r_cwb], writes=[r_act[ab][f]])

            def emit_dn(si):
                w, tb = steps[si]
                b = w % NW
                sl = slice(tb * 512, (tb + 1) * 512)
                ab = si % NACT
                for oc in range(8):
                    psd, r_psd = DN.next()
                    for f in range(G):
                        MM(psd[:], wdb[b][:, f, oc * 128:(oc + 1) * 128], actb[ab][:, f, :], f == 0, f == G - 1,
                           [r_wd[b], r_act[ab][f]], [r_psd])
                    P.op('dve', lambda e, psd=psd, oc=oc, sl=sl: e.tensor_tensor(
                        out=xT[:, oc, sl], in0=psd[:], in1=xT[:, oc, sl], op=ALU.add),
                        reads=[r_psd, r_xT[oc][tb]], writes=[r_xT[oc][tb]])

            issue(0)
            if len(work) > 1:
                issue(1)
            if before_compute is not None:
                before_compute()
            emit_gu(0)
            for si in range(len(steps)):
                if si + 1 < len(steps):
                    emit_gu(si + 1)
                    w1_, tb1_ = steps[si + 1]
                    if tb1_ == NT - 1 and w1_ + 2 < len(work):
                        issue_gu(w1_ + 2)
                emit_dn(si)
                w, tb = steps[si]
                if tb == NT - 1 and w + 2 < len(work):
                    issue_d(w + 2)
            P.barrier()
            A.release(m)

        def phase_conformer(xT, r_xT, hT, r_hT):
            m = A.mark()
            gluT = A.alloc('gluT', [128, 8, L + 30], BF16)
            r_glu = [Res('glu%d' % c) for c in range(8)]
            for c in range(8):
                P.op('pool', lambda e, c=c: e.memset(gluT[:, c, 0:15], 0.0), writes=[r_glu[c]])
                P.op('pool', lambda e, c=c: e.memset(gluT[:, c, L + 15:L + 30], 0.0), writes=[r_glu[c]])
            wa = [A.alloc('wa%d' % i, [128, 8, 128], BF16) for i in range(2)]
            wgt = [A.alloc('wgt%d' % i, [128, 8, 128], BF16) for i in range(2)]
            r_w = [Res('pw1_0'), Res('pw1_1')]
            w1v = od_pw1_w.rearrange('(kc p) n -> p kc n', p=128)

            def load_pw1(oc):
                b = oc % 2
                P.dma('pool', wa[b][:], w1v[:, :, oc * 128:(oc + 1) * 128], writes=[r_w[b]], sem='pw1_%d' % b)
                P.dma('pool', wgt[b][:], w1v[:, :, D + oc * 128:D + (oc + 1) * 128], writes=[r_w[b]], sem='pw1_%d' % b)

            load_pw1(0)
            load_pw1(1)
            norm_all(xT, r_xT, 'g_mix1', hT, r_hT)
            dwf = A.alloc('dwf', [128, 31, 8], F32)
            r_dwf = Res('dwf')
            P.dma('sp', dwf[:], od_dw_w.rearrange('j (c p) -> p j c', p=128), writes=[r_dwf], sem='dwf', **NCD)
            w2 = A.alloc('pw2', [128, 8, D], BF16)
            r_w2 = Res('pw2')
            for kc in range(8):
                P.dma('pool', w2[:, kc, :], od_pw2_w[kc * 128:(kc + 1) * 128, :], writes=[r_w2], sem='pw2')
            NDG = 4
            diag = [A.alloc('diag%d' % i, [128, 31, 128], BF16) for i in range(NDG)]
            r_diag = [[Res('diag%d_%d' % (i, j)) for j in range(31)] for i in range(NDG)]
            n_ = [0]

            def build_diag(c, db):
                for j in range(31):
                    n_[0] += 1
                    if n_[0] % 3 != 0:
                        P.op('dve', lambda e, c=c, j=j, db=db: e.tensor_scalar(
                            out=diag[db][:, j, :], in0=identb[:], scalar1=dwf[:, j, c:c + 1], scalar2=None, op0=ALU.mult),
                            reads=[r_dwf, r_const], writes=[r_diag[db][j]])
                    else:
                        P.op('act', lambda e, c=c, j=j, db=db: e.activation(
                            out=diag[db][:, j, :], in_=identb[:], func=AF.Copy, scale=dwf[:, j, c:c + 1]),
                            reads=[r_dwf, r_const], writes=[r_diag[db][j]])
            m1 = A.mark()
            sgb = [A.alloc('sgb%d' % i, [128, 512], F32) for i in range(2)]
            r_sgb = [Res('sgb0'), Res('sgb1')]
            k = 0
            for oc in range(8):
                b = oc % 2
                for tb in range(NT):
                    sl = slice(tb * 512, (tb + 1) * 512)
                    psa, r_psa = psrot.next()
                    psg, r_psg = psrot.next()
                    for kc in range(8):
                        MM(psa[:], wa[b][:, kc, :], hT[:, kc, sl], kc == 0, kc == 7, [r_w[b], r_hT[tb]], [r_psa])
                    for kc in range(8):
                        MM(psg[:], wgt[b][:, kc, :], hT[:, kc, sl], kc == 0, kc == 7, [r_w[b], r_hT[tb]], [r_psg])
                    s_, r_s = sgb[k % 2], r_sgb[k % 2]
                    k += 1
                    P.op('act', lambda e, psg=psg, s_=s_, oc=oc: e.activation(
                        out=s_[:], in_=psg[:], func=AF.Sigmoid, bias=V('pw1_b', 8 + oc), scale=1.0),
                        reads=[r_psg, r_vec], writes=[r_s])
                    P.op('dve', lambda e, psa=psa, s_=s_, oc=oc, tb=tb: e.scalar_tensor_tensor(
                        out=gluT[:, oc, 15 + tb * 512:15 + (tb + 1) * 512], in0=psa[:], scalar=V('pw1_b', oc),
                        in1=s_[:], op0=ALU.add, op1=ALU.mult), reads=[r_psa, r_s, r_vec], writes=[r_glu[oc]])
                if oc + 2 < 8:
                    load_pw1(oc + 2)
                if 4 <= oc < 4 + (NDG - 1):
                    build_diag(oc - 4, oc - 4)
            P.barrier()
            A.release(m1)
            m2 = A.mark()
            AH = Arena(nc, base=hT2_off, limit=hT2_off + 32768)
            dwv = AH.alloc('dwv', [128, 8, 512], F32)
            sqv = AH.alloc('sqv', [128, 8, 512], BF16)
            r_dwv = [Res('dwv%d' % c) for c in range(8)]
            r_sqv = [Res('sqv%d' % c) for c in range(8)]
            mean = A.alloc('mean', [128, 512], F32)
            rstd = A.alloc('rstd', [128, 512], F32)
            var = A.alloc('var', [128, 512], F32)
            r_stat = Res('stat')
            swT = AH.alloc('swT', [128, 8, 512], BF16)
            r_sw = [Res('sw%d' % c) for c in range(8)]
            dtmp = [A.alloc('dtmp%d' % i, [128, 512], F32) for i in range(2)]
            r_dt = [Res('dtmp0'), Res('dtmp1')]
            CV = Rot(PS[0:3])
            citems = [(tb, c) for tb in range(NT) for c in range(8)]

            def ln_chain(tb):
                ps_s, r_pss = PS[3]
                ps_q, r_psq = PS[4]
                for c in range(8):
                    MM(ps_s[:], onesf[:], dwv[:, c, :], c == 0, c == 7, [r_const, r_dwv[c]], [r_pss])
                for c in range(8):
                    MM(ps_q[:], onesb[:], sqv[:, c, :], c == 0, c == 7, [r_const, r_sqv[c]], [r_psq])
                P.op('dve', lambda e: e.tensor_scalar(out=mean[:], in0=ps_s[:], scalar1=1.0 / D, scalar2=None, op0=ALU.mult),
                     reads=[r_pss], writes=[r_stat])
                P.op('dve', lambda e: e.tensor_tensor(out=var[:], in0=mean[:], in1=mean[:], op=ALU.mult),
                     reads=[r_stat], writes=[r_stat])
                P.op('dve', lambda e: e.scalar_tensor_tensor(out=var[:], in0=ps_q[:], scalar=1.0 / D, in1=var[:],
                                                             op0=ALU.mult, op1=ALU.subtract),
                     reads=[r_psq, r_stat], writes=[r_stat])
                P.op('act', lambda e: e.activation(out=rstd[:], in_=var[:], func=AF.Sqrt, bias=epsT[:], scale=1.0),
                     reads=[r_stat, r_const], writes=[r_stat])
                P.op('dve', lambda e: e.reciprocal(out=rstd[:], in_=rstd[:]), reads=[r_stat], writes=[r_stat])
                for c in range(8):
                    d_, r_d = dtmp[c % 2], r_dt[c % 2]
                    P.op('dve', lambda e, c=c, d_=d_: e.tensor_tensor(out=d_[:], in0=dwv[:, c, :], in1=mean[:], op=ALU.subtract),
                         reads=[r_dwv[c], r_stat], writes=[r_d])
                    P.op('pool', lambda e, d_=d_: e.tensor_tensor(out=d_[:], in0=d_[:], in1=rstd[:], op=ALU.mult),
                         reads=[r_d, r_stat], writes=[r_d])
                    P.op('act', lambda e, c=c, d_=d_: e.activation(out=swT[:, c, :], in_=d_[:], func=AF.Silu,
                                                                   bias=V('ln_b', c), scale=V('ln_g', c)),
                         reads=[r_d, r_vec], writes=[r_sw[c]])

            def pw2_mm(tb):
                sl = slice(tb * 512, (tb + 1) * 512)
                for oc in range(8):
                    pst, r_ps = PS[5 + oc % 3]
                    for kc in range(8):
                        MM(pst[:], w2[:, kc, oc * 128:(oc + 1) * 128], swT[:, kc, :], kc == 0, kc == 7,
                           [r_w2, r_sw[kc]], [r_ps])
                    P.op('dve', lambda e, pst=pst, oc=oc, sl=sl: e.scalar_tensor_tensor(
                        out=xT[:, oc, sl], in0=pst[:], scalar=V('pw2_b', oc), in1=xT[:, oc, sl],
                        op0=ALU.add, op1=ALU.add), reads=[r_ps, r_vec, r_xT[oc][tb]], writes=[r_xT[oc][tb]])

            LA = NDG - 1
            pending_pw2 = None
            for i, (tb, c) in enumerate(citems):
                pst, r_ps = CV.next()
                db = i % NDG
                for j in range(31):
                    MM(pst[:], diag[db][:, j, :], gluT[:, c, tb * 512 + j:tb * 512 + j + 512], j == 0, j == 30,
                       [r_diag[db][j], r_glu[c]], [r_ps])
                if i + LA < len(citems):
                    build_diag(citems[i + LA][1], (i + LA) % NDG)
                if pending_pw2 is not None and c == 1:
                    pw2_mm(pending_pw2)
                    pending_pw2 = None
                P.op('act', lambda e, pst=pst, c=c: e.activation(out=dwv[:, c, :], in_=pst[:], func=AF.Identity,
                                                                 bias=V('dw_b', c), scale=1.0),
                     reads=[r_ps, r_vec], writes=[r_dwv[c]])
                P.op('act', lambda e, pst=pst, c=c: e.activation(out=sqv[:, c, :], in_=pst[:], func=AF.Square,
                                                                 bias=V('dw_b', c), scale=1.0),
                     reads=[r_ps, r_vec], writes=[r_sqv[c]])
                if c == 7:
                    ln_chain(tb)
                    pending_pw2 = tb
            pw2_mm(pending_pw2)
            P.barrier()
            A.release(m2)
            A.release(m)

        def phase_moe(xT, r_xT, hT, r_hT):
            m = A.mark()
            rt = A.alloc('router', [128, 8, NE], F32)
            r_rt = Res('router')
            P.dma('sp', rt[:], od_router.rearrange('(kc p) e -> p kc e', p=128), writes=[r_rt], sem='c')
            cw = A.alloc('cw', [128, NB, NE], F32)
            r_cw = Res('cw')
            cwT = A.alloc('cwT', [8, L], F32)
            r_cwT = Res('cwT')
            sel = A.alloc('sel', [8, 8 * 128], F32)
            P.dma('sp', sel[:], c_sel, writes=[r_rt], sem='c')
            lgall = A.alloc('lgall', [128, NB, NE], F32)
            r_lg = [Res('lg%d' % i) for i in range(NB)]
            rsc = A.alloc('rsc', [128, 8, NB], F32)
            r_rsc = Res('rsc')
            eq1 = A.alloc('eq1', [128, NB, NE], F32)
            eq2 = A.alloc('eq2', [128, NB, NE], F32)
            l2 = A.alloc('l2', [128, NB, NE], F32)
            r_eq = Res('eq')

            def route(tb, hf, r_hf):
                for j in range(4):
                    blk = tb * 4 + j
                    pst, r_ps = psrot.next()
                    for kc in range(8):
                        MM(pst[:, 0:NE], hf[:, kc, j * 128:(j + 1) * 128], rt[:, kc, :], kc == 0, kc == 7,
                           [r_hf, r_rt], [r_ps])
                    P.op('act', lambda e, pst=pst, blk=blk: e.copy(out=lgall[:, blk, :], in_=pst[:, 0:NE]),
                         reads=[r_ps], writes=[r_lg[blk]])

            def route_finish():
                M1, M2, DL, EX, DEN, G1, G2 = [rsc[:, i, :] for i in range(7)]
                P.op('dve', lambda e: e.tensor_reduce(out=M1, in_=lgall[:], axis=AX.X, op=ALU.max),
                     reads=r_lg, writes=[r_rsc])
                for blk in range(NB):
                    P.op('dve', lambda e, blk=blk: e.tensor_scalar(out=eq1[:, blk, :], in0=lgall[:, blk, :],
                                                                   scalar1=rsc[:, 0, blk:blk + 1], scalar2=None, op0=ALU.is_equal),
                         reads=[r_rsc, r_lg[blk]], writes=[r_eq])
                P.op('dve', lambda e: e.scalar_tensor_tensor(out=l2[:], in0=eq1[:], scalar=-1e30, in1=lgall[:],
                                                             op0=ALU.mult, op1=ALU.add), reads=[r_eq] + r_lg, writes=[r_eq])
                P.op('dve', lambda e: e.tensor_reduce(out=M2, in_=l2[:], axis=AX.X, op=ALU.max), reads=[r_eq], writes=[r_rsc])
                for blk in range(NB):
                    P.op('dve', lambda e, blk=blk: e.tensor_scalar(out=eq2[:, blk, :], in0=l2[:, blk, :],
                                                                   scalar1=rsc[:, 1, blk:blk + 1], scalar2=None, op0=ALU.is_equal),
                         reads=[r_rsc, r_eq], writes=[r_eq])
                P.op('dve', lambda e: e.tensor_tensor(out=DL, in0=M2, in1=M1, op=ALU.subtract), reads=[r_rsc], writes=[r_rsc])
                P.op('act', lambda e: e.activation(out=EX, in_=DL, func=AF.Exp), reads=[r_rsc], writes=[r_rsc])
                P.op('dve', lambda e: e.tensor_scalar(out=DEN, in0=EX, scalar1=1.0, scalar2=None, op0=ALU.add),
                     reads=[r_rsc], writes=[r_rsc])
                P.op('dve', lambda e: e.reciprocal(out=G1, in_=DEN), reads=[r_rsc], writes=[r_rsc])
                P.op('dve', lambda e: e.tensor_tensor(out=G2, in0=EX, in1=G1, op=ALU.mult), reads=[r_rsc], writes=[r_rsc])
                for blk in range(NB):
                    P.op('dve', lambda e, blk=blk: e.tensor_scalar(out=eq2[:, blk, :], in0=eq2[:, blk, :],
                                                                   scalar1=rsc[:, 6, blk:blk + 1], scalar2=None, op0=ALU.mult),
                         reads=[r_rsc, r_eq], writes=[r_eq])
                    P.op('dve', lambda e, blk=blk: e.scalar_tensor_tensor(
                        out=cw[:, blk, :], in0=eq1[:, blk, :], scalar=rsc[:, 5, blk:blk + 1], in1=eq2[:, blk, :],
                        op0=ALU.mult, op1=ALU.add), reads=[r_rsc, r_eq], writes=[r_cw])
                for q4 in range(4):
                    ps2, r_ps2 = psrot.next()
                    for j in range(4):
                        blk = q4 * 4 + j
                        TR(ps2[0:8, j * 128:(j + 1) * 128], cw[:, blk, :], ident[:], [r_cw, r_const], [r_ps2])
                    P.op('dve', lambda e, ps2=ps2, q4=q4: e.tensor_copy(out=cwT[:, q4 * 512:(q4 + 1) * 512], in_=ps2[0:8, :]),
                         reads=[r_ps2], writes=[r_cwT])

            norm_all(xT, r_xT, 'g_ffn1', hT, r_hT, hf_cb=route)
            route_finish()
            cwb = [A.alloc('cwb%d' % i, [128, L], F32) for i in range(2)]
            r_cwb = [Res('cwb0'), Res('cwb1')]

            def cw_prep(ex):
                b = ex % 2
                for tb in range(NT):
                    pst, r_ps = PS[4 + tb]
                    MM(pst[:], sel[:, ex * 128:(ex + 1) * 128], cwT[:, tb * 512:(tb + 1) * 512], True, True,
                       [r_rt, r_cwT], [r_ps])
                    P.op('act', lambda e, pst=pst, b=b, tb=tb: e.copy(out=cwb[b][:, tb * 512:(tb + 1) * 512], in_=pst[:]),
                         reads=[r_ps], writes=[r_cwb[b]])
                return cwb[b], r_cwb[b]

            swiglu_multi(xT, r_xT, hT, r_hT,
                         [(od_moe_wg[ex], od_moe_wu[ex], od_moe_wd[ex], FFE) for ex in range(NE)], 4,
                         tag='m', cw_prep=cw_prep)
            A.release(m)

        xT = A.alloc('xT', [128, 8, L], F32)
        xT_off = A.last_off
        r_xT = [[Res('xT%d_%d' % (c, tb)) for tb in range(NT)] for c in range(8)]

        AW = Arena(nc, base=xT_off, limit=xT_off + 65536)
        w_in_v0 = ev_w_in.rearrange('(kc p) n -> p kc n', p=128)
        wq_all = [AW.alloc('wqa%d' % i, [128, 8, 128], BF16) for i in range(5)]
        wr_all = [AW.alloc('wra%d' % i, [128, 8, 128], BF16) for i in range(5)]
        wv_pre = AW.alloc('wvp', [128, 8, 128], BF16)
        r_wpre = Res('wpre')
        if stop != 'in':
            for ci in range(5):
                if ci < 4:
                    runs_p = [(0, ci * 64, 64), (64, (ci + 4) * 64, 64)]
                    runs_r = []
                    for hi, h in enumerate((ci, ci + 4)):
                        runs_r.append((hi * 64, h * 64 + 32, 32))
                        runs_r.append((hi * 64 + 32, h * 64, 32))
                else:
                    runs_p = [(0, 512, 128)]
                    runs_r = [(0, 512 + 32, 32), (32, 512, 32), (64, 576 + 32, 32), (96, 576, 32)]
                for (o, s_, n) in runs_p:
                    P.dma('pool', wq_all[ci][:, :, o:o + n], w_in_v0[:, :, s_:s_ + n], writes=[r_wpre], sem='wpre')
                for (o, s_, n) in runs_r:
                    P.dma('pool', wr_all[ci][:, :, o:o + n], w_in_v0[:, :, s_:s_ + n], writes=[r_wpre], sem='wpre')
            P.dma('pool', wv_pre[:], w_in_v0[:, :, 640:768], writes=[r_wpre], sem='wpre')

        phase_filter_needed = stop not in ('attn', 'in')
        if phase_filter_needed:
            phase_filter()

        if stop == 'in':
            for tb in range(NT):
                r = Res('xTb')
                load_x_block(tb, xT[:, :, tb * 512:(tb + 1) * 512], r)
                for c in range(8):
                    r_xT[c][tb] = r
            finish(lambda tb: (xT[:, :, tb * 512:(tb + 1) * 512], r_xT[0][tb]))
            return nc

        mixT, r_mix = phase_layer0_mixer(xT, r_xT)
        if stop in ('attn', 'mix'):
            for tb in range(NT):
                sl = slice(tb * 512, (tb + 1) * 512)
                for c in range(8):
                    P.op('dve', lambda e, c=c, sl=sl: e.tensor_copy(out=xT[:, c, sl], in_=mixT[:, c, sl]),
                         reads=[r_mix[c][tb]], writes=[r_xT[c][tb]])
            P.barrier()

        def xblk(tb):
            r = Res('xall')
            return xT[:, :, tb * 512:(tb + 1) * 512], r

        if stop in ('attn', 'mix', 'l0mix'):
            P.barrier()
            finish(xblk)
            return nc

        hT = A.alloc('hT2', [128, 8, L], BF16)
        hT2_off = A.last_off
        r_hT = [Res('hT2_%d' % tb) for tb in range(NT)]
        swiglu_multi(xT, r_xT, hT, r_hT, [(ev_ffn_wg, ev_ffn_wu, ev_ffn_wd, FF)], 2, tag='f',
                     before_compute=lambda: norm_all(xT, r_xT, 'g_ffn0', hT, r_hT))
        if stop == 'l0':
            P.barrier()
            finish(xblk)
            return nc
        phase_conformer(xT, r_xT, hT, r_hT)
        if stop == 'conf':
            P.barrier()
            finish(xblk)
            return nc
        phase_moe(xT, r_xT, hT, r_hT)
        if stop == 'moe':
            P.barrier()
            finish(xblk)
            return nc
        P.barrier()
        AF2 = Arena(nc, base=hT2_off, limit=hT2_off + 32768)
        oTs = [AF2.alloc('oT%d' % i, [128, 8, 512], F32) for i in range(2)]
        r_oTs = [Res('oT0'), Res('oT1')]
        sqf = [A.alloc('sq%d' % i, [128, 8, 512], BF16) for i in range(2)]
        rsf = [A.alloc('rs%d' % i, [128, 512], F32) for i in range(2)]
        r_sqf = [Res('sq0'), Res('sq1')]
        r_rsf = [Res('rs0'), Res('rs1')]
        otk = [A.alloc('otok%d' % i, [128, 4, D], F32) for i in range(2)]
        r_otk = [Res('otok0'), Res('otok1')]
        for tb in range(NT):
            b = tb % 2
            sl = slice(tb * 512, (tb + 1) * 512)
            sq, rs, oT = sqf[b], rsf[b], oTs[b]
            r_sq, r_rs, r_o = r_sqf[b], r_rsf[b], r_oTs[b]
            P.op('act', lambda e, sl=sl, sq=sq: e.activation(out=sq[:], in_=xT[:, :, sl], func=AF.Square), writes=[r_sq])
            pst, r_ps = psrot.next()
            for c in range(8):
                MM(pst[:], onesb[:], sq[:, c, :], c == 0, c == 7, [r_sq, r_const], [r_ps])
            P.op('act', lambda e, pst=pst, rs=rs: e.activation(out=rs[:], in_=pst[:], func=AF.Sqrt, bias=epsD[:], scale=1.0),
                 reads=[r_ps, r_const], writes=[r_rs])
            P.op('dve', lambda e, rs=rs: e.reciprocal(out=rs[:], in_=rs[:]), reads=[r_rs], writes=[r_rs])
            for c in range(8):
                P.op('dve', lambda e, c=c, sl=sl, oT=oT, rs=rs: e.scalar_tensor_tensor(
                    out=oT[:, c, :], in0=xT[:, c, sl], scalar=V('g_fin', c), in1=rs[:],
                    op0=ALU.mult, op1=ALU.mult), reads=[r_rs, r_vec], writes=[r_o])
            store_block(tb, oT[:], r_o, stage=(otk[b], r_otk[b], 'xout%d' % b))
        P.barrier()
        P.emit()
    return nc


_CONST = {}


def _bf16(a):
    return np.ascontiguousarray(a.astype(ml_dtypes.bfloat16))


def host_constants():
    if _CONST:
        return _CONST
    c = {}
    c['c_ident'] = np.eye(128, dtype=np.float32)
    half = 32
    inv = 10000.0 ** (-np.arange(half, dtype=np.float32) / half)
    pos = np.arange(L, dtype=np.float32)
    ang = pos[None, :] * inv[:, None]
    cos = np.cos(ang).astype(np.float32)
    sin = np.sin(ang).astype(np.float32)
    cos64 = np.concatenate([cos, cos], 0)
    sin64 = np.concatenate([-sin, sin], 0)
    c['c_cos'] = np.ascontiguousarray(np.concatenate([cos64, cos64], 0))
    c['c_sin'] = np.ascontiguousarray(np.concatenate([sin64, sin64], 0))
    r = np.arange(128)[:, None]
    j = np.arange(384)[None, :]
    rel = j - 128 - r
    c['c_mask'] = np.where(np.abs(rel) <= 128, 0.0, -30000.0).astype(np.float32)
    bands = 16
    l = np.arange(L + 1, dtype=np.float64)
    t = (l / (L - 1))
    w = 2.0 * math.pi * l / L
    fb = np.linspace(1e-4, bands - 1, bands)
    zw = fb[None, :] * w[:, None]
    z = np.concatenate([t[:, None], np.cos(zw), -np.sin(zw)], axis=-1)
    c['c_zf'] = np.ascontiguousarray(z.T.astype(np.float32))
    min_decay = math.log(1e-2) / 1.5
    max_decay = math.log(1e-2) / 0.3
    deltas = np.abs(np.linspace(min_decay, max_decay, 512))
    tt = np.linspace(0.0, 1.0, L)
    dec = np.exp(-tt[:, None] * deltas[None, :])
    decsh = np.zeros_like(dec)
    decsh[:L - 1] = dec[1:]
    c['c_dec'] = dec.astype(np.float32)
    c['c_decsh'] = decsh.astype(np.float32)
    N = 2 * L
    a = np.arange(L, dtype=np.float64) + 0.5
    psi = 2.0 * math.pi * np.outer(a, a) / N
    cq = _bf16(np.cos(psi))
    sq = _bf16(np.sin(psi))
    c['c_cq'] = cq
    c['c_sq'] = sq

    def fwd_layout(m):
        return np.ascontiguousarray(m.reshape(16, 128, 16, 128).transpose(2, 1, 0, 3).reshape(16, 128, 2048))
    c['c_cqf'] = fwd_layout(cq)
    c['c_sqf'] = fwd_layout(sq)
    phi = 2.0 * math.pi * (np.arange(L) + 0.5) / N
    cf = (2.0 / N) * np.cos(phi / 2)
    sf = (2.0 / N) * np.sin(phi / 2)
    c['c_cfsf'] = np.ascontiguousarray(np.concatenate([cf.reshape(16, 128).T, sf.reshape(16, 128).T], 1).astype(np.float32))
    sel = np.zeros((8, 8, 128), np.float32)
    for e in range(8):
        sel[e, e, :] = 1.0
    c['c_sel'] = sel.reshape(8, 1024)
    _CONST.update(c)
    return _CONST


_NC_CACHE = {}


def kernel(**inputs):
    stop = inputs.pop('_stop', 'full')
    cores = inputs.pop('_cores', list(range(8)))
    if stop not in _NC_CACHE:
        _NC_CACHE[stop] = build_program(stop)
    nc = _NC_CACHE[stop]
    consts = host_constants()
    shared = {}
    for k, v in inputs.items():
        if k == 'x':
            continue
        a = np.asarray(v, dtype=np.float32)
        if k != 'final_norm':
            a = a.reshape(a.shape[1:])
        shared[k] = np.ascontiguousarray(a)
    shared.update(consts)
    x = np.asarray(inputs['x'], dtype=np.float32)
    in_maps = []
    for b in cores:
        d = dict(shared)
        d['x'] = np.ascontiguousarray(x[b])
        in_maps.append(d)
    res = run_bass_kernel_spmd(nc, in_maps, core_ids=list(range(len(cores))))
    out = np.stack([r['out'] for r in res.results], axis=0)
    return out.astype(np.float32)
```

```python
import math
from contextlib import ExitStack
import numpy as np
import ml_dtypes
import concourse.bass as bass
import concourse.mybir as mybir
from concourse.bass_utils import run_bass_kernel_spmd

F32 = mybir.dt.float32
BF16 = mybir.dt.bfloat16
ALU = mybir.AluOpType
AF = mybir.ActivationFunctionType
AX = mybir.AxisListType

ENGS = ('pe', 'act', 'dve', 'pool', 'sp')
EPOCH = 24000

D = 1024
L = 2048
NB = 16
NT = 4
FF = 2816
FFE = 3584
NE = 8
EPS = 1e-6


class Res:
    __slots__ = ('name', 'w', 'r')

    def __init__(self, name):
        self.name = name
        self.w = None
        self.r = {}


class Tok:
    __slots__ = ('sem', 'val', 'eng')

    def __init__(self, sem, val, eng):
        self.sem = sem
        self.val = val
        self.eng = eng


class Prog:
    def __init__(self, nc, es, same_engine_raw=True):
        self.nc = nc
        self.es = es
        self.ops = {e: [] for e in ENGS}
        self.count = {e: 0 for e in ENGS}
        self.epoch = {e: 0 for e in ENGS}
        self.csem = {}
        for e in ('pe', 'act', 'dve', 'pool'):
            self.csem[e] = [es.enter_context(nc.semaphore('c_%s_0' % e))]
        self.waited = {e: {} for e in ENGS}
        self.dsem = {}
        self.same_engine_raw = same_engine_raw
        self.all_toks = {e: None for e in ENGS}

    def dma_sem(self, name):
        if name not in self.dsem:
            self.dsem[name] = [self.es.enter_context(self.nc.semaphore('d_' + name)), 0]
        return self.dsem[name]

    def _need(self, eng, tok, waits, raw):
        if tok is None:
            return
        if tok.eng == eng:
            if not (raw and self.same_engine_raw and eng in ('act', 'dve', 'pool')):
                return
        k = id(tok.sem)
        if self.waited[eng].get(k, 0) >= tok.val:
            return
        if k in waits:
            if waits[k][1] < tok.val:
                waits[k] = (tok.sem, tok.val)
        else:
            waits[k] = (tok.sem, tok.val)

    def _hazards(self, eng, reads, writes):
        waits = {}
        for r in reads:
            self._need(eng, r.w, waits, True)
        for w in writes:
            self._need(eng, w.w, waits, False)
            for t in w.r.values():
                self._need(eng, t, waits, False)
        for k, (s, v) in waits.items():
            self.waited[eng][k] = v
        return list(waits.values())

    def op(self, eng, fn, reads=(), writes=()):
        waits = self._hazards(eng, reads, writes)
        self.count[eng] += 1
        if self.count[eng] > EPOCH:
            self.epoch[eng] += 1
            self.csem[eng].append(self.es.enter_context(
                self.nc.semaphore('c_%s_%d' % (eng, self.epoch[eng]))))
            self.count[eng] = 1
        sem = self.csem[eng][-1]
        tok = Tok(sem, self.count[eng], eng)
        for r in reads:
            r.r[eng] = tok
        for w in writes:
            w.w = tok
            w.r = {}
        self.ops[eng].append((waits, fn, (sem, 1)))
        self.all_toks[eng] = tok
        return tok

    def dma(self, q, out, in_, reads=(), writes=(), sem='dma', **kw):
        waits = self._hazards(q, reads, writes)
        s = self.dma_sem(sem)
        s[1] += 16
        tok = Tok(s[0], s[1], None)
        key = 'dma_' + sem
        for r in reads:
            r.r[key] = tok
        for w in writes:
            w.w = tok
            w.r = {}

        def fn(e, out=out, in_=in_, kw=kw):
            return e.dma_start(out=out, in_=in_, **kw)
        self.ops[q].append((waits, fn, (s[0], 16)))
        return tok

    def barrier(self):
        toks = [t for t in self.all_toks.values() if t is not None]
        for name, (s, v) in self.dsem.items():
            if v > 0:
                toks.append(Tok(s, v, None))
        for e in ENGS:
            waits = {}
            for t in toks:
                if t.eng == e:
                    continue
                self._need(e, t, waits, True)
            for k, (s, v) in waits.items():
                self.waited[e][k] = v
            if waits:
                self.ops[e].append((list(waits.values()), None, None))

    def emit(self):
        nc = self.nc
        with nc.Block() as block:
            def mk(ename):
                def body(e):
                    for waits, fn, inc in self.ops[ename]:
                        for (s, v) in waits:
                            e.wait_ge(s, v)
                        if fn is not None:
                            ins = fn(e)
                            if inc is not None:
                                ins.then_inc(inc[0], inc[1])
                return body
            block.tensor(mk('pe'))
            block.scalar(mk('act'))
            block.vector(mk('dve'))
            block.gpsimd(mk('pool'))
            block.sync(mk('sp'))


class Arena:
    def __init__(self, nc, base=17408, limit=224 * 1024 - 256):
        self.nc = nc
        self.top = base
        self.limit = limit
        self.n = 0
        self.peak = 0
        self.prog = None

    def alloc(self, name, shape, dtype):
        free = int(np.prod(shape[1:]))
        nbytes = free * (4 if dtype == F32 else 2)
        off = (self.top + 63) // 64 * 64
        self.n += 1
        t = self.nc.alloc_sbuf_tensor_at('%s_%d' % (name, self.n), list(shape), dtype, offset=off)
        self.last_off = off
        self.top = off + nbytes
        self.peak = max(self.peak, self.top)
        assert self.top <= self.limit, ('SBUF overflow', name, self.top)
        return t

    def mark(self):
        return self.top

    def release(self, m):
        if self.prog is not None:
            self.prog.barrier()
        self.top = m


class Rot:
    def __init__(self, items):
        self.items = items
        self.i = 0

    def next(self):
        it = self.items[self.i % len(self.items)]
        self.i += 1
        return it


def build_program(stop='full'):
    import os
    SKIP = set(os.environ.get('KSKIP', '').split(','))
    nc = bass.Bass("TRN2", target_bir_lowering=False)

    def din(name, shape, dt=F32):
        return nc.dram_tensor(name, list(shape), dt, kind="ExternalInput").ap()

    x_d = din('x', [L, D])
    out_d = nc.dram_tensor('out', [L, D], F32, kind="ExternalOutput").ap()
    ev_norm_mix = din('ev_norm_mix', [D])
    ev_w_in = din('ev_w_in', [D, 2304])
    ev_sink = din('ev_sink', [8])
    ev_short_w = din('ev_short_w', [3, 1536])
    ev_short_b = din('ev_short_b', [1536])
    ev_filt_w1 = din('ev_filt_w1', [33, 64])
    ev_filt_b1 = din('ev_filt_b1', [64])
    ev_filt_w2 = din('ev_filt_w2', [64, 64])
    ev_filt_b2 = din('ev_filt_b2', [64])
    ev_filt_w3 = din('ev_filt_w3', [64, 2048])
    ev_filt_b3 = din('ev_filt_b3', [2048])
    ev_filt_freq = din('ev_filt_freq', [2, 64])
    ev_dskip = din('ev_dskip', [2, 512])
    ev_w_out = din('ev_w_out', [D, D])
    ev_norm_ffn = din('ev_norm_ffn', [D])
    ev_ffn_wg = din('ev_ffn_wg', [D, FF])
    ev_ffn_wu = din('ev_ffn_wu', [D, FF])
    ev_ffn_wd = din('ev_ffn_wd', [FF, D])
    od_norm_mix = din('od_norm_mix', [D])
    od_pw1_w = din('od_pw1_w', [D, 2048])
    od_pw1_b = din('od_pw1_b', [2048])
    od_dw_w = din('od_dw_w', [31, D])
    od_dw_b = din('od_dw_b', [D])
    od_ln_g = din('od_ln_g', [D])
    od_ln_b = din('od_ln_b', [D])
    od_pw2_w = din('od_pw2_w', [D, D])
    od_pw2_b = din('od_pw2_b', [D])
    od_norm_ffn = din('od_norm_ffn', [D])
    od_router = din('od_router', [D, NE])
    od_moe_wg = din('od_moe_wg', [NE, D, FFE])
    od_moe_wu = din('od_moe_wu', [NE, D, FFE])
    od_moe_wd = din('od_moe_wd', [NE, FFE, D])
    final_norm = din('final_norm', [D])
    c_ident = din('c_ident', [128, 128])
    c_cos = din('c_cos', [128, L])
    c_sin = din('c_sin', [128, L])
    c_mask = din('c_mask', [128, 384])
    c_zf = din('c_zf', [33, L + 1])
    c_dec = din('c_dec', [L, 512])
    c_decsh = din('c_decsh', [L, 512])
    c_cq = din('c_cq', [L, L], BF16)
    c_sq = din('c_sq', [L, L], BF16)
    c_cqf = din('c_cqf', [16, 128, 16 * 128], BF16)
    c_sqf = din('c_sqf', [16, 128, 16 * 128], BF16)
    c_cfsf = din('c_cfsf', [128, 32])
    c_sel = din('c_sel', [8, 8 * 128])
    kspec = nc.dram_tensor('kspec', [16, 128, 2 * 2 * 512], BF16, kind="Internal").ap()

    es = ExitStack()
    with es:
        P = Prog(nc, es)
        A = Arena(nc)
        A.prog = P
        NCD = dict(allow_slow_non_contiguous=True)

        PS = []
        for i in range(8):
            t = es.enter_context(nc.psum_tensor('ps%d' % i, [128, 512], F32))
            PS.append((t, Res('ps%d' % i)))

        def ps_bf(t):
            return t[:].bitcast(BF16)

        ident = A.alloc('ident', [128, 128], F32)
        identb = A.alloc('identb', [128, 128], BF16)
        onesb = A.alloc('onesb', [128, 128], BF16)
        onesf = A.alloc('onesf', [128, 128], F32)
        negpi = A.alloc('negpi', [128, 1], F32)
        epsD = A.alloc('epsD', [128, 1], F32)
        epsT = A.alloc('epsT', [128, 1], F32)
        r_const = Res('const')
        P.dma('sp', ident[:], c_ident, writes=[r_const], sem='cid')
        P.op('dve', lambda e: e.tensor_copy(out=identb[:], in_=ident[:]), reads=[r_const], writes=[r_const])
        P.op('dve', lambda e: e.memset(onesb[:], 1.0), writes=[r_const])
        P.op('dve', lambda e: e.memset(onesf[:], 1.0), writes=[r_const])
        P.op('dve', lambda e: e.memset(negpi[:], -math.pi), writes=[r_const])
        P.op('dve', lambda e: e.memset(epsD[:], float(D * EPS)), writes=[r_const])
        P.op('dve', lambda e: e.memset(epsT[:], float(EPS)), writes=[r_const])

        vecs = A.alloc('vecs', [128, 160], F32)
        r_vec = Res('vecs')
        vofs = {}
        vo = [0]

        def load_vec(name, ap, n):
            nch = n // 128
            vofs[name] = vo[0]
            if 'vecs' in SKIP:
                vo[0] += nch
                return
            P.dma('act', vecs[:, vo[0]:vo[0] + nch], ap.rearrange('(c p) -> p c', p=128),
                  writes=[r_vec], sem='cv', **NCD)
            vo[0] += nch

        load_vec('g_mix0', ev_norm_mix, D)
        load_vec('g_ffn0', ev_norm_ffn, D)
        load_vec('g_mix1', od_norm_mix, D)
        load_vec('g_ffn1', od_norm_ffn, D)
        load_vec('g_fin', final_norm, D)
        load_vec('short_b', ev_short_b, 1536)
        load_vec('sw0', ev_short_w[0], 1536)
        load_vec('sw1', ev_short_w[1], 1536)
        load_vec('sw2', ev_short_w[2], 1536)
        load_vec('ds0', ev_dskip[0], 512)
        load_vec('ds1', ev_dskip[1], 512)
        load_vec('pw1_b', od_pw1_b, 2048)
        load_vec('dw_b', od_dw_b, D)
        load_vec('ln_g', od_ln_g, D)
        load_vec('ln_b', od_ln_b, D)
        load_vec('pw2_b', od_pw2_b, D)
        assert vo[0] <= 160

        def V(name, c):
            o = vofs[name] + c
            return vecs[:, o:o + 1]

        sinkb = A.alloc('sinkb', [128, 8], F32)
        nsinkb = A.alloc('nsinkb', [128, 8], F32)
        cfsf = A.alloc('cfsf', [128, 32], F32)
        r_sink = Res('sink')
        r_cfsf = Res('cfsf')
        if 'sink' not in SKIP:
            P.dma('sp', sinkb[:], ev_sink.partition_broadcast(128), writes=[r_sink], sem='csk', **NCD)
        P.dma('sp', cfsf[:], c_cfsf, writes=[r_cfsf], sem='ccf')
        P.op('dve', lambda e: e.tensor_scalar(out=nsinkb[:], in0=sinkb[:], scalar1=-1.0, scalar2=None,
                                              op0=ALU.mult), reads=[r_sink], writes=[r_sink])
        P.op('dve', lambda e: e.tensor_scalar(out=vecs[:, 0:40], in0=vecs[:, 0:40], scalar1=float(math.sqrt(D)),
                                              scalar2=None, op0=ALU.mult), reads=[r_vec], writes=[r_vec])

        persist_mark = A.mark()
        if stop == 'const':
            P.barrier()
            P.emit()
            return nc

        def MM(ps_ap, lhsT, rhs, start, stop, R, W):
            P.op('pe', lambda e: e.matmul(ps_ap, lhsT=lhsT, rhs=rhs, start=start, stop=stop),
                 reads=R, writes=W)

        def TR(ps_ap, in_ap, id_ap, R, W):
            P.op('pe', lambda e: e.transpose(ps_ap, in_ap, id_ap), reads=R, writes=W)

        psrot = Rot(PS)

        def rmsnorm_block(xTb, r_x, gname, hT_ap, r_h, hf_ap=None, tmp=None):
            m = A.mark()
            if tmp is None:
                sq = A.alloc('sq', [128, 8, 512], BF16)
                rs = A.alloc('rs', [128, 512], F32)
                r_sq, r_rs = Res('sq'), Res('rs')
            else:
                sq, rs, r_sq, r_rs = tmp
            P.op('act', lambda e: e.activation(out=sq[:], in_=xTb, func=AF.Square), reads=[r_x], writes=[r_sq])
            pst, r_ps = psrot.next()
            for c in range(8):
                MM(pst[:], onesb[:], sq[:, c, :], c == 0, c == 7, [r_sq, r_const], [r_ps])
            P.op('act', lambda e: e.activation(out=rs[:], in_=pst[:], func=AF.Sqrt, bias=epsD[:], scale=1.0),
                 reads=[r_ps, r_const], writes=[r_rs])
            P.op('dve', lambda e: e.reciprocal(out=rs[:], in_=rs[:]), reads=[r_rs], writes=[r_rs])
            for c in range(8):
                P.op('dve', lambda e, c=c: e.scalar_tensor_tensor(
                    out=hT_ap[:, c, :], in0=xTb[:, c, :], scalar=V(gname, c), in1=rs[:],
                    op0=ALU.mult, op1=ALU.mult), reads=[r_x, r_rs, r_vec], writes=[r_h])
                if hf_ap is not None:
                    P.op('dve', lambda e, c=c: e.scalar_tensor_tensor(
                        out=hf_ap[:, c, :], in0=xTb[:, c, :], scalar=V(gname, c), in1=rs[:],
                        op0=ALU.mult, op1=ALU.mult), reads=[r_x, r_rs, r_vec], writes=[r_h])
            if tmp is None:
                A.release(m)

        def load_x_block(tb, xTb, r_xTb, stage=None):
            m = A.mark()
            if stage is None:
                xtok = A.alloc('xtok', [128, 4, D], F32)
                r_xtok = Res('xtok')
                semn = 'xin'
            else:
                xtok, r_xtok, semn = stage
            P.dma('sp', xtok[:], x_d[tb * 512:(tb + 1) * 512, :].rearrange('(j p) d -> p j d', p=128),
                  writes=[r_xtok], sem=semn)
            for c in range(8):
                pst, r_ps = psrot.next()
                for j in range(4):
                    TR(pst[:, j * 128:(j + 1) * 128], xtok[:, j, c * 128:(c + 1) * 128], ident[:],
                       [r_xtok, r_const], [r_ps])
                eng = 'act' if c % 2 == 0 else 'dve'
                if eng == 'act':
                    P.op('act', lambda e, c=c, pst=pst: e.copy(out=xTb[:, c, :], in_=pst[:]),
                         reads=[r_ps], writes=[r_xTb])
                else:
                    P.op('dve', lambda e, c=c, pst=pst: e.tensor_copy(out=xTb[:, c, :], in_=pst[:]),
                         reads=[r_ps], writes=[r_xTb])
            if stage is None:
                A.release(m)

        def store_block(tb, oT, r_oT, stage=None):
            m = A.mark()
            if stage is None:
                otok = A.alloc('otok', [128, 4, D], F32)
                r_otok = Res('otok')
                semn = 'xout'
            else:
                otok, r_otok, semn = stage
            for j in range(4):
                for half in range(2):
                    pst, r_ps = psrot.next()
                    for cc in range(4):
                        c = half * 4 + cc
                        TR(pst[:, cc * 128:(cc + 1) * 128], oT[:, c, j * 128:(j + 1) * 128], ident[:],
                           [r_oT, r_const], [r_ps])
                    if (j + half) % 2 == 0:
                        P.op('act', lambda e, j=j, half=half, pst=pst: e.copy(
                            out=otok[:, j, half * 512:(half + 1) * 512], in_=pst[:]),
                            reads=[r_ps], writes=[r_otok])
                    else:
                        P.op('dve', lambda e, j=j, half=half, pst=pst: e.tensor_copy(
                            out=otok[:, j, half * 512:(half + 1) * 512], in_=pst[:]),
                            reads=[r_ps], writes=[r_otok])
            t = P.dma('sp', out_d[tb * 512:(tb + 1) * 512, :].rearrange('(j p) d -> p j d', p=128), otok[:],
                      reads=[r_otok], sem=semn)
            if stage is None:
                P.barrier()
                A.release(m)
            return t

        def finish(xT_blocks_fn):
            for tb in range(NT):
                oT, r = xT_blocks_fn(tb)
                store_block(tb, oT, r)
            P.barrier()
            P.emit()

        def phase_filter():
            m = A.mark()
            ktc = A.alloc('ktc', [128, NB, 2, 512], BF16)
            kts = A.alloc('kts', [128, NB, 2, 512], BF16)
            m_mlp = A.mark()
            zf = A.alloc('zf', [33, L + 1], F32)
            w1 = A.alloc('fw1', [33, 64], F32)
            w2 = A.alloc('fw2', [64, 64], F32)
            w3a = A.alloc('fw3a', [65, 2048], F32)
            fv = A.alloc('fv', [64, 4], F32)
            h1 = A.alloc('fh1', [64, L + 1], F32)
            h2a = A.alloc('fh2a', [65, L + 1], F32)
            r_f = Res('filt_in')
            r_h1, r_h2, r_kt = Res('h1'), Res('h2'), Res('kt')
            P.dma('sp', zf[:], c_zf, writes=[r_f], sem='c')
            P.dma('sp', w1[:], ev_filt_w1, writes=[r_f], sem='c')
            P.dma('sp', w2[:], ev_filt_w2, writes=[r_f], sem='c')
            P.dma('sp', w3a[0:64, :], ev_filt_w3, writes=[r_f], sem='c')
            P.dma('sp', w3a[64:65, :], ev_filt_b3.rearrange('(o n) -> o n', o=1), writes=[r_f], sem='c')
            P.dma('sp', fv[:, 0:1], ev_filt_b1.rearrange('(p o) -> p o', o=1), writes=[r_f], sem='c', **NCD)
            P.dma('sp', fv[:, 1:2], ev_filt_b2.rearrange('(p o) -> p o', o=1), writes=[r_f], sem='c', **NCD)
            P.dma('sp', fv[:, 2:4], ev_filt_freq.rearrange('t p -> p t'), writes=[r_f], sem='c', **NCD)
            P.op('dve', lambda e: e.memset(h2a[0:64, :], 0.0), writes=[r_h2])
            P.op('dve', lambda e: e.memset(h2a[64:65, :], 1.0), writes=[r_h2])
            P.op('dve', lambda e: e.memset(h1[:], 0.0), writes=[r_h1])

            fargs = [A.alloc('farg%d' % i, [64, 512], F32) for i in range(2)]
            fkfs = [A.alloc('fkf%d' % i, [64, 512], F32) for i in range(2)]
            r_fargs = [Res('farg0'), Res('farg1')]
            lcnt = [0]

            def layer(wt, K, src, r_src, dst, r_dst, bcol, fcol):
                for tb in range(NT):
                    pst, r_ps = psrot.next()
                    sl = slice(tb * 512, (tb + 1) * 512)
                    MM(pst[0:64, :], wt[0:K, :], src[0:K, sl], True, True, [r_f, r_src], [r_ps])
                    bb = lcnt[0] % 2
                    lcnt[0] += 1
                    arg, kf, r_arg = fargs[bb], fkfs[bb], r_fargs[bb]
                    P.op('dve', lambda e, pst=pst, arg=arg: e.tensor_scalar(
                        out=arg[:], in0=pst[0:64, :], scalar1=fv[:, bcol:bcol + 1], scalar2=fv[:, fcol:fcol + 1],
                        op0=ALU.add, op1=ALU.mult), reads=[r_ps, r_f], writes=[r_arg])
                    P.op('dve', lambda e, arg=arg: e.tensor_scalar(
                        out=arg[:], in0=arg[:], scalar1=float(1.0 / (2 * math.pi)), scalar2=64.0,
                        op0=ALU.mult, op1=ALU.add), reads=[r_arg], writes=[r_arg])
                    P.op('dve', lambda e, arg=arg, kf=kf: e.tensor_scalar(
                        out=kf[:], in0=arg[:], scalar1=8388608.0, scalar2=8388608.0,
                        op0=ALU.add, op1=ALU.subtract), reads=[r_arg], writes=[r_arg])
                    P.op('dve', lambda e, arg=arg, kf=kf: e.tensor_tensor(
                        out=arg[:], in0=arg[:], in1=kf[:], op=ALU.subtract), reads=[r_arg], writes=[r_arg])
                    P.op('act', lambda e, arg=arg, sl=sl: e.activation(
                        out=dst[0:64, sl], in_=arg[:], func=AF.Sin, scale=6.28318),
                        reads=[r_arg, r_const], writes=[r_dst])

            layer(w1, 33, zf, r_f, h1, r_h1, 0, 2)
            layer(w2, 64, h1, r_h1, h2a, r_h2, 1, 3)

            mk_ = A.mark()
            decb = [A.alloc('dec%d' % i, [128, 512], F32) for i in range(2)]
            decshb = [A.alloc('decsh%d' % i, [128, 512], F32) for i in range(2)]
            r_decb = [Res('dec0'), Res('dec1')]
            fdb = [A.alloc('fd%d' % i, [128, 512], F32) for i in range(2)]
            bdb = [A.alloc('bd%d' % i, [128, 512], F32) for i in range(2)]
            r_fdb = [Res('fd0'), Res('fd1')]
            r_bdb = [Res('bd0'), Res('bd1')]
            ki = 0
            for blk in range(NB):
                db_ = blk % 2
                dec, decsh, r_dec = decb[db_], decshb[db_], r_decb[db_]
                P.dma('sp', dec[:], c_dec[blk * 128:(blk + 1) * 128, :], writes=[r_dec], sem='dec%d' % db_)
                P.dma('sp', decsh[:], c_decsh[blk * 128:(blk + 1) * 128, :], writes=[r_dec], sem='dec%d' % db_)
                for n in range(2):
                    psf, r_psf = psrot.next()
                    psb, r_psb = psrot.next()
                    MM(psf[:], h2a[:, blk * 128:(blk + 1) * 128], w3a[:, n * 1024:n * 1024 + 512], True, True,
                       [r_h2, r_f], [r_psf])
                    MM(psb[:], h2a[:, blk * 128 + 1:(blk + 1) * 128 + 1], w3a[:, n * 1024 + 512:n * 1024 + 1024],
                       True, True, [r_h2, r_f], [r_psb])
                    fd, bd, r_fd, r_bd = fdb[ki % 2], bdb[ki % 2], r_fdb[ki % 2], r_bdb[ki % 2]
                    ki += 1
                    P.op('dve', lambda e, psf=psf, fd=fd, dec=dec: e.tensor_tensor(out=fd[:], in0=psf[:], in1=dec[:], op=ALU.mult),
                         reads=[r_psf, r_dec], writes=[r_fd])
                    P.op('dve', lambda e, psb=psb, bd=bd, decsh=decsh: e.tensor_tensor(out=bd[:], in0=psb[:], in1=decsh[:], op=ALU.mult),
                         reads=[r_psb, r_dec], writes=[r_bd])
                    P.op('pool', lambda e, fd=fd, bd=bd, n=n, blk=blk: e.tensor_tensor(
                        out=ktc[:, blk, n, :], in0=fd[:], in1=bd[:], op=ALU.add), reads=[r_fd, r_bd], writes=[r_kt])
                    P.op('dve', lambda e, fd=fd, bd=bd, n=n, blk=blk: e.tensor_tensor(
                        out=kts[:, blk, n, :], in0=bd[:], in1=fd[:], op=ALU.subtract), reads=[r_fd, r_bd], writes=[r_kt])
            A.release(m_mlp)

            cqs = [A.alloc('cqf%d' % i, [128, 16, 128], BF16) for i in range(3)]
            sqs = [A.alloc('sqf%d' % i, [128, 16, 128], BF16) for i in range(3)]
            r_m = [Res('mf0'), Res('mf1'), Res('mf2')]
            kst = [A.alloc('kst%d' % i, [128, 2, 2, 512], BF16) for i in range(3)]
            r_kst = [Res('kst0'), Res('kst1'), Res('kst2')]
            t1s = [A.alloc('kt1_%d' % i, [128, 512], F32) for i in range(4)]
            r_t1s = [Res('kt1_%d' % i) for i in range(4)]
            ti = 0
            def load_f(j):
                b = j % 3
                P.dma('sp', cqs[b][:], c_cqf[j].rearrange('p (i q) -> p i q', q=128), writes=[r_m[b]], sem='mf%d' % b)
                P.dma('sp', sqs[b][:], c_sqf[j].rearrange('p (i q) -> p i q', q=128), writes=[r_m[b]], sem='mf%d' % b)

            load_f(0)
            load_f(1)
            for j in range(16):
                b = j % 3
                if j + 2 < 16:
                    load_f(j + 2)
                for n in range(2):
                    psc, r_psc = psrot.next()
                    pss, r_pss = psrot.next()
                    for i in range(16):
                        MM(psc[:], cqs[b][:, i, :], ktc[:, i, n, :], i == 0, i == 15, [r_m[b], r_kt], [r_psc])
                    for i in range(16):
                        MM(pss[:], sqs[b][:, i, :], kts[:, i, n, :], i == 0, i == 15, [r_m[b], r_kt], [r_pss])
                    cf = cfsf[:, j:j + 1]
                    sf = cfsf[:, 16 + j:17 + j]
                    ta, r_ta = t1s[ti % 4], r_t1s[ti % 4]
                    tb_, r_tb = t1s[(ti + 1) % 4], r_t1s[(ti + 1) % 4]
                    ti += 2
                    P.op('act', lambda e, pss=pss, ta=ta, sf=sf: e.activation(out=ta[:], in_=pss[:], func=AF.Identity, scale=sf),
                         reads=[r_pss, r_cfsf], writes=[r_ta])
                    P.op('act', lambda e, pss=pss, tb_=tb_, cf=cf: e.activation(out=tb_[:], in_=pss[:], func=AF.Identity, scale=cf),
                         reads=[r_pss, r_cfsf], writes=[r_tb])
                    P.op('dve', lambda e, psc=psc, ta=ta, cf=cf, b=b, n=n: e.scalar_tensor_tensor(
                        out=kst[b][:, n, 0, :], in0=psc[:], scalar=cf, in1=ta[:], op0=ALU.mult, op1=ALU.subtract),
                        reads=[r_psc, r_ta, r_cfsf], writes=[r_kst[b]])
                    P.op('dve', lambda e, psc=psc, tb_=tb_, sf=sf, b=b, n=n: e.scalar_tensor_tensor(
                        out=kst[b][:, n, 1, :], in0=psc[:], scalar=sf, in1=tb_[:], op0=ALU.mult, op1=ALU.add),
                        reads=[r_psc, r_tb, r_cfsf], writes=[r_kst[b]])
                P.dma('sp', kspec[j].rearrange('p (a b c) -> p a b c', a=2, b=2), kst[b][:], reads=[r_kst[b]],
                      sem='kst%d' % b)
            P.barrier()
            A.release(m)

        def cast_load(dst_ap, src_ap, r_dst, sem):
            return P.dma('pool', dst_ap, src_ap, writes=[r_dst], sem=sem)

        def phase_layer0_mixer(xT, r_xT):
            m0 = A.mark()
            mixT = A.alloc('mixT', [128, 8, L], BF16)
            r_mix = [[Res('mix%d_%d' % (c, tb)) for tb in range(NT)] for c in range(8)]
            m_h = A.mark()
            hT = A.alloc('hT', [128, 8, L], BF16)
            r_hT = [Res('hT%d' % tb) for tb in range(NT)]
            AXr = Arena(nc, base=xT_off, limit=xT_off + 65536)
            AXr.prog = P
            mm_ = A.mark()
            xtk = [A.alloc('xtok%d' % i, [128, 4, D], F32) for i in range(2)]
            r_xtk = [Res('xtok0'), Res('xtok1')]
            xTbs = [AW.alloc('xTb%d' % i, [128, 8, 512], F32) for i in range(2)]
            r_xTbs = [Res('xTb0'), Res('xTb1')]
            sq1 = [A.alloc('sq%d' % i, [128, 8, 512], BF16) for i in range(2)]
            rs1 = [A.alloc('rs%d' % i, [128, 512], F32) for i in range(2)]
            r_sq1 = [Res('sq0'), Res('sq1')]
            r_rs1 = [Res('rs0'), Res('rs1')]
            for tb in range(NT):
                b = tb % 2
                load_x_block(tb, xTbs[b], r_xTbs[b], stage=(xtk[b], r_xtk[b], 'xin%d' % b))
                rmsnorm_block(xTbs[b][:], r_xTbs[b], 'g_mix0', hT[:, :, tb * 512:(tb + 1) * 512], r_hT[tb],
                              tmp=(sq1[b], rs1[b], r_sq1[b], r_rs1[b]))
            A.release(mm_)

            m2 = A.mark()
            qT = A.alloc('qT', [128, 4, L], BF16)
            kT = A.alloc('kT', [128, L], BF16)
            Vt = A.alloc('Vt', [128, NB, 128], BF16)
            cosT = A.alloc('cosT', [128, L], F32)
            sinT = A.alloc('sinT', [128, L], F32)
            maskt = A.alloc('mask', [128, 384], F32)
            r_tab = Res('tabs')
            r_q, r_k, r_v = Res('qT'), Res('kT'), Res('Vt')
            P.dma('sp', cosT[:], c_cos, writes=[r_tab], sem='c')
            P.dma('sp', sinT[:], c_sin, writes=[r_tab], sem='c')
            P.dma('sp', maskt[:], c_mask, writes=[r_tab], sem='c')
            w_in_v = ev_w_in.rearrange('(kc p) n -> p kc n', p=128)
            rope_t = [A.alloc('rope_t%d' % i, [128, 512], F32) for i in range(4)]
            r_rope = [Res('rope_t%d' % i) for i in range(4)]
            ri = 0
            for ci in range(5):
                for tb in range(NT):
                    sl = slice(tb * 512, (tb + 1) * 512)
                    ps1, r_ps1 = psrot.next()
                    ps2, r_ps2 = psrot.next()
                    for kc in range(8):
                        MM(ps1[:], wq_all[ci][:, kc, :], hT[:, kc, sl], kc == 0, kc == 7, [r_wpre, r_hT[tb]], [r_ps1])
                    for kc in range(8):
                        MM(ps2[:], wr_all[ci][:, kc, :], hT[:, kc, sl], kc == 0, kc == 7, [r_wpre, r_hT[tb]], [r_ps2])
                    t1, r_t1 = rope_t[ri % 4], r_rope[ri % 4]
                    t2, r_t2 = rope_t[(ri + 1) % 4], r_rope[(ri + 1) % 4]
                    ri += 2
                    P.op('dve', lambda e, ps1=ps1, t1=t1, sl=sl: e.tensor_tensor(out=t1[:], in0=ps1[:], in1=cosT[:, sl], op=ALU.mult),
                         reads=[r_ps1, r_tab], writes=[r_t1])
                    P.op('dve', lambda e, ps2=ps2, t2=t2, sl=sl: e.tensor_tensor(out=t2[:], in0=ps2[:], in1=sinT[:, sl], op=ALU.mult),
                         reads=[r_ps2, r_tab], writes=[r_t2])
                    dst = qT[:, ci, sl] if ci < 4 else kT[:, sl]
                    P.op('pool', lambda e, t1=t1, t2=t2, dst=dst: e.tensor_tensor(out=dst, in0=t1[:], in1=t2[:], op=ALU.add),
                         reads=[r_t1, r_t2], writes=[r_q if ci < 4 else r_k])
            wv = wv_pre
            r_wv = r_wpre
            for blk in range(NB):
                pst, r_ps = psrot.next()
                for kc in range(8):
                    MM(pst[:, 0:128], hT[:, kc, blk * 128:(blk + 1) * 128], wv[:, kc, :], kc == 0, kc == 7,
                       [r_hT[blk // 4], r_wv], [r_ps])
                P.op('act', lambda e, pst=pst, blk=blk: e.copy(out=Vt[:, blk, :], in_=pst[:, 0:128]),
                     reads=[r_ps], writes=[r_v])
            P.barrier()

            LEAD = 3
            NR = 6
            sm_b = [A.alloc('sm%d' % i, [128, 384], F32) for i in range(NR)]
            p_b = [A.alloc('pb%d' % i, [128, 384], BF16) for i in range(NR)]
            pt_b = [A.alloc('ptb%d' % i, [128, 384], BF16) for i in range(NR)]
            st_b = [A.alloc('st%d' % i, [128, 8], F32) for i in range(NR)]
            r_sm = [Res('sm%d' % i) for i in range(NR)]
            r_p = [Res('p%d' % i) for i in range(NR)]
            r_pt = [Res('pt%d' % i) for i in range(NR)]
            r_st = [Res('st%d' % i) for i in range(NR)]
            r_st2 = [Res('st2_%d' % i) for i in range(NR)]
            atok = [A.alloc('atok%d' % i, [128, 512], BF16) for i in range(2)]
            r_atok = [[Res('atok%d_%d' % (i, h)) for h in range(8)] for i in range(2)]
            S_ps = Rot(PS[0:3])
            T_ps = Rot(PS[3:5])
            O_ps = [PS[5], PS[6]]
            r_O = [[Res('O%d_%d' % (i, h)) for h in range(8)] for i in range(2)]
            r_Ob = [Res('Obank0'), Res('Obank1')]
            items = [(qb, h) for qb in range(NB) for h in range(8)]

            def geom(qb):
                kbs = [kb for kb in (qb - 1, qb, qb + 1) if 0 <= kb < NB]
                nk = len(kbs)
                return kbs, nk, (kbs[0] - (qb - 1)) * 128, nk * 128, kbs[0] * 128

            def stage_a(i):
                qb, h = items[i]
                kbs, nk, mcol0, W, k0 = geom(qb)
                c, half = h % 4, h // 4
                pr = slice(half * 64, half * 64 + 64)
                bi = i % NR
                sps, r_sps = S_ps.next()
                MM(sps[:, 0:W], qT[pr, c, qb * 128:(qb + 1) * 128], kT[pr, k0:k0 + W], True, True,
                   [r_q, r_k], [r_sps])
                sm, p_, st = sm_b[bi], p_b[bi], st_b[bi]
                P.op('dve', lambda e: e.tensor_tensor(
                    out=sm[:, 0:W], in0=sps[:, 0:W], in1=maskt[:, mcol0:mcol0 + W], op=ALU.add),
                    reads=[r_sps, r_tab], writes=[r_sm[bi]])
                P.op('dve', lambda e: e.tensor_reduce(
                    out=st[:, 0:1], in_=sm[:, 0:W], axis=AX.X, op=ALU.max),
                    reads=[r_sm[bi]], writes=[r_st[bi]])
                P.op('dve', lambda e: e.tensor_scalar(
                    out=st[:, 1:2], in0=st[:, 0:1], scalar1=-0.125, scalar2=nsinkb[:, h:h + 1],
                    op0=ALU.mult, op1=ALU.min), reads=[r_st[bi], r_sink], writes=[r_st[bi]])
                P.op('act', lambda e: e.activation(
                    out=p_[:, 0:W], in_=sm[:, 0:W], func=AF.Exp, bias=st[:, 1:2], scale=0.125,
                    accum_out=st[:, 2:3]), reads=[r_sm[bi], r_st[bi]], writes=[r_p[bi], r_st2[bi]])
                P.op('act', lambda e: e.activation(
                    out=st[:, 3:4], in_=st[:, 1:2], func=AF.Exp, bias=sinkb[:, h:h + 1], scale=1.0),
                    reads=[r_st[bi], r_sink], writes=[r_st2[bi]])

            def stage_b(i):
                qb, h = items[i]
                kbs, nk, mcol0, W, k0 = geom(qb)
                c, half = h % 4, h // 4
                bi = i % NR
                ob = qb % 2
                ops_t, _ = O_ps[ob]
                p_, pt, st = p_b[bi], pt_b[bi], st_b[bi]
                P.op('dve', lambda e: e.tensor_tensor(out=st[:, 4:5], in0=st[:, 2:3], in1=st[:, 3:4], op=ALU.add),
                     reads=[r_st2[bi]], writes=[r_st2[bi]])
                P.op('dve', lambda e: e.reciprocal(out=st[:, 5:6], in_=st[:, 4:5]),
                     reads=[r_st2[bi]], writes=[r_st2[bi]])
                tps, r_tps = T_ps.next()
                tpb = ps_bf(tps)
                for k in range(nk):
                    TR(tpb[:, k * 128:(k + 1) * 128], p_[:, k * 128:(k + 1) * 128], identb[:],
                       [r_p[bi], r_const], [r_tps])
                if h % 2 == 0:
                    P.op('act', lambda e: e.copy(out=pt[:, 0:W], in_=tpb[:, 0:W]),
                         reads=[r_tps], writes=[r_pt[bi]])
                else:
                    P.op('dve', lambda e: e.tensor_copy(out=pt[:, 0:W], in_=tpb[:, 0:W]),
                         reads=[r_tps], writes=[r_pt[bi]])

            def stage_b2(i):
                qb, h = items[i]
                kbs, nk, mcol0, W, k0 = geom(qb)
                c, half = h % 4, h // 4
                bi = i % NR
                ob = qb % 2
                ops_t, _ = O_ps[i % 2]
                p_, pt, st = p_b[bi], pt_b[bi], st_b[bi]
                for k in range(nk):
                    MM(ops_t[:, h * 64:(h + 1) * 64], pt[:, k * 128:(k + 1) * 128],
                       Vt[:, kbs[k], half * 64:half * 64 + 64], k == 0, k == nk - 1,
                       [r_pt[bi], r_v], [r_Ob[i % 2]])
                P.op('act', lambda e: e.activation(
                    out=atok[ob][:, h * 64:(h + 1) * 64], in_=ops_t[:, h * 64:(h + 1) * 64],
                    func=AF.Identity, scale=st[:, 5:6]),
                    reads=[r_Ob[i % 2], r_st2[bi]], writes=[r_atok[ob][h]])
                if h == 7:
                    ps7, r_ps7 = PS[7]
                    p7b = ps_bf(ps7)
                    for cc in range(4):
                        TR(p7b[:, cc * 128:(cc + 1) * 128], atok[ob][:, cc * 128:(cc + 1) * 128], identb[:],
                           [r_atok[ob][2 * cc], r_atok[ob][2 * cc + 1], r_const], [r_ps7])
                    P.op('dve', lambda e: e.tensor_copy(
                        out=mixT[:, 0:4, qb * 128:(qb + 1) * 128],
                        in_=p7b[:, 0:512].rearrange('p (c t) -> p c t', c=4)),
                        reads=[r_ps7], writes=[r_mix[cc_][qb // 4] for cc_ in range(4)])

            n_it = len(items)
            for i in range(min(LEAD, n_it)):
                stage_a(i)
            for i in range(n_it + 1):
                if i + LEAD < n_it:
                    stage_a(i + LEAD)
                if i < n_it:
                    stage_b(i)
                if i >= 1:
                    stage_b2(i - 1)
            P.barrier()
            A.release(m2)
            if stop == 'attn':
                return mixT, r_mix

            g0T = AXr.alloc('g0T', [128, 4, L], BF16)
            g1T = AXr.alloc('g1T', [128, 4, L], BF16)
            zT = AXr.alloc('zT', [128, 4, L], BF16)
            r_g0 = [[Res('g0_%d_%d' % (c, tb)) for tb in range(NT)] for c in range(4)]
            r_g1 = [[Res('g1_%d_%d' % (c, tb)) for tb in range(NT)] for c in range(4)]
            r_z = [[Res('z_%d_%d' % (c, tb)) for tb in range(NT)] for c in range(4)]
            m3 = A.mark()
            wu_ = [A.alloc('wu%d' % i, [128, 8, 128], BF16) for i in range(2)]
            r_wu = [Res('wu0'), Res('wu1')]
            upad = [A.alloc('upad%d' % i, [128, L + 2], F32) for i in range(2)]
            r_up = [Res('upad0'), Res('upad1')]
            t0b = [A.alloc('t0b%d' % i, [128, L], F32) for i in range(2)]
            r_t0 = [Res('t0b0'), Res('t0b1')]
            for i in range(2):
                P.op('dve', lambda e, i=i: e.memset(upad[i][:, 0:1], 0.0), writes=[r_up[i]])
                P.op('dve', lambda e, i=i: e.memset(upad[i][:, L + 1:L + 2], 0.0), writes=[r_up[i]])
            dsts = [(g0T, r_g0), (g1T, r_g1), (zT, r_z)]
            for uc in range(12):
                b = uc % 2
                cast_load(wu_[b][:], w_in_v[:, :, 768 + uc * 128:768 + (uc + 1) * 128], r_wu[b], 'wu%d' % b)
                for tb in range(NT):
                    pst, r_ps = psrot.next()
                    sl = slice(tb * 512, (tb + 1) * 512)
                    for kc in range(8):
                        MM(pst[:], wu_[b][:, kc, :], hT[:, kc, sl], kc == 0, kc == 7, [r_wu[b], r_hT[tb]], [r_ps])
                    P.op('act', lambda e, pst=pst, b=b, tb=tb: e.copy(out=upad[b][:, 1 + tb * 512:1 + (tb + 1) * 512], in_=pst[:]),
                         reads=[r_ps], writes=[r_up[b]])
                dt_, rr = dsts[uc // 4]
                cc = uc % 4
                P.op('act', lambda e, b=b, uc=uc: e.activation(
                    out=t0b[b][:], in_=upad[b][:, 1:L + 1], func=AF.Identity, bias=V('short_b', uc), scale=V('sw1', uc)),
                    reads=[r_up[b], r_vec], writes=[r_t0[b]])
                P.op('dve', lambda e, b=b, uc=uc: e.scalar_tensor_tensor(
                    out=t0b[b][:], in0=upad[b][:, 0:L], scalar=V('sw0', uc), in1=t0b[b][:], op0=ALU.mult, op1=ALU.add),
                    reads=[r_up[b], r_t0[b], r_vec], writes=[r_t0[b]])
                P.op('dve', lambda e, b=b, uc=uc, dt_=dt_, cc=cc: e.scalar_tensor_tensor(
                    out=dt_[:, cc, :], in0=upad[b][:, 2:L + 2], scalar=V('sw2', uc), in1=t0b[b][:], op0=ALU.mult, op1=ALU.add),
                    reads=[r_up[b], r_t0[b], r_vec], writes=rr[cc])
            P.barrier()
            A.release(m_h)

            wo = A.alloc('wo', [128, 8, D], BF16)
            r_wo = Res('wo')
            for kc in range(8):
                cast_load(wo[:, kc, :], ev_w_out[kc * 128:(kc + 1) * 128, :], r_wo, 'wo')
            m_p4 = A.mark()
            ztok = A.alloc('ztok', [128, NB, 512], BF16)
            r_ztok = Res('ztok')
            Yb = A.alloc('Yb', [128, 16, 2, 512], BF16)
            r_Y = Res('Yb')
            for n in range(2):
                for blk in range(NB):
                    pst, r_ps = psrot.next()
                    pb = ps_bf(pst)
                    for cc in range(4):
                        TR(pb[:, cc * 128:(cc + 1) * 128], zT[:, cc, blk * 128:(blk + 1) * 128], identb[:],
                           [r_z[cc][blk // 4], r_const], [r_ps])
                    if blk % 2 == 0:
                        P.op('act', lambda e, pb=pb, blk=blk: e.copy(out=ztok[:, blk, :], in_=pb[:, 0:512]),
                             reads=[r_ps], writes=[r_ztok])
                    else:
                        P.op('dve', lambda e, pb=pb, blk=blk: e.tensor_copy(out=ztok[:, blk, :], in_=pb[:, 0:512]),
                             reads=[r_ps], writes=[r_ztok])
                m4 = A.mark()
                cqs = [A.alloc('cqf%d' % i, [128, 16, 128], BF16) for i in range(3)]
                sqs = [A.alloc('sqf%d' % i, [128, 16, 128], BF16) for i in range(3)]
                ksb = [A.alloc('ksb%d' % i, [128, 2, 512], BF16) for i in range(3)]
                r_m = [Res('mf0'), Res('mf1'), Res('mf2')]
                mt = [A.alloc('mt%d' % i, [128, 512], F32) for i in range(4)]
                r_mt = [Res('mt%d' % i) for i in range(4)]
                for j in range(16):
                    b = j % 3
                    P.dma('sp', cqs[b][:], c_cqf[j].rearrange('p (i q) -> p i q', q=128), writes=[r_m[b]], sem='mf%d' % b)
                    P.dma('sp', sqs[b][:], c_sqf[j].rearrange('p (i q) -> p i q', q=128), writes=[r_m[b]], sem='mf%d' % b)
                    P.dma('sp', ksb[b][:], kspec[j].rearrange('p (a b c) -> p a b c', a=2, b=2)[:, n, :, :],
                          writes=[r_m[b]], sem='mf%d' % b)
                    psa, r_psa = psrot.next()
                    psb, r_psb = psrot.next()
                    for i in range(16):
                        MM(psa[:], cqs[b][:, i, :], ztok[:, i, :], i == 0, i == 15, [r_m[b], r_ztok], [r_psa])
                    for i in range(16):
                        MM(psb[:], sqs[b][:, i, :], ztok[:, i, :], i == 0, i == 15, [r_m[b], r_ztok], [r_psb])
                    kr, ki = ksb[b][:, 0, :], ksb[b][:, 1, :]
                    P.op('dve', lambda e, psa=psa, kr=kr: e.tensor_tensor(out=mt[0][:], in0=psa[:], in1=kr, op=ALU.mult),
                         reads=[r_psa, r_m[b]], writes=[r_mt[0]])
                    P.op('dve', lambda e, psb=psb, ki=ki: e.tensor_tensor(out=mt[1][:], in0=psb[:], in1=ki, op=ALU.mult),
                         reads=[r_psb, r_m[b]], writes=[r_mt[1]])
                    P.op('pool', lambda e, j=j: e.tensor_tensor(out=Yb[:, j, 0, :], in0=mt[0][:], in1=mt[1][:], op=ALU.add),
                         reads=[r_mt[0], r_mt[1]], writes=[r_Y])
                    P.op('dve', lambda e, psb=psb, kr=kr: e.tensor_tensor(out=mt[2][:], in0=psb[:], in1=kr, op=ALU.mult),
                         reads=[r_psb, r_m[b]], writes=[r_mt[2]])
                    P.op('dve', lambda e, psa=psa, ki=ki: e.tensor_tensor(out=mt[3][:], in0=psa[:], in1=ki, op=ALU.mult),
                         reads=[r_psa, r_m[b]], writes=[r_mt[3]])
                    P.op('pool', lambda e, j=j: e.tensor_tensor(out=Yb[:, j, 1, :], in0=mt[2][:], in1=mt[3][:], op=ALU.subtract),
                         reads=[r_mt[2], r_mt[3]], writes=[r_Y])
                P.barrier()
                A.release(m4)
                m5 = A.mark()
                cqh = [A.alloc('cqh%d' % i, [128, 8, 512], BF16) for i in range(2)]
                sqh = [A.alloc('sqh%d' % i, [128, 8, 512], BF16) for i in range(2)]
                r_mh = [Res('mh0'), Res('mh1')]
                yt = [A.alloc('yt%d' % i, [128, 512], F32) for i in range(2)]
                r_yt = [Res('yt0'), Res('yt1')]
                dsn = 'ds%d' % n
                cq_v = c_cq.rearrange('(j p) t -> p j t', p=128)
                sq_v = c_sq.rearrange('(j p) t -> p j t', p=128)

                def load_half(tb, hf_):
                    sl = slice(tb * 512, (tb + 1) * 512)
                    P.dma('sp', cqh[hf_][:], cq_v[:, hf_ * 8:(hf_ + 1) * 8, sl], writes=[r_mh[hf_]], sem='mh%d' % hf_)
                    P.dma('sp', sqh[hf_][:], sq_v[:, hf_ * 8:(hf_ + 1) * 8, sl], writes=[r_mh[hf_]], sem='mh%d' % hf_)

                load_half(0, 0)
                load_half(0, 1)
                for tb in range(NT):
                    sl = slice(tb * 512, (tb + 1) * 512)
                    banks = PS[0:4] if tb % 2 == 0 else PS[4:8]
                    for hf_ in range(2):
                        for cc in range(4):
                            pst, r_ps = banks[cc]
                            for jj in range(8):
                                j = hf_ * 8 + jj
                                MM(pst[:], Yb[:, j, 0, cc * 128:(cc + 1) * 128], cqh[hf_][:, jj, :],
                                   hf_ == 0 and jj == 0, False, [r_Y, r_mh[hf_]], [r_ps])
                            for jj in range(8):
                                j = hf_ * 8 + jj
                                MM(pst[:], Yb[:, j, 1, cc * 128:(cc + 1) * 128], sqh[hf_][:, jj, :],
                                   False, hf_ == 1 and jj == 7, [r_Y, r_mh[hf_]], [r_ps])
                        if tb + 1 < NT:
                            load_half(tb + 1, hf_)
                    for cc in range(4):
                        pst, r_ps = banks[cc]
                        y_, r_y = yt[cc % 2], r_yt[cc % 2]
                        P.op('dve', lambda e, pst=pst, y_=y_, cc=cc, sl=sl, dsn=dsn: e.scalar_tensor_tensor(
                            out=y_[:], in0=zT[:, cc, sl], scalar=V(dsn, cc), in1=pst[:], op0=ALU.mult, op1=ALU.add),
                            reads=[r_ps, r_z[cc][tb], r_vec], writes=[r_y])
                        if n == 0:
                            P.op('pool', lambda e, y_=y_, cc=cc, sl=sl: e.tensor_tensor(
                                out=zT[:, cc, sl], in0=y_[:], in1=g0T[:, cc, sl], op=ALU.mult),
                                reads=[r_y, r_g0[cc][tb]], writes=[r_z[cc][tb]])
                        else:
                            P.op('pool', lambda e, y_=y_, cc=cc, sl=sl: e.tensor_tensor(
                                out=mixT[:, 4 + cc, sl], in0=y_[:], in1=g1T[:, cc, sl], op=ALU.mult),
                                reads=[r_y, r_g1[cc][tb]], writes=[r_mix[4 + cc][tb]])
                P.barrier()
                A.release(m5)
            if stop == 'mix':
                return mixT, r_mix

            A.release(m_p4)
            m6 = A.mark()
            xtk = [A.alloc('xtok%d' % i, [128, 4, D], F32) for i in range(2)]
            r_xtk = [Res('xtok0'), Res('xtok1')]
            xTbs = [A.alloc('xTb%d' % i, [128, 8, 512], F32) for i in range(2)]
            r_xTbs = [Res('xTb0'), Res('xTb1')]
            for tb in range(NT):
                b = tb % 2
                xTb, r_xTb = xTbs[b], r_xTbs[b]
                load_x_block(tb, xTb, r_xTb, stage=(xtk[b], r_xtk[b], 'xin%d' % b))
                sl = slice(tb * 512, (tb + 1) * 512)
                for oc in range(8):
                    pst, r_ps = psrot.next()
                    for kc in range(8):
                        MM(pst[:], wo[:, kc, oc * 128:(oc + 1) * 128], mixT[:, kc, sl], kc == 0, kc == 7,
                           [r_wo, r_mix[kc][tb]], [r_ps])
                    P.op('dve', lambda e, pst=pst, oc=oc, sl=sl, xTb=xTb: e.tensor_tensor(
                        out=xT[:, oc, sl], in0=pst[:], in1=xTb[:, oc, :], op=ALU.add),
                        reads=[r_ps, r_xTb], writes=[r_xT[oc][tb]])
            A.release(m0)
            return None, None

        def norm_all(xT, r_xT, gname, hT, r_hT, hf_cb=None):
            m = A.mark()
            sqs_ = [A.alloc('sq%d' % i, [128, 8, 512], BF16) for i in range(2)]
            rss_ = [A.alloc('rs%d' % i, [128, 512], F32) for i in range(2)]
            r_sqs = [Res('sq0'), Res('sq1')]
            r_rss = [Res('rs0'), Res('rs1')]
            hfs, r_hfs = None, None
            if hf_cb is not None:
                hfs = [A.alloc('hf%d' % i, [128, 8, 512], F32) for i in range(2)]
                r_hfs = [Res('hf0'), Res('hf1')]
            for tb in range(NT):
                sl = slice(tb * 512, (tb + 1) * 512)
                b = tb % 2
                sq, rs, r_sq, r_rs = sqs_[b], rss_[b], r_sqs[b], r_rss[b]
                rx_all = [r_xT[c][tb] for c in range(8)]
                P.op('act', lambda e, sl=sl, sq=sq: e.activation(out=sq[:], in_=xT[:, :, sl], func=AF.Square),
                     reads=rx_all, writes=[r_sq])
                pst, r_ps = psrot.next()
                for c in range(8):
                    MM(pst[:], onesb[:], sq[:, c, :], c == 0, c == 7, [r_sq, r_const], [r_ps])
                P.op('act', lambda e, pst=pst, rs=rs: e.activation(out=rs[:], in_=pst[:], func=AF.Sqrt, bias=epsD[:], scale=1.0),
                     reads=[r_ps, r_const], writes=[r_rs])
                P.op('dve', lambda e, rs=rs: e.reciprocal(out=rs[:], in_=rs[:]), reads=[r_rs], writes=[r_rs])
                for c in range(8):
                    P.op('dve', lambda e, c=c, sl=sl, rs=rs: e.scalar_tensor_tensor(
                        out=hT[:, c, sl], in0=xT[:, c, sl], scalar=V(gname, c), in1=rs[:],
                        op0=ALU.mult, op1=ALU.mult), reads=[r_xT[c][tb], r_rs, r_vec], writes=[r_hT[tb]])
                    if hfs is not None:
                        P.op('dve', lambda e, c=c, sl=sl, rs=rs, hf=hfs[b]: e.scalar_tensor_tensor(
                            out=hf[:, c, :], in0=xT[:, c, sl], scalar=V(gname, c), in1=rs[:],
                            op0=ALU.mult, op1=ALU.mult), reads=[r_xT[c][tb], r_rs, r_vec], writes=[r_hfs[b]])
                if hfs is not None and tb >= 1:
                    hf_cb(tb - 1, hfs[(tb - 1) % 2], r_hfs[(tb - 1) % 2])
            if hfs is not None:
                hf_cb(NT - 1, hfs[(NT - 1) % 2], r_hfs[(NT - 1) % 2])
            A.release(m)

        def swiglu_multi(xT, r_xT, hT, r_hT, experts, G, tag='f', cw_prep=None, before_compute=None):
            m = A.mark()
            NW = 2
            wgb = [A.alloc('wgb%d' % i, [128, 8, G * 128], BF16) for i in range(NW)]
            wub = [A.alloc('wub%d' % i, [128, 8, G * 128], BF16) for i in range(NW)]
            wdb = [A.alloc('wdb%d' % i, [128, G, D], BF16) for i in range(NW)]
            r_w = [Res('w%d' % i) for i in range(NW)]
            r_wd = [Res('wd%d' % i) for i in range(NW)]
            NA = 3
            sg = [A.alloc('sg%d' % i, [128, 512], F32) for i in range(NA)]
            r_sg = [Res('sg%d' % i) for i in range(NA)]
            a1 = [A.alloc('a1_%d' % i, [128, 512], F32) for i in range(NA)]
            r_a1 = [Res('a1_%d' % i) for i in range(NA)]
            NACT = 3
            actb = [A.alloc('actb%d' % i, [128, G, 512], BF16) for i in range(NACT)]
            r_act = [[Res('act%d_%d' % (i, f)) for f in range(G)] for i in range(NACT)]
            GU = Rot(PS[0:4])
            DN = Rot(PS[4:8])
            work = []
            for xi, (wg_d, wu_d, wd_d, nff) in enumerate(experts):
                nch = nff // 128
                assert nch % G == 0
                for g in range(nch // G):
                    work.append((xi, g))
            views = [(wg_d.rearrange('(kc p) f -> p kc f', p=128), wu_d.rearrange('(kc p) f -> p kc f', p=128),
                      wd_d.rearrange('(fc p) d -> p fc d', p=128)) for (wg_d, wu_d, wd_d, nff) in experts]

            def issue_gu(w):
                xi, g = work[w]
                b = w % NW
                wg_v, wu_v, wd_v = views[xi]
                fs = slice(g * G * 128, (g + 1) * G * 128)
                P.dma('pool', wgb[b][:], wg_v[:, :, fs], writes=[r_w[b]], sem='%sw%d' % (tag, b))
                P.dma('pool', wub[b][:], wu_v[:, :, fs], writes=[r_w[b]], sem='%sw%d' % (tag, b))

            def issue_d(w):
                xi, g = work[w]
                b = w % NW
                wg_v, wu_v, wd_v = views[xi]
                P.dma('pool', wdb[b][:], wd_v[:, g * G:(g + 1) * G, :], writes=[r_wd[b]], sem='%sd%d' % (tag, b))

            def issue(w):
                issue_gu(w)
                issue_d(w)

            steps = [(w, tb) for w in range(len(work)) for tb in range(NT)]
            cw_cur = {}
            kcnt = [0]

            def emit_gu(si):
                w, tb = steps[si]
                xi, g = work[w]
                b = w % NW
                if tb == 0:
                    if cw_prep is not None and g == 0:
                        cw_cur[xi] = cw_prep(xi)
                cwb, r_cwb = cw_cur[xi] if cw_prep is not None else (None, None)
                sl = slice(tb * 512, (tb + 1) * 512)
                ab = si % NACT
                for f in range(G):
                    psg, r_psg = GU.next()
                    psu, r_psu = GU.next()
                    for kc in range(8):
                        MM(psg[:], wgb[b][:, kc, f * 128:(f + 1) * 128], hT[:, kc, sl], kc == 0, kc == 7,
                           [r_w[b], r_hT[tb]], [r_psg])
                    for kc in range(8):
                        MM(psu[:], wub[b][:, kc, f * 128:(f + 1) * 128], hT[:, kc, sl], kc == 0, kc == 7,
                           [r_w[b], r_hT[tb]], [r_psu])
                    k = kcnt[0]
                    kcnt[0] += 1
                    s_, r_s = sg[k % NA], r_sg[k % NA]
                    a_, r_a = a1[k % NA], r_a1[k % NA]
                    P.op('act', lambda e, psg=psg, s_=s_: e.activation(out=s_[:], in_=psg[:], func=AF.Silu),
                         reads=[r_psg], writes=[r_s])
                    if cwb is None:
                        P.op('dve', lambda e, psu=psu, s_=s_, ab=ab, f=f: e.tensor_tensor(
                            out=actb[ab][:, f, :], in0=psu[:], in1=s_[:], op=ALU.mult),
                            reads=[r_psu, r_s], writes=[r_act[ab][f]])
                    else:
                        P.op('dve', lambda e, psu=psu, s_=s_, a_=a_: e.tensor_tensor(
                            out=a_[:], in0=psu[:], in1=s_[:], op=ALU.mult),
                            reads=[r_psu, r_s], writes=[r_a])
                        P.op('pool', lambda e, a_=a_, ab=ab, f=f, sl=sl, cwb=cwb: e.tensor_tensor(
                            out=actb[ab][:, f, :], in0=a_[:], in1=cwb[:, sl], op=ALU.mult),
                            reads=[r_a, r_cwb], writes=[r_act[ab][f]])

            def emit_dn(si):
                w, tb = steps[si]
                b = w % NW
                sl = slice(tb * 512, (tb + 1) * 512)
                ab = si % NACT
                for oc in range(8):
                    psd, r_psd = DN.next()
                    for f in range(G):
                        MM(psd[:], wdb[b][:, f, oc * 128:(oc + 1) * 128], actb[ab][:, f, :], f == 0, f == G - 1,
                           [r_wd[b], r_act[ab][f]], [r_psd])
                    P.op('dve', lambda e, psd=psd, oc=oc, sl=sl: e.tensor_tensor(
                        out=xT[:, oc, sl], in0=psd[:], in1=xT[:, oc, sl], op=ALU.add),
                        reads=[r_psd, r_xT[oc][tb]], writes=[r_xT[oc][tb]])

            issue(0)
            if len(work) > 1:
                issue(1)
            if before_compute is not None:
                before_compute()
            emit_gu(0)
            for si in range(len(steps)):
                if si + 1 < len(steps):
                    emit_gu(si + 1)
                    w1_, tb1_ = steps[si + 1]
                    if tb1_ == NT - 1 and w1_ + 2 < len(work):
                        issue_gu(w1_ + 2)
                emit_dn(si)
                w, tb = steps[si]
                if tb == NT - 1 and w + 2 < len(work):
                    issue_d(w + 2)
            P.barrier()
            A.release(m)

        def phase_conformer(xT, r_xT, hT, r_hT):
            m = A.mark()
            gluT = A.alloc('gluT', [128, 8, L + 30], BF16)
            r_glu = [Res('glu%d' % c) for c in range(8)]
            for c in range(8):
                P.op('pool', lambda e, c=c: e.memset(gluT[:, c, 0:15], 0.0), writes=[r_glu[c]])
                P.op('pool', lambda e, c=c: e.memset(gluT[:, c, L + 15:L + 30], 0.0), writes=[r_glu[c]])
            wa = [A.alloc('wa%d' % i, [128, 8, 128], BF16) for i in range(2)]
            wgt = [A.alloc('wgt%d' % i, [128, 8, 128], BF16) for i in range(2)]
            r_w = [Res('pw1_0'), Res('pw1_1')]
            w1v = od_pw1_w.rearrange('(kc p) n -> p kc n', p=128)

            def load_pw1(oc):
                b = oc % 2
                P.dma('pool', wa[b][:], w1v[:, :, oc * 128:(oc + 1) * 128], writes=[r_w[b]], sem='pw1_%d' % b)
                P.dma('pool', wgt[b][:], w1v[:, :, D + oc * 128:D + (oc + 1) * 128], writes=[r_w[b]], sem='pw1_%d' % b)

            load_pw1(0)
            load_pw1(1)
            norm_all(xT, r_xT, 'g_mix1', hT, r_hT)
            dwf = A.alloc('dwf', [128, 31, 8], F32)
            r_dwf = Res('dwf')
            P.dma('sp', dwf[:], od_dw_w.rearrange('j (c p) -> p j c', p=128), writes=[r_dwf], sem='dwf', **NCD)
            w2 = A.alloc('pw2', [128, 8, D], BF16)
            r_w2 = Res('pw2')
            for kc in range(8):
                P.dma('pool', w2[:, kc, :], od_pw2_w[kc * 128:(kc + 1) * 128, :], writes=[r_w2], sem='pw2')
            NDG = 4
            diag = [A.alloc('diag%d' % i, [128, 31, 128], BF16) for i in range(NDG)]
            r_diag = [[Res('diag%d_%d' % (i, j)) for j in range(31)] for i in range(NDG)]
            n_ = [0]

            def build_diag(c, db):
                for j in range(31):
                    n_[0] += 1
                    if n_[0] % 3 != 0:
                        P.op('dve', lambda e, c=c, j=j, db=db: e.tensor_scalar(
                            out=diag[db][:, j, :], in0=identb[:], scalar1=dwf[:, j, c:c + 1], scalar2=None, op0=ALU.mult),
                            reads=[r_dwf, r_const], writes=[r_diag[db][j]])
                    else:
                        P.op('act', lambda e, c=c, j=j, db=db: e.activation(
                            out=diag[db][:, j, :], in_=identb[:], func=AF.Copy, scale=dwf[:, j, c:c + 1]),
                            reads=[r_dwf, r_const], writes=[r_diag[db][j]])
            m1 = A.mark()
            sgb = [A.alloc('sgb%d' % i, [128, 512], F32) for i in range(2)]
            r_sgb = [Res('sgb0'), Res('sgb1')]
            k = 0
            for oc in range(8):
                b = oc % 2
                for tb in range(NT):
                    sl = slice(tb * 512, (tb + 1) * 512)
                    psa, r_psa = psrot.next()
                    psg, r_psg = psrot.next()
                    for kc in range(8):
                        MM(psa[:], wa[b][:, kc, :], hT[:, kc, sl], kc == 0, kc == 7, [r_w[b], r_hT[tb]], [r_psa])
                    for kc in range(8):
                        MM(psg[:], wgt[b][:, kc, :], hT[:, kc, sl], kc == 0, kc == 7, [r_w[b], r_hT[tb]], [r_psg])
                    s_, r_s = sgb[k % 2], r_sgb[k % 2]
                    k += 1
                    P.op('act', lambda e, psg=psg, s_=s_, oc=oc: e.activation(
                        out=s_[:], in_=psg[:], func=AF.Sigmoid, bias=V('pw1_b', 8 + oc), scale=1.0),
                        reads=[r_psg, r_vec], writes=[r_s])
                    P.op('dve', lambda e, psa=psa, s_=s_, oc=oc, tb=tb: e.scalar_tensor_tensor(
                        out=gluT[:, oc, 15 + tb * 512:15 + (tb + 1) * 512], in0=psa[:], scalar=V('pw1_b', oc),
                        in1=s_[:], op0=ALU.add, op1=ALU.mult), reads=[r_psa, r_s, r_vec], writes=[r_glu[oc]])
                if oc + 2 < 8:
                    load_pw1(oc + 2)
                if 4 <= oc < 4 + (NDG - 1):
                    build_diag(oc - 4, oc - 4)
            P.barrier()
            A.release(m1)
            m2 = A.mark()
            AH = Arena(nc, base=hT2_off, limit=hT2_off + 32768)
            dwv = AH.alloc('dwv', [128, 8, 512], F32)
            sqv = AH.alloc('sqv', [128, 8, 512], BF16)
            r_dwv = [Res('dwv%d' % c) for c in range(8)]
            r_sqv = [Res('sqv%d' % c) for c in range(8)]
            mean = A.alloc('mean', [128, 512], F32)
            rstd = A.alloc('rstd', [128, 512], F32)
            var = A.alloc('var', [128, 512], F32)
            r_stat = Res('stat')
            swT = AH.alloc('swT', [128, 8, 512], BF16)
            r_sw = [Res('sw%d' % c) for c in range(8)]
            dtmp = [A.alloc('dtmp%d' % i, [128, 512], F32) for i in range(2)]
            r_dt = [Res('dtmp0'), Res('dtmp1')]
            CV = Rot(PS[0:3])
            citems = [(tb, c) for tb in range(NT) for c in range(8)]

            def ln_chain(tb):
                ps_s, r_pss = PS[3]
                ps_q, r_psq = PS[4]
                for c in range(8):
                    MM(ps_s[:], onesf[:], dwv[:, c, :], c == 0, c == 7, [r_const, r_dwv[c]], [r_pss])
                for c in range(8):
                    MM(ps_q[:], onesb[:], sqv[:, c, :], c == 0, c == 7, [r_const, r_sqv[c]], [r_psq])
                P.op('dve', lambda e: e.tensor_scalar(out=mean[:], in0=ps_s[:], scalar1=1.0 / D, scalar2=None, op0=ALU.mult),
                     reads=[r_pss], writes=[r_stat])
                P.op('dve', lambda e: e.tensor_tensor(out=var[:], in0=mean[:], in1=mean[:], op=ALU.mult),
                     reads=[r_stat], writes=[r_stat])
                P.op('dve', lambda e: e.scalar_tensor_tensor(out=var[:], in0=ps_q[:], scalar=1.0 / D, in1=var[:],
                                                             op0=ALU.mult, op1=ALU.subtract),
                     reads=[r_psq, r_stat], writes=[r_stat])
                P.op('act', lambda e: e.activation(out=rstd[:], in_=var[:], func=AF.Sqrt, bias=epsT[:], scale=1.0),
                     reads=[r_stat, r_const], writes=[r_stat])
                P.op('dve', lambda e: e.reciprocal(out=rstd[:], in_=rstd[:]), reads=[r_stat], writes=[r_stat])
                for c in range(8):
                    d_, r_d = dtmp[c % 2], r_dt[c % 2]
                    P.op('dve', lambda e, c=c, d_=d_: e.tensor_tensor(out=d_[:], in0=dwv[:, c, :], in1=mean[:], op=ALU.subtract),
                         reads=[r_dwv[c], r_stat], writes=[r_d])
                    P.op('pool', lambda e, d_=d_: e.tensor_tensor(out=d_[:], in0=d_[:], in1=rstd[:], op=ALU.mult),
                         reads=[r_d, r_stat], writes=[r_d])
                    P.op('act', lambda e, c=c, d_=d_: e.activation(out=swT[:, c, :], in_=d_[:], func=AF.Silu,
                                                                   bias=V('ln_b', c), scale=V('ln_g', c)),
                         reads=[r_d, r_vec], writes=[r_sw[c]])

            def pw2_mm(tb):
                sl = slice(tb * 512, (tb + 1) * 512)
                for oc in range(8):
                    pst, r_ps = PS[5 + oc % 3]
                    for kc in range(8):
                        MM(pst[:], w2[:, kc, oc * 128:(oc + 1) * 128], swT[:, kc, :], kc == 0, kc == 7,
                           [r_w2, r_sw[kc]], [r_ps])
                    P.op('dve', lambda e, pst=pst, oc=oc, sl=sl: e.scalar_tensor_tensor(
                        out=xT[:, oc, sl], in0=pst[:], scalar=V('pw2_b', oc), in1=xT[:, oc, sl],
                        op0=ALU.add, op1=ALU.add), reads=[r_ps, r_vec, r_xT[oc][tb]], writes=[r_xT[oc][tb]])

            LA = NDG - 1
            pending_pw2 = None
            for i, (tb, c) in enumerate(citems):
                pst, r_ps = CV.next()
                db = i % NDG
                for j in range(31):
                    MM(pst[:], diag[db][:, j, :], gluT[:, c, tb * 512 + j:tb * 512 + j + 512], j == 0, j == 30,
                       [r_diag[db][j], r_glu[c]], [r_ps])
                if i + LA < len(citems):
                    build_diag(citems[i + LA][1], (i + LA) % NDG)
                if pending_pw2 is not None and c == 1:
                    pw2_mm(pending_pw2)
                    pending_pw2 = None
                P.op('act', lambda e, pst=pst, c=c: e.activation(out=dwv[:, c, :], in_=pst[:], func=AF.Identity,
                                                                 bias=V('dw_b', c), scale=1.0),
                     reads=[r_ps, r_vec], writes=[r_dwv[c]])
                P.op('act', lambda e, pst=pst, c=c: e.activation(out=sqv[:, c, :], in_=pst[:], func=AF.Square,
                                                                 bias=V('dw_b', c), scale=1.0),
                     reads=[r_ps, r_vec], writes=[r_sqv[c]])
                if c == 7:
                    ln_chain(tb)
                    pending_pw2 = tb
            pw2_mm(pending_pw2)
            P.barrier()
            A.release(m2)
            A.release(m)

        def phase_moe(xT, r_xT, hT, r_hT):
            m = A.mark()
            rt = A.alloc('router', [128, 8, NE], F32)
            r_rt = Res('router')
            P.dma('sp', rt[:], od_router.rearrange('(kc p) e -> p kc e', p=128), writes=[r_rt], sem='c')
            cw = A.alloc('cw', [128, NB, NE], F32)
            r_cw = Res('cw')
            cwT = A.alloc('cwT', [8, L], F32)
            r_cwT = Res('cwT')
            sel = A.alloc('sel', [8, 8 * 128], F32)
            P.dma('sp', sel[:], c_sel, writes=[r_rt], sem='c')
            lgall = A.alloc('lgall', [128, NB, NE], F32)
            r_lg = [Res('lg%d' % i) for i in range(NB)]
            rsc = A.alloc('rsc', [128, 8, NB], F32)
            r_rsc = Res('rsc')
            eq1 = A.alloc('eq1', [128, NB, NE], F32)
            eq2 = A.alloc('eq2', [128, NB, NE], F32)
            l2 = A.alloc('l2', [128, NB, NE], F32)
            r_eq = Res('eq')

            def route(tb, hf, r_hf):
                for j in range(4):
                    blk = tb * 4 + j
                    pst, r_ps = psrot.next()
                    for kc in range(8):
                        MM(pst[:, 0:NE], hf[:, kc, j * 128:(j + 1) * 128], rt[:, kc, :], kc == 0, kc == 7,
                           [r_hf, r_rt], [r_ps])
                    P.op('act', lambda e, pst=pst, blk=blk: e.copy(out=lgall[:, blk, :], in_=pst[:, 0:NE]),
                         reads=[r_ps], writes=[r_lg[blk]])

            def route_finish():
                M1, M2, DL, EX, DEN, G1, G2 = [rsc[:, i, :] for i in range(7)]
                P.op('dve', lambda e: e.tensor_reduce(out=M1, in_=lgall[:], axis=AX.X, op=ALU.max),
                     reads=r_lg, writes=[r_rsc])
                for blk in range(NB):
                    P.op('dve', lambda e, blk=blk: e.tensor_scalar(out=eq1[:, blk, :], in0=lgall[:, blk, :],
                                                                   scalar1=rsc[:, 0, blk:blk + 1], scalar2=None, op0=ALU.is_equal),
                         reads=[r_rsc, r_lg[blk]], writes=[r_eq])
                P.op('dve', lambda e: e.scalar_tensor_tensor(out=l2[:], in0=eq1[:], scalar=-1e30, in1=lgall[:],
                                                             op0=ALU.mult, op1=ALU.add), reads=[r_eq] + r_lg, writes=[r_eq])
                P.op('dve', lambda e: e.tensor_reduce(out=M2, in_=l2[:], axis=AX.X, op=ALU.max), reads=[r_eq], writes=[r_rsc])
                for blk in range(NB):
                    P.op('dve', lambda e, blk=blk: e.tensor_scalar(out=eq2[:, blk, :], in0=l2[:, blk, :],
                                                                   scalar1=rsc[:, 1, blk:blk + 1], scalar2=None, op0=ALU.is_equal),
                         reads=[r_rsc, r_eq], writes=[r_eq])
                P.op('dve', lambda e: e.tensor_tensor(out=DL, in0=M2, in1=M1, op=ALU.subtract), reads=[r_rsc], writes=[r_rsc])
                P.op('act', lambda e: e.activation(out=EX, in_=DL, func=AF.Exp), reads=[r_rsc], writes=[r_rsc])
                P.op('dve', lambda e: e.tensor_scalar(out=DEN, in0=EX, scalar1=1.0, scalar2=None, op0=ALU.add),
                     reads=[r_rsc], writes=[r_rsc])
                P.op('dve', lambda e: e.reciprocal(out=G1, in_=DEN), reads=[r_rsc], writes=[r_rsc])
                P.op('dve', lambda e: e.tensor_tensor(out=G2, in0=EX, in1=G1, op=ALU.mult), reads=[r_rsc], writes=[r_rsc])
                for blk in range(NB):
                    P.op('dve', lambda e, blk=blk: e.tensor_scalar(out=eq2[:, blk, :], in0=eq2[:, blk, :],
                                                                   scalar1=rsc[:, 6, blk:blk + 1], scalar2=None, op0=ALU.mult),
                         reads=[r_rsc, r_eq], writes=[r_eq])
                    P.op('dve', lambda e, blk=blk: e.scalar_tensor_tensor(
                        out=cw[:, blk, :], in0=eq1[:, blk, :], scalar=rsc[:, 5, blk:blk + 1], in1=eq2[:, blk, :],
                        op0=ALU.mult, op1=ALU.add), reads=[r_rsc, r_eq], writes=[r_cw])
                for q4 in range(4):
                    ps2, r_ps2 = psrot.next()
                    for j in range(4):
                        blk = q4 * 4 + j
                        TR(ps2[0:8, j * 128:(j + 1) * 128], cw[:, blk, :], ident[:], [r_cw, r_const], [r_ps2])
                    P.op('dve', lambda e, ps2=ps2, q4=q4: e.tensor_copy(out=cwT[:, q4 * 512:(q4 + 1) * 512], in_=ps2[0:8, :]),
                         reads=[r_ps2], writes=[r_cwT])

            norm_all(xT, r_xT, 'g_ffn1', hT, r_hT, hf_cb=route)
            route_finish()
            cwb = [A.alloc('cwb%d' % i, [128, L], F32) for i in range(2)]
            r_cwb = [Res('cwb0'), Res('cwb1')]

            def cw_prep(ex):
                b = ex % 2
                for tb in range(NT):
                    pst, r_ps = PS[4 + tb]
                    MM(pst[:], sel[:, ex * 128:(ex + 1) * 128], cwT[:, tb * 512:(tb + 1) * 512], True, True,
                       [r_rt, r_cwT], [r_ps])
                    P.op('act', lambda e, pst=pst, b=b, tb=tb: e.copy(out=cwb[b][:, tb * 512:(tb + 1) * 512], in_=pst[:]),
                         reads=[r_ps], writes=[r_cwb[b]])
                return cwb[b], r_cwb[b]

            swiglu_multi(xT, r_xT, hT, r_hT,
                         [(od_moe_wg[ex], od_moe_wu[ex], od_moe_wd[ex], FFE) for ex in range(NE)], 4,
                         tag='m', cw_prep=cw_prep)
            A.release(m)

        xT = A.alloc('xT', [128, 8, L], F32)
        xT_off = A.last_off
        r_xT = [[Res('xT%d_%d' % (c, tb)) for tb in range(NT)] for c in range(8)]

        AW = Arena(nc, base=xT_off, limit=xT_off + 65536)
        w_in_v0 = ev_w_in.rearrange('(kc p) n -> p kc n', p=128)
        wq_all = [AW.alloc('wqa%d' % i, [128, 8, 128], BF16) for i in range(5)]
        wr_all = [AW.alloc('wra%d' % i, [128, 8, 128], BF16) for i in range(5)]
        wv_pre = AW.alloc('wvp', [128, 8, 128], BF16)
        r_wpre = Res('wpre')
        if stop != 'in':
            for ci in range(5):
                if ci < 4:
                    runs_p = [(0, ci * 64, 64), (64, (ci + 4) * 64, 64)]
                    runs_r = []
                    for hi, h in enumerate((ci, ci + 4)):
                        runs_r.append((hi * 64, h * 64 + 32, 32))
                        runs_r.append((hi * 64 + 32, h * 64, 32))
                else:
                    runs_p = [(0, 512, 128)]
                    runs_r = [(0, 512 + 32, 32), (32, 512, 32), (64, 576 + 32, 32), (96, 576, 32)]
                for (o, s_, n) in runs_p:
                    P.dma('pool', wq_all[ci][:, :, o:o + n], w_in_v0[:, :, s_:s_ + n], writes=[r_wpre], sem='wpre')
                for (o, s_, n) in runs_r:
                    P.dma('pool', wr_all[ci][:, :, o:o + n], w_in_v0[:, :, s_:s_ + n], writes=[r_wpre], sem='wpre')
            P.dma('pool', wv_pre[:], w_in_v0[:, :, 640:768], writes=[r_wpre], sem='wpre')

        phase_filter_needed = stop not in ('attn', 'in')
        if phase_filter_needed:
            phase_filter()

        if stop == 'in':
            for tb in range(NT):
                r = Res('xTb')
                load_x_block(tb, xT[:, :, tb * 512:(tb + 1) * 512], r)
                for c in range(8):
                    r_xT[c][tb] = r
            finish(lambda tb: (xT[:, :, tb * 512:(tb + 1) * 512], r_xT[0][tb]))
            return nc

        mixT, r_mix = phase_layer0_mixer(xT, r_xT)
        if stop in ('attn', 'mix'):
            for tb in range(NT):
                sl = slice(tb * 512, (tb + 1) * 512)
                for c in range(8):
                    P.op('dve', lambda e, c=c, sl=sl: e.tensor_copy(out=xT[:, c, sl], in_=mixT[:, c, sl]),
                         reads=[r_mix[c][tb]], writes=[r_xT[c][tb]])
            P.barrier()

        def xblk(tb):
            r = Res('xall')
            return xT[:, :, tb * 512:(tb + 1) * 512], r

        if stop in ('attn', 'mix', 'l0mix'):
            P.barrier()
            finish(xblk)
            return nc

        hT = A.alloc('hT2', [128, 8, L], BF16)
        hT2_off = A.last_off
        r_hT = [Res('hT2_%d' % tb) for tb in range(NT)]
        swiglu_multi(xT, r_xT, hT, r_hT, [(ev_ffn_wg, ev_ffn_wu, ev_ffn_wd, FF)], 2, tag='f',
                     before_compute=lambda: norm_all(xT, r_xT, 'g_ffn0', hT, r_hT))
        if stop == 'l0':
            P.barrier()
            finish(xblk)
            return nc
        phase_conformer(xT, r_xT, hT, r_hT)
        if stop == 'conf':
            P.barrier()
            finish(xblk)
            return nc
        phase_moe(xT, r_xT, hT, r_hT)
        if stop == 'moe':
            P.barrier()
            finish(xblk)
            return nc
        P.barrier()
        AF2 = Arena(nc, base=hT2_off, limit=hT2_off + 32768)
        oTs = [AF2.alloc('oT%d' % i, [128, 8, 512], F32) for i in range(2)]
        r_oTs = [Res('oT0'), Res('oT1')]
        sqf = [A.alloc('sq%d' % i, [128, 8, 512], BF16) for i in range(2)]
        rsf = [A.alloc('rs%d' % i, [128, 512], F32) for i in range(2)]
        r_sqf = [Res('sq0'), Res('sq1')]
        r_rsf = [Res('rs0'), Res('rs1')]
        otk = [A.alloc('otok%d' % i, [128, 4, D], F32) for i in range(2)]
        r_otk = [Res('otok0'), Res('otok1')]
        for tb in range(NT):
            b = tb % 2
            sl = slice(tb * 512, (tb + 1) * 512)
            sq, rs, oT = sqf[b], rsf[b], oTs[b]
            r_sq, r_rs, r_o = r_sqf[b], r_rsf[b], r_oTs[b]
            P.op('act', lambda e, sl=sl, sq=sq: e.activation(out=sq[:], in_=xT[:, :, sl], func=AF.Square), writes=[r_sq])
            pst, r_ps = psrot.next()
            for c in range(8):
                MM(pst[:], onesb[:], sq[:, c, :], c == 0, c == 7, [r_sq, r_const], [r_ps])
            P.op('act', lambda e, pst=pst, rs=rs: e.activation(out=rs[:], in_=pst[:], func=AF.Sqrt, bias=epsD[:], scale=1.0),
                 reads=[r_ps, r_const], writes=[r_rs])
            P.op('dve', lambda e, rs=rs: e.reciprocal(out=rs[:], in_=rs[:]), reads=[r_rs], writes=[r_rs])
            for c in range(8):
                P.op('dve', lambda e, c=c, sl=sl, oT=oT, rs=rs: e.scalar_tensor_tensor(
                    out=oT[:, c, :], in0=xT[:, c, sl], scalar=V('g_fin', c), in1=rs[:],
                    op0=ALU.mult, op1=ALU.mult), reads=[r_rs, r_vec], writes=[r_o])
            store_block(tb, oT[:], r_o, stage=(otk[b], r_otk[b], 'xout%d' % b))
        P.barrier()
        P.emit()
    return nc


_CONST = {}


def _bf16(a):
    return np.ascontiguousarray(a.astype(ml_dtypes.bfloat16))


def host_constants():
    if _CONST:
        return _CONST
    c = {}
    c['c_ident'] = np.eye(128, dtype=np.float32)
    half = 32
    inv = 10000.0 ** (-np.arange(half, dtype=np.float32) / half)
    pos = np.arange(L, dtype=np.float32)
    ang = pos[None, :] * inv[:, None]
    cos = np.cos(ang).astype(np.float32)
    sin = np.sin(ang).astype(np.float32)
    cos64 = np.concatenate([cos, cos], 0)
    sin64 = np.concatenate([-sin, sin], 0)
    c['c_cos'] = np.ascontiguousarray(np.concatenate([cos64, cos64], 0))
    c['c_sin'] = np.ascontiguousarray(np.concatenate([sin64, sin64], 0))
    r = np.arange(128)[:, None]
    j = np.arange(384)[None, :]
    rel = j - 128 - r
    c['c_mask'] = np.where(np.abs(rel) <= 128, 0.0, -30000.0).astype(np.float32)
    bands = 16
    l = np.arange(L + 1, dtype=np.float64)
    t = (l / (L - 1))
    w = 2.0 * math.pi * l / L
    fb = np.linspace(1e-4, bands - 1, bands)
    zw = fb[None, :] * w[:, None]
    z = np.concatenate([t[:, None], np.cos(zw), -np.sin(zw)], axis=-1)
    c['c_zf'] = np.ascontiguousarray(z.T.astype(np.float32))
    min_decay = math.log(1e-2) / 1.5
    max_decay = math.log(1e-2) / 0.3
    deltas = np.abs(np.linspace(min_decay, max_decay, 512))
    tt = np.linspace(0.0, 1.0, L)
    dec = np.exp(-tt[:, None] * deltas[None, :])
    decsh = np.zeros_like(dec)
    decsh[:L - 1] = dec[1:]
    c['c_dec'] = dec.astype(np.float32)
    c['c_decsh'] = decsh.astype(np.float32)
    N = 2 * L
    a = np.arange(L, dtype=np.float64) + 0.5
    psi = 2.0 * math.pi * np.outer(a, a) / N
    cq = _bf16(np.cos(psi))
    sq = _bf16(np.sin(psi))
    c['c_cq'] = cq
    c['c_sq'] = sq

    def fwd_layout(m):
        return np.ascontiguousarray(m.reshape(16, 128, 16, 128).transpose(2, 1, 0, 3).reshape(16, 128, 2048))
    c['c_cqf'] = fwd_layout(cq)
    c['c_sqf'] = fwd_layout(sq)
    phi = 2.0 * math.pi * (np.arange(L) + 0.5) / N
    cf = (2.0 / N) * np.cos(phi / 2)
    sf = (2.0 / N) * np.sin(phi / 2)
    c['c_cfsf'] = np.ascontiguousarray(np.concatenate([cf.reshape(16, 128).T, sf.reshape(16, 128).T], 1).astype(np.float32))
    sel = np.zeros((8, 8, 128), np.float32)
    for e in range(8):
        sel[e, e, :] = 1.0
    c['c_sel'] = sel.reshape(8, 1024)
    _CONST.update(c)
    return _CONST


_NC_CACHE = {}


def kernel(**inputs):
    stop = inputs.pop('_stop', 'full')
    cores = inputs.pop('_cores', list(range(8)))
    if stop not in _NC_CACHE:
        _NC_CACHE[stop] = build_program(stop)
    nc = _NC_CACHE[stop]
    consts = host_constants()
    shared = {}
    for k, v in inputs.items():
        if k == 'x':
            continue
        a = np.asarray(v, dtype=np.float32)
        if k != 'final_norm':
            a = a.reshape(a.shape[1:])
        shared[k] = np.ascontiguousarray(a)
    shared.update(consts)
    x = np.asarray(inputs['x'], dtype=np.float32)
    in_maps = []
    for b in cores:
        d = dict(shared)
        d['x'] = np.ascontiguousarray(x[b])
        in_maps.append(d)
    res = run_bass_kernel_spmd(nc, in_maps, core_ids=list(range(len(cores))))
    out = np.stack([r['out'] for r in res.results], axis=0)
    return out.astype(np.float32)
```

```python
import math
from contextlib import ExitStack
import numpy as np
import ml_dtypes
import concourse.bass as bass
import concourse.mybir as mybir
from concourse.bass_utils import run_bass_kernel_spmd

F32 = mybir.dt.float32
BF16 = mybir.dt.bfloat16
ALU = mybir.AluOpType
AF = mybir.ActivationFunctionType
AX = mybir.AxisListType

ENGS = ('pe', 'act', 'dve', 'pool', 'sp')
EPOCH = 24000

D = 1024
L = 2048
NB = 16
NT = 4
FF = 2816
FFE = 3584
NE = 8
EPS = 1e-6


class Res:
    __slots__ = ('name', 'w', 'r')

    def __init__(self, name):
        self.name = name
        self.w = None
        self.r = {}


class Tok:
    __slots__ = ('sem', 'val', 'eng')

    def __init__(self, sem, val, eng):
        self.sem = sem
        self.val = val
        self.eng = eng


class Prog:
    def __init__(self, nc, es, same_engine_raw=True):
        self.nc = nc
        self.es = es
        self.ops = {e: [] for e in ENGS}
        self.count = {e: 0 for e in ENGS}
        self.epoch = {e: 0 for e in ENGS}
        self.csem = {}
        for e in ('pe', 'act', 'dve', 'pool'):
            self.csem[e] = [es.enter_context(nc.semaphore('c_%s_0' % e))]
        self.waited = {e: {} for e in ENGS}
        self.dsem = {}
        self.same_engine_raw = same_engine_raw
        self.all_toks = {e: None for e in ENGS}

    def dma_sem(self, name):
        if name not in self.dsem:
            self.dsem[name] = [self.es.enter_context(self.nc.semaphore('d_' + name)), 0]
        return self.dsem[name]

    def _need(self, eng, tok, waits, raw):
        if tok is None:
            return
        if tok.eng == eng:
            if not (raw and self.same_engine_raw and eng in ('act', 'dve', 'pool')):
                return
        k = id(tok.sem)
        if self.waited[eng].get(k, 0) >= tok.val:
            return
        if k in waits:
            if waits[k][1] < tok.val:
                waits[k] = (tok.sem, tok.val)
        else:
            waits[k] = (tok.sem, tok.val)

    def _hazards(self, eng, reads, writes):
        waits = {}
        for r in reads:
            self._need(eng, r.w, waits, True)
        for w in writes:
            self._need(eng, w.w, waits, False)
            for t in w.r.values():
                self._need(eng, t, waits, False)
        for k, (s, v) in waits.items():
            self.waited[eng][k] = v
        return list(waits.values())

    def op(self, eng, fn, reads=(), writes=()):
        waits = self._hazards(eng, reads, writes)
        self.count[eng] += 1
        if self.count[eng] > EPOCH:
            self.epoch[eng] += 1
            self.csem[eng].append(self.es.enter_context(
                self.nc.semaphore('c_%s_%d' % (eng, self.epoch[eng]))))
            self.count[eng] = 1
        sem = self.csem[eng][-1]
        tok = Tok(sem, self.count[eng], eng)
        for r in reads:
            r.r[eng] = tok
        for w in writes:
            w.w = tok
            w.r = {}
        self.ops[eng].append((waits, fn, (sem, 1)))
        self.all_toks[eng] = tok
        return tok

    def dma(self, q, out, in_, reads=(), writes=(), sem='dma', **kw):
        waits = self._hazards(q, reads, writes)
        s = self.dma_sem(sem)
        s[1] += 16
        tok = Tok(s[0], s[1], None)
        key = 'dma_' + sem
        for r in reads:
            r.r[key] = tok
        for w in writes:
            w.w = tok
            w.r = {}

        def fn(e, out=out, in_=in_, kw=kw):
            return e.dma_start(out=out, in_=in_, **kw)
        self.ops[q].append((waits, fn, (s[0], 16)))
        return tok

    def barrier(self):
        toks = [t for t in self.all_toks.values() if t is not None]
        for name, (s, v) in self.dsem.items():
            if v > 0:
                toks.append(Tok(s, v, None))
        for e in ENGS:
            waits = {}
            for t in toks:
                if t.eng == e:
                    continue
                self._need(e, t, waits, True)
            for k, (s, v) in waits.items():
                self.waited[e][k] = v
            if waits:
                self.ops[e].append((list(waits.values()), None, None))

    def emit(self):
        nc = self.nc
        with nc.Block() as block:
            def mk(ename):
                def body(e):
                    for waits, fn, inc in self.ops[ename]:
                        for (s, v) in waits:
                            e.wait_ge(s, v)
                        if fn is not None:
                            ins = fn(e)
                            if inc is not None:
                                ins.then_inc(inc[0], inc[1])
                return body
            block.tensor(mk('pe'))
            block.scalar(mk('act'))
            block.vector(mk('dve'))
            block.gpsimd(mk('pool'))
            block.sync(mk('sp'))


class Arena:
    def __init__(self, nc, base=17408, limit=224 * 1024 - 256):
        self.nc = nc
        self.top = base
        self.limit = limit
        self.n = 0
        self.peak = 0
        self.prog = None

    def alloc(self, name, shape, dtype):
        free = int(np.prod(shape[1:]))
        nbytes = free * (4 if dtype == F32 else 2)
        off = (self.top + 63) // 64 * 64
        self.n += 1
        t = self.nc.alloc_sbuf_tensor_at('%s_%d' % (name, self.n), list(shape), dtype, offset=off)
        self.last_off = off
        self.top = off + nbytes
        self.peak = max(self.peak, self.top)
        assert self.top <= self.limit, ('SBUF overflow', name, self.top)
        return t

    def mark(self):
        return self.top

    def release(self, m):
        if self.prog is not None:
            self.prog.barrier()
        self.top = m


class Rot:
    def __init__(self, items):
        self.items = items
        self.i = 0

    def next(self):
        it = self.items[self.i % len(self.items)]
        self.i += 1
        return it


def build_program(stop='full'):
    import os
    SKIP = set(os.environ.get('KSKIP', '').split(','))
    nc = bass.Bass("TRN2", target_bir_lowering=False)

    def din(name, shape, dt=F32):
        return nc.dram_tensor(name, list(shape), dt, kind="ExternalInput").ap()

    x_d = din('x', [L, D])
    out_d = nc.dram_tensor('out', [L, D], F32, kind="ExternalOutput").ap()
    ev_norm_mix = din('ev_norm_mix', [D])
    ev_w_in = din('ev_w_in', [D, 2304])
    ev_sink = din('ev_sink', [8])
    ev_short_w = din('ev_short_w', [3, 1536])
    ev_short_b = din('ev_short_b', [1536])
    ev_filt_w1 = din('ev_filt_w1', [33, 64])
    ev_filt_b1 = din('ev_filt_b1', [64])
    ev_filt_w2 = din('ev_filt_w2', [64, 64])
    ev_filt_b2 = din('ev_filt_b2', [64])
    ev_filt_w3 = din('ev_filt_w3', [64, 2048])
    ev_filt_b3 = din('ev_filt_b3', [2048])
    ev_filt_freq = din('ev_filt_freq', [2, 64])
    ev_dskip = din('ev_dskip', [2, 512])
    ev_w_out = din('ev_w_out', [D, D])
    ev_norm_ffn = din('ev_norm_ffn', [D])
    ev_ffn_wg = din('ev_ffn_wg', [D, FF])
    ev_ffn_wu = din('ev_ffn_wu', [D, FF])
    ev_ffn_wd = din('ev_ffn_wd', [FF, D])
    od_norm_mix = din('od_norm_mix', [D])
    od_pw1_w = din('od_pw1_w', [D, 2048])
    od_pw1_b = din('od_pw1_b', [2048])
    od_dw_w = din('od_dw_w', [31, D])
    od_dw_b = din('od_dw_b', [D])
    od_ln_g = din('od_ln_g', [D])
    od_ln_b = din('od_ln_b', [D])
    od_pw2_w = din('od_pw2_w', [D, D])
    od_pw2_b = din('od_pw2_b', [D])
    od_norm_ffn = din('od_norm_ffn', [D])
    od_router = din('od_router', [D, NE])
    od_moe_wg = din('od_moe_wg', [NE, D, FFE])
    od_moe_wu = din('od_moe_wu', [NE, D, FFE])
    od_moe_wd = din('od_moe_wd', [NE, FFE, D])
    final_norm = din('final_norm', [D])
    c_ident = din('c_ident', [128, 128])
    c_cos = din('c_cos', [128, L])
    c_sin = din('c_sin', [128, L])
    c_mask = din('c_mask', [128, 384])
    c_zf = din('c_zf', [33, L + 1])
    c_dec = din('c_dec', [L, 512])
    c_decsh = din('c_decsh', [L, 512])
    c_cq = din('c_cq', [L, L], BF16)
    c_sq = din('c_sq', [L, L], BF16)
    c_cqf = din('c_cqf', [16, 128, 16 * 128], BF16)
    c_sqf = din('c_sqf', [16, 128, 16 * 128], BF16)
    c_cfsf = din('c_cfsf', [128, 32])
    c_sel = din('c_sel', [8, 8 * 128])
    kspec = nc.dram_tensor('kspec', [16, 128, 2 * 2 * 512], BF16, kind="Internal").ap()

    es = ExitStack()
    with es:
        P = Prog(nc, es)
        A = Arena(nc)
        A.prog = P
        NCD = dict(allow_slow_non_contiguous=True)

        PS = []
        for i in range(8):
            t = es.enter_context(nc.psum_tensor('ps%d' % i, [128, 512], F32))
            PS.append((t, Res('ps%d' % i)))

        def ps_bf(t):
            return t[:].bitcast(BF16)

        ident = A.alloc('ident', [128, 128], F32)
        identb = A.alloc('identb', [128, 128], BF16)
        onesb = A.alloc('onesb', [128, 128], BF16)
        onesf = A.alloc('onesf', [128, 128], F32)
        negpi = A.alloc('negpi', [128, 1], F32)
        epsD = A.alloc('epsD', [128, 1], F32)
        epsT = A.alloc('epsT', [128, 1], F32)
        r_const = Res('const')
        P.dma('sp', ident[:], c_ident, writes=[r_const], sem='cid')
        P.op('dve', lambda e: e.tensor_copy(out=identb[:], in_=ident[:]), reads=[r_const], writes=[r_const])
        P.op('dve', lambda e: e.memset(onesb[:], 1.0), writes=[r_const])
        P.op('dve', lambda e: e.memset(onesf[:], 1.0), writes=[r_const])
        P.op('dve', lambda e: e.memset(negpi[:], -math.pi), writes=[r_const])
        P.op('dve', lambda e: e.memset(epsD[:], float(D * EPS)), writes=[r_const])
        P.op('dve', lambda e: e.memset(epsT[:], float(EPS)), writes=[r_const])

        vecs = A.alloc('vecs', [128, 160], F32)
        r_vec = Res('vecs')
        vofs = {}
        vo = [0]

        def load_vec(name, ap, n):
            nch = n // 128
            vofs[name] = vo[0]
            if 'vecs' in SKIP:
                vo[0] += nch
                return
            P.dma('act', vecs[:, vo[0]:vo[0] + nch], ap.rearrange('(c p) -> p c', p=128),
                  writes=[r_vec], sem='cv', **NCD)
            vo[0] += nch

        load_vec('g_mix0', ev_norm_mix, D)
        load_vec('g_ffn0', ev_norm_ffn, D)
        load_vec('g_mix1', od_norm_mix, D)
        load_vec('g_ffn1', od_norm_ffn, D)
        load_vec('g_fin', final_norm, D)
        load_vec('short_b', ev_short_b, 1536)
        load_vec('sw0', ev_short_w[0], 1536)
        load_vec('sw1', ev_short_w[1], 1536)
        load_vec('sw2', ev_short_w[2], 1536)
        load_vec('ds0', ev_dskip[0], 512)
        load_vec('ds1', ev_dskip[1], 512)
        load_vec('pw1_b', od_pw1_b, 2048)
        load_vec('dw_b', od_dw_b, D)
        load_vec('ln_g', od_ln_g, D)
        load_vec('ln_b', od_ln_b, D)
        load_vec('pw2_b', od_pw2_b, D)
        assert vo[0] <= 160

        def V(name, c):
            o = vofs[name] + c
            return vecs[:, o:o + 1]

        sinkb = A.alloc('sinkb', [128, 8], F32)
        nsinkb = A.alloc('nsinkb', [128, 8], F32)
        cfsf = A.alloc('cfsf', [128, 32], F32)
        r_sink = Res('sink')
        r_cfsf = Res('cfsf')
        if 'sink' not in SKIP:
            P.dma('sp', sinkb[:], ev_sink.partition_broadcast(128), writes=[r_sink], sem='csk', **NCD)
        P.dma('sp', cfsf[:], c_cfsf, writes=[r_cfsf], sem='ccf')
        P.op('dve', lambda e: e.tensor_scalar(out=nsinkb[:], in0=sinkb[:], scalar1=-1.0, scalar2=None,
                                              op0=ALU.mult), reads=[r_sink], writes=[r_sink])
        P.op('dve', lambda e: e.tensor_scalar(out=vecs[:, 0:40], in0=vecs[:, 0:40], scalar1=float(math.sqrt(D)),
                                              scalar2=None, op0=ALU.mult), reads=[r_vec], writes=[r_vec])

        persist_mark = A.mark()
        if stop == 'const':
            P.barrier()
            P.emit()
            return nc

        def MM(ps_ap, lhsT, rhs, start, stop, R, W):
            P.op('pe', lambda e: e.matmul(ps_ap, lhsT=lhsT, rhs=rhs, start=start, stop=stop),
                 reads=R, writes=W)

        def TR(ps_ap, in_ap, id_ap, R, W):
            P.op('pe', lambda e: e.transpose(ps_ap, in_ap, id_ap), reads=R, writes=W)

        psrot = Rot(PS)

        def rmsnorm_block(xTb, r_x, gname, hT_ap, r_h, hf_ap=None, tmp=None):
            m = A.mark()
            if tmp is None:
                sq = A.alloc('sq', [128, 8, 512], BF16)
                rs = A.alloc('rs', [128, 512], F32)
                r_sq, r_rs = Res('sq'), Res('rs')
            else:
                sq, rs, r_sq, r_rs = tmp
            P.op('act', lambda e: e.activation(out=sq[:], in_=xTb, func=AF.Square), reads=[r_x], writes=[r_sq])
            pst, r_ps = psrot.next()
            for c in range(8):
                MM(pst[:], onesb[:], sq[:, c, :], c == 0, c == 7, [r_sq, r_const], [r_ps])
            P.op('act', lambda e: e.activation(out=rs[:], in_=pst[:], func=AF.Sqrt, bias=epsD[:], scale=1.0),
                 reads=[r_ps, r_const], writes=[r_rs])
            P.op('dve', lambda e: e.reciprocal(out=rs[:], in_=rs[:]), reads=[r_rs], writes=[r_rs])
            for c in range(8):
                P.op('dve', lambda e, c=c: e.scalar_tensor_tensor(
                    out=hT_ap[:, c, :], in0=xTb[:, c, :], scalar=V(gname, c), in1=rs[:],
                    op0=ALU.mult, op1=ALU.mult), reads=[r_x, r_rs, r_vec], writes=[r_h])
                if hf_ap is not None:
                    P.op('dve', lambda e, c=c: e.scalar_tensor_tensor(
                        out=hf_ap[:, c, :], in0=xTb[:, c, :], scalar=V(gname, c), in1=rs[:],
                        op0=ALU.mult, op1=ALU.mult), reads=[r_x, r_rs, r_vec], writes=[r_h])
            if tmp is None:
                A.release(m)

        def load_x_block(tb, xTb, r_xTb, stage=None):
            m = A.mark()
            if stage is None:
                xtok = A.alloc('xtok', [128, 4, D], F32)
                r_xtok = Res('xtok')
                semn = 'xin'
            else:
                xtok, r_xtok, semn = stage
            P.dma('sp', xtok[:], x_d[tb * 512:(tb + 1) * 512, :].rearrange('(j p) d -> p j d', p=128),
                  writes=[r_xtok], sem=semn)
            for c in range(8):
                pst, r_ps = psrot.next()
                for j in range(4):
                    TR(pst[:, j * 128:(j + 1) * 128], xtok[:, j, c * 128:(c + 1) * 128], ident[:],
                       [r_xtok, r_const], [r_ps])
                eng = 'act' if c % 2 == 0 else 'dve'
                if eng == 'act':
                    P.op('act', lambda e, c=c, pst=pst: e.copy(out=xTb[:, c, :], in_=pst[:]),
                         reads=[r_ps], writes=[r_xTb])
                else:
                    P.op('dve', lambda e, c=c, pst=pst: e.tensor_copy(out=xTb[:, c, :], in_=pst[:]),
                         reads=[r_ps], writes=[r_xTb])
            if stage is None:
                A.release(m)

        def store_block(tb, oT, r_oT, stage=None):
            m = A.mark()
            if stage is None:
                otok = A.alloc('otok', [128, 4, D], F32)
                r_otok = Res('otok')
                semn = 'xout'
            else:
                otok, r_otok, semn = stage
            for j in range(4):
                for half in range(2):
                    pst, r_ps = psrot.next()
                    for cc in range(4):
                        c = half * 4 + cc
                        TR(pst[:, cc * 128:(cc + 1) * 128], oT[:, c, j * 128:(j + 1) * 128], ident[:],
                           [r_oT, r_const], [r_ps])
                    if (j + half) % 2 == 0:
                        P.op('act', lambda e, j=j, half=half, pst=pst: e.copy(
                            out=otok[:, j, half * 512:(half + 1) * 512], in_=pst[:]),
                            reads=[r_ps], writes=[r_otok])
                    else:
                        P.op('dve', lambda e, j=j, half=half, pst=pst: e.tensor_copy(
                            out=otok[:, j, half * 512:(half + 1) * 512], in_=pst[:]),
                            reads=[r_ps], writes=[r_otok])
            t = P.dma('sp', out_d[tb * 512:(tb + 1) * 512, :].rearrange('(j p) d -> p j d', p=128), otok[:],
                      reads=[r_otok], sem=semn)
            if stage is None:
                P.barrier()
                A.release(m)
            return t

        def finish(xT_blocks_fn):
            for tb in range(NT):
                oT, r = xT_blocks_fn(tb)
                store_block(tb, oT, r)
            P.barrier()
            P.emit()

        def phase_filter():
            m = A.mark()
            ktc = A.alloc('ktc', [128, NB, 2, 512], BF16)
            kts = A.alloc('kts', [128, NB, 2, 512], BF16)
            m_mlp = A.mark()
            zf = A.alloc('zf', [33, L + 1], F32)
            w1 = A.alloc('fw1', [33, 64], F32)
            w2 = A.alloc('fw2', [64, 64], F32)
            w3a = A.alloc('fw3a', [65, 2048], F32)
            fv = A.alloc('fv', [64, 4], F32)
            h1 = A.alloc('fh1', [64, L + 1], F32)
            h2a = A.alloc('fh2a', [65, L + 1], F32)
            r_f = Res('filt_in')
            r_h1, r_h2, r_kt = Res('h1'), Res('h2'), Res('kt')
            P.dma('sp', zf[:], c_zf, writes=[r_f], sem='c')
            P.dma('sp', w1[:], ev_filt_w1, writes=[r_f], sem='c')
            P.dma('sp', w2[:], ev_filt_w2, writes=[r_f], sem='c')
            P.dma('sp', w3a[0:64, :], ev_filt_w3, writes=[r_f], sem='c')
            P.dma('sp', w3a[64:65, :], ev_filt_b3.rearrange('(o n) -> o n', o=1), writes=[r_f], sem='c')
            P.dma('sp', fv[:, 0:1], ev_filt_b1.rearrange('(p o) -> p o', o=1), writes=[r_f], sem='c', **NCD)
            P.dma('sp', fv[:, 1:2], ev_filt_b2.rearrange('(p o) -> p o', o=1), writes=[r_f], sem='c', **NCD)
            P.dma('sp', fv[:, 2:4], ev_filt_freq.rearrange('t p -> p t'), writes=[r_f], sem='c', **NCD)
            P.op('dve', lambda e: e.memset(h2a[0:64, :], 0.0), writes=[r_h2])
            P.op('dve', lambda e: e.memset(h2a[64:65, :], 1.0), writes=[r_h2])
            P.op('dve', lambda e: e.memset(h1[:], 0.0), writes=[r_h1])

            fargs = [A.alloc('farg%d' % i, [64, 512], F32) for i in range(2)]
            fkfs = [A.alloc('fkf%d' % i, [64, 512], F32) for i in range(2)]
            r_fargs = [Res('farg0'), Res('farg1')]
            lcnt = [0]

            def layer(wt, K, src, r_src, dst, r_dst, bcol, fcol):
                for tb in range(NT):
                    pst, r_ps = psrot.next()
                    sl = slice(tb * 512, (tb + 1) * 512)
                    MM(pst[0:64, :], wt[0:K, :], src[0:K, sl], True, True, [r_f, r_src], [r_ps])
                    bb = lcnt[0] % 2
                    lcnt[0] += 1
                    arg, kf, r_arg = fargs[bb], fkfs[bb], r_fargs[bb]
                    P.op('dve', lambda e, pst=pst, arg=arg: e.tensor_scalar(
                        out=arg[:], in0=pst[0:64, :], scalar1=fv[:, bcol:bcol + 1], scalar2=fv[:, fcol:fcol + 1],
                        op0=ALU.add, op1=ALU.mult), reads=[r_ps, r_f], writes=[r_arg])
                    P.op('dve', lambda e, arg=arg: e.tensor_scalar(
                        out=arg[:], in0=arg[:], scalar1=float(1.0 / (2 * math.pi)), scalar2=64.0,
                        op0=ALU.mult, op1=ALU.add), reads=[r_arg], writes=[r_arg])
                    P.op('dve', lambda e, arg=arg, kf=kf: e.tensor_scalar(
                        out=kf[:], in0=arg[:], scalar1=8388608.0, scalar2=8388608.0,
                        op0=ALU.add, op1=ALU.subtract), reads=[r_arg], writes=[r_arg])
                    P.op('dve', lambda e, arg=arg, kf=kf: e.tensor_tensor(
                        out=arg[:], in0=arg[:], in1=kf[:], op=ALU.subtract), reads=[r_arg], writes=[r_arg])
                    P.op('act', lambda e, arg=arg, sl=sl: e.activation(
                        out=dst[0:64, sl], in_=arg[:], func=AF.Sin, scale=6.28318),
                        reads=[r_arg, r_const], writes=[r_dst])

            layer(w1, 33, zf, r_f, h1, r_h1, 0, 2)
            layer(w2, 64, h1, r_h1, h2a, r_h2, 1, 3)

            mk_ = A.mark()
            decb = [A.alloc('dec%d' % i, [128, 512], F32) for i in range(2)]
            decshb = [A.alloc('decsh%d' % i, [128, 512], F32) for i in range(2)]
            r_decb = [Res('dec0'), Res('dec1')]
            fdb = [A.alloc('fd%d' % i, [128, 512], F32) for i in range(2)]
            bdb = [A.alloc('bd%d' % i, [128, 512], F32) for i in range(2)]
            r_fdb = [Res('fd0'), Res('fd1')]
            r_bdb = [Res('bd0'), Res('bd1')]
            ki = 0
            for blk in range(NB):
                db_ = blk % 2
                dec, decsh, r_dec = decb[db_], decshb[db_], r_decb[db_]
                P.dma('sp', dec[:], c_dec[blk * 128:(blk + 1) * 128, :], writes=[r_dec], sem='dec%d' % db_)
                P.dma('sp', decsh[:], c_decsh[blk * 128:(blk + 1) * 128, :], writes=[r_dec], sem='dec%d' % db_)
                for n in range(2):
                    psf, r_psf = psrot.next()
                    psb, r_psb = psrot.next()
                    MM(psf[:], h2a[:, blk * 128:(blk + 1) * 128], w3a[:, n * 1024:n * 1024 + 512], True, True,
                       [r_h2, r_f], [r_psf])
                    MM(psb[:], h2a[:, blk * 128 + 1:(blk + 1) * 128 + 1], w3a[:, n * 1024 + 512:n * 1024 + 1024],
                       True, True, [r_h2, r_f], [r_psb])
                    fd, bd, r_fd, r_bd = fdb[ki % 2], bdb[ki % 2], r_fdb[ki % 2], r_bdb[ki % 2]
                    ki += 1
                    P.op('dve', lambda e, psf=psf, fd=fd, dec=dec: e.tensor_tensor(out=fd[:], in0=psf[:], in1=dec[:], op=ALU.mult),
                         reads=[r_psf, r_dec], writes=[r_fd])
                    P.op('dve', lambda e, psb=psb, bd=bd, decsh=decsh: e.tensor_tensor(out=bd[:], in0=psb[:], in1=decsh[:], op=ALU.mult),
                         reads=[r_psb, r_dec], writes=[r_bd])
                    P.op('pool', lambda e, fd=fd, bd=bd, n=n, blk=blk: e.tensor_tensor(
                        out=ktc[:, blk, n, :], in0=fd[:], in1=bd[:], op=ALU.add), reads=[r_fd, r_bd], writes=[r_kt])
                    P.op('dve', lambda e, fd=fd, bd=bd, n=n, blk=blk: e.tensor_tensor(
                        out=kts[:, blk, n, :], in0=bd[:], in1=fd[:], op=ALU.subtract), reads=[r_fd, r_bd], writes=[r_kt])
            A.release(m_mlp)

            cqs = [A.alloc('cqf%d' % i, [128, 16, 128], BF16) for i in range(3)]
            sqs = [A.alloc('sqf%d' % i, [128, 16, 128], BF16) for i in range(3)]
            r_m = [Res('mf0'), Res('mf1'), Res('mf2')]
            kst = [A.alloc('kst%d' % i, [128, 2, 2, 512], BF16) for i in range(3)]
            r_kst = [Res('kst0'), Res('kst1'), Res('kst2')]
            t1s = [A.alloc('kt1_%d' % i, [128, 512], F32) for i in range(4)]
            r_t1s = [Res('kt1_%d' % i) for i in range(4)]
            ti = 0
            def load_f(j):
                b = j % 3
                P.dma('sp', cqs[b][:], c_cqf[j].rearrange('p (i q) -> p i q', q=128), writes=[r_m[b]], sem='mf%d' % b)
                P.dma('sp', sqs[b][:], c_sqf[j].rearrange('p (i q) -> p i q', q=128), writes=[r_m[b]], sem='mf%d' % b)

            load_f(0)
            load_f(1)
            for j in range(16):
                b = j % 3
                if j + 2 < 16:
                    load_f(j + 2)
                for n in range(2):
                    psc, r_psc = psrot.next()
                    pss, r_pss = psrot.next()
                    for i in range(16):
                        MM(psc[:], cqs[b][:, i, :], ktc[:, i, n, :], i == 0, i == 15, [r_m[b], r_kt], [r_psc])
                    for i in range(16):
                        MM(pss[:], sqs[b][:, i, :], kts[:, i, n, :], i == 0, i == 15, [r_m[b], r_kt], [r_pss])
                    cf = cfsf[:, j:j + 1]
                    sf = cfsf[:, 16 + j:17 + j]
                    ta, r_ta = t1s[ti % 4], r_t1s[ti % 4]
                    tb_, r_tb = t1s[(ti + 1) % 4], r_t1s[(ti + 1) % 4]
                    ti += 2
                    P.op('act', lambda e, pss=pss, ta=ta, sf=sf: e.activation(out=ta[:], in_=pss[:], func=AF.Identity, scale=sf),
                         reads=[r_pss, r_cfsf], writes=[r_ta])
                    P.op('act', lambda e, pss=pss, tb_=tb_, cf=cf: e.activation(out=tb_[:], in_=pss[:], func=AF.Identity, scale=cf),
                         reads=[r_pss, r_cfsf], writes=[r_tb])
                    P.op('dve', lambda e, psc=psc, ta=ta, cf=cf, b=b, n=n: e.scalar_tensor_tensor(
                        out=kst[b][:, n, 0, :], in0=psc[:], scalar=cf, in1=ta[:], op0=ALU.mult, op1=ALU.subtract),
                        reads=[r_psc, r_ta, r_cfsf], writes=[r_kst[b]])
                    P.op('dve', lambda e, psc=psc, tb_=tb_, sf=sf, b=b, n=n: e.scalar_tensor_tensor(
                        out=kst[b][:, n, 1, :], in0=psc[:], scalar=sf, in1=tb_[:], op0=ALU.mult, op1=ALU.add),
                        reads=[r_psc, r_tb, r_cfsf], writes=[r_kst[b]])
                P.dma('sp', kspec[j].rearrange('p (a b c) -> p a b c', a=2, b=2), kst[b][:], reads=[r_kst[b]],
                      sem='kst%d' % b)
            P.barrier()
            A.release(m)

        def cast_load(dst_ap, src_ap, r_dst, sem):
            return P.dma('pool', dst_ap, src_ap, writes=[r_dst], sem=sem)

        def phase_layer0_mixer(xT, r_xT):
            m0 = A.mark()
            mixT = A.alloc('mixT', [128, 8, L], BF16)
            r_mix = [[Res('mix%d_%d' % (c, tb)) for tb in range(NT)] for c in range(8)]
            m_h = A.mark()
            hT = A.alloc('hT', [128, 8, L], BF16)
            r_hT = [Res('hT%d' % tb) for tb in range(NT)]
            AXr = Arena(nc, base=xT_off, limit=xT_off + 65536)
            AXr.prog = P
            mm_ = A.mark()
            xtk = [A.alloc('xtok%d' % i, [128, 4, D], F32) for i in range(2)]
            r_xtk = [Res('xtok0'), Res('xtok1')]
            xTbs = [AW.alloc('xTb%d' % i, [128, 8, 512], F32) for i in range(2)]
            r_xTbs = [Res('xTb0'), Res('xTb1')]
            sq1 = [A.alloc('sq%d' % i, [128, 8, 512], BF16) for i in range(2)]
            rs1 = [A.alloc('rs%d' % i, [128, 512], F32) for i in range(2)]
            r_sq1 = [Res('sq0'), Res('sq1')]
            r_rs1 = [Res('rs0'), Res('rs1')]
            for tb in range(NT):
                b = tb % 2
                load_x_block(tb, xTbs[b], r_xTbs[b], stage=(xtk[b], r_xtk[b], 'xin%d' % b))
                rmsnorm_block(xTbs[b][:], r_xTbs[b], 'g_mix0', hT[:, :, tb * 512:(tb + 1) * 512], r_hT[tb],
                              tmp=(sq1[b], rs1[b], r_sq1[b], r_rs1[b]))
            A.release(mm_)

            m2 = A.mark()
            qT = A.alloc('qT', [128, 4, L], BF16)
            kT = A.alloc('kT', [128, L], BF16)
            Vt = A.alloc('Vt', [128, NB, 128], BF16)
            cosT = A.alloc('cosT', [128, L], F32)
            sinT = A.alloc('sinT', [128, L], F32)
            maskt = A.alloc('mask', [128, 384], F32)
            r_tab = Res('tabs')
            r_q, r_k, r_v = Res('qT'), Res('kT'), Res('Vt')
            P.dma('sp', cosT[:], c_cos, writes=[r_tab], sem='c')
            P.dma('sp', sinT[:], c_sin, writes=[r_tab], sem='c')
            P.dma('sp', maskt[:], c_mask, writes=[r_tab], sem='c')
            w_in_v = ev_w_in.rearrange('(kc p) n -> p kc n', p=128)
            rope_t = [A.alloc('rope_t%d' % i, [128, 512], F32) for i in range(4)]
            r_rope = [Res('rope_t%d' % i) for i in range(4)]
            ri = 0
            for ci in range(5):
                for tb in range(NT):
                    sl = slice(tb * 512, (tb + 1) * 512)
                    ps1, r_ps1 = psrot.next()
                    ps2, r_ps2 = psrot.next()
                    for kc in range(8):
                        MM(ps1[:], wq_all[ci][:, kc, :], hT[:, kc, sl], kc == 0, kc == 7, [r_wpre, r_hT[tb]], [r_ps1])
                    for kc in range(8):
                        MM(ps2[:], wr_all[ci][:, kc, :], hT[:, kc, sl], kc == 0, kc == 7, [r_wpre, r_hT[tb]], [r_ps2])
                    t1, r_t1 = rope_t[ri % 4], r_rope[ri % 4]
                    t2, r_t2 = rope_t[(ri + 1) % 4], r_rope[(ri + 1) % 4]
                    ri += 2
                    P.op('dve', lambda e, ps1=ps1, t1=t1, sl=sl: e.tensor_tensor(out=t1[:], in0=ps1[:], in1=cosT[:, sl], op=ALU.mult),
                         reads=[r_ps1, r_tab], writes=[r_t1])
                    P.op('dve', lambda e, ps2=ps2, t2=t2, sl=sl: e.tensor_tensor(out=t2[:], in0=ps2[:], in1=sinT[:, sl], op=ALU.mult),
                         reads=[r_ps2, r_tab], writes=[r_t2])
                    dst = qT[:, ci, sl] if ci < 4 else kT[:, sl]
                    P.op('pool', lambda e, t1=t1, t2=t2, dst=dst: e.tensor_tensor(out=dst, in0=t1[:], in1=t2[:], op=ALU.add),
                         reads=[r_t1, r_t2], writes=[r_q if ci < 4 else r_k])
            wv = wv_pre
            r_wv = r_wpre
            for blk in range(NB):
                pst, r_ps = psrot.next()
                for kc in range(8):
                    MM(pst[:, 0:128], hT[:, kc, blk * 128:(blk + 1) * 128], wv[:, kc, :], kc == 0, kc == 7,
                       [r_hT[blk // 4], r_wv], [r_ps])
                P.op('act', lambda e, pst=pst, blk=blk: e.copy(out=Vt[:, blk, :], in_=pst[:, 0:128]),
                     reads=[r_ps], writes=[r_v])
            P.barrier()

            LEAD = 3
            NR = 6
            sm_b = [A.alloc('sm%d' % i, [128, 384], F32) for i in range(NR)]
            p_b = [A.alloc('pb%d' % i, [128, 384], BF16) for i in range(NR)]
            pt_b = [A.alloc('ptb%d' % i, [128, 384], BF16) for i in range(NR)]
            st_b = [A.alloc('st%d' % i, [128, 8], F32) for i in range(NR)]
            r_sm = [Res('sm%d' % i) for i in range(NR)]
            r_p = [Res('p%d' % i) for i in range(NR)]
            r_pt = [Res('pt%d' % i) for i in range(NR)]
            r_st = [Res('st%d' % i) for i in range(NR)]
            r_st2 = [Res('st2_%d' % i) for i in range(NR)]
            atok = [A.alloc('atok%d' % i, [128, 512], BF16) for i in range(2)]
            r_atok = [[Res('atok%d_%d' % (i, h)) for h in range(8)] for i in range(2)]
            S_ps = Rot(PS[0:3])
            T_ps = Rot(PS[3:5])
            O_ps = [PS[5], PS[6]]
            r_O = [[Res('O%d_%d' % (i, h)) for h in range(8)] for i in range(2)]
            r_Ob = [Res('Obank0'), Res('Obank1')]
            items = [(qb, h) for qb in range(NB) for h in range(8)]

            def geom(qb):
                kbs = [kb for kb in (qb - 1, qb, qb + 1) if 0 <= kb < NB]
                nk = len(kbs)
                return kbs, nk, (kbs[0] - (qb - 1)) * 128, nk * 128, kbs[0] * 128

            def stage_a(i):
                qb, h = items[i]
                kbs, nk, mcol0, W, k0 = geom(qb)
                c, half = h % 4, h // 4
                pr = slice(half * 64, half * 64 + 64)
                bi = i % NR
                sps, r_sps = S_ps.next()
                MM(sps[:, 0:W], qT[pr, c, qb * 128:(qb + 1) * 128], kT[pr, k0:k0 + W], True, True,
                   [r_q, r_k], [r_sps])
                sm, p_, st = sm_b[bi], p_b[bi], st_b[bi]
                P.op('dve', lambda e: e.tensor_tensor(
                    out=sm[:, 0:W], in0=sps[:, 0:W], in1=maskt[:, mcol0:mcol0 + W], op=ALU.add),
                    reads=[r_sps, r_tab], writes=[r_sm[bi]])
                P.op('dve', lambda e: e.tensor_reduce(
                    out=st[:, 0:1], in_=sm[:, 0:W], axis=AX.X, op=ALU.max),
                    reads=[r_sm[bi]], writes=[r_st[bi]])
                P.op('dve', lambda e: e.tensor_scalar(
                    out=st[:, 1:2], in0=st[:, 0:1], scalar1=-0.125, scalar2=nsinkb[:, h:h + 1],
                    op0=ALU.mult, op1=ALU.min), reads=[r_st[bi], r_sink], writes=[r_st[bi]])
                P.op('act', lambda e: e.activation(
                    out=p_[:, 0:W], in_=sm[:, 0:W], func=AF.Exp, bias=st[:, 1:2], scale=0.125,
                    accum_out=st[:, 2:3]), reads=[r_sm[bi], r_st[bi]], writes=[r_p[bi], r_st2[bi]])
                P.op('act', lambda e: e.activation(
                    out=st[:, 3:4], in_=st[:, 1:2], func=AF.Exp, bias=sinkb[:, h:h + 1], scale=1.0),
                    reads=[r_st[bi], r_sink], writes=[r_st2[bi]])

            def stage_b(i):
                qb, h = items[i]
                kbs, nk, mcol0, W, k0 = geom(qb)
                c, half = h % 4, h // 4
                bi = i % NR
                ob = qb % 2
                ops_t, _ = O_ps[ob]
                p_, pt, st = p_b[bi], pt_b[bi], st_b[bi]
                P.op('dve', lambda e: e.tensor_tensor(out=st[:, 4:5], in0=st[:, 2:3], in1=st[:, 3:4], op=ALU.add),
                     reads=[r_st2[bi]], writes=[r_st2[bi]])
                P.op('dve', lambda e: e.reciprocal(out=st[:, 5:6], in_=st[:, 4:5]),
                     reads=[r_st2[bi]], writes=[r_st2[bi]])
                tps, r_tps = T_ps.next()
                tpb = ps_bf(tps)
                for k in range(nk):
                    TR(tpb[:, k * 128:(k + 1) * 128], p_[:, k * 128:(k + 1) * 128], identb[:],
                       [r_p[bi], r_const], [r_tps])
                if h % 4 != 3:
                    P.op('act', lambda e: e.copy(out=pt[:, 0:W], in_=tpb[:, 0:W]),
                         reads=[r_tps], writes=[r_pt[bi]])
                else:
                    P.op('dve', lambda e: e.tensor_copy(out=pt[:, 0:W], in_=tpb[:, 0:W]),
                         reads=[r_tps], writes=[r_pt[bi]])

            def stage_b2(i):
                qb, h = items[i]
                kbs, nk, mcol0, W, k0 = geom(qb)
                c, half = h % 4, h // 4
                bi = i % NR
                ob = qb % 2
                ops_t, _ = O_ps[i % 2]
                p_, pt, st = p_b[bi], pt_b[bi], st_b[bi]
                for k in range(nk):
                    MM(ops_t[:, h * 64:(h + 1) * 64], pt[:, k * 128:(k + 1) * 128],
                       Vt[:, kbs[k], half * 64:half * 64 + 64], k == 0, k == nk - 1,
                       [r_pt[bi], r_v], [r_Ob[i % 2]])
                P.op('act', lambda e: e.activation(
                    out=atok[ob][:, h * 64:(h + 1) * 64], in_=ops_t[:, h * 64:(h + 1) * 64],
                    func=AF.Identity, scale=st[:, 5:6]),
                    reads=[r_Ob[i % 2], r_st2[bi]], writes=[r_atok[ob][h]])
                if h == 7:
                    ps7, r_ps7 = PS[7]
                    p7b = ps_bf(ps7)
                    for cc in range(4):
                        TR(p7b[:, cc * 128:(cc + 1) * 128], atok[ob][:, cc * 128:(cc + 1) * 128], identb[:],
                           [r_atok[ob][2 * cc], r_atok[ob][2 * cc + 1], r_const], [r_ps7])
                    P.op('dve', lambda e: e.tensor_copy(
                        out=mixT[:, 0:4, qb * 128:(qb + 1) * 128],
                        in_=p7b[:, 0:512].rearrange('p (c t) -> p c t', c=4)),
                        reads=[r_ps7], writes=[r_mix[cc_][qb // 4] for cc_ in range(4)])

            n_it = len(items)
            for i in range(min(LEAD, n_it)):
                stage_a(i)
            for i in range(n_it + 1):
                if i + LEAD < n_it:
                    stage_a(i + LEAD)
                if i < n_it:
                    stage_b(i)
                if i >= 1:
                    stage_b2(i - 1)
            P.barrier()
            A.release(m2)
            if stop == 'attn':
                return mixT, r_mix

            g0T = AXr.alloc('g0T', [128, 4, L], BF16)
            g1T = AXr.alloc('g1T', [128, 4, L], BF16)
            zT = AXr.alloc('zT', [128, 4, L], BF16)
            r_g0 = [[Res('g0_%d_%d' % (c, tb)) for tb in range(NT)] for c in range(4)]
            r_g1 = [[Res('g1_%d_%d' % (c, tb)) for tb in range(NT)] for c in range(4)]
            r_z = [[Res('z_%d_%d' % (c, tb)) for tb in range(NT)] for c in range(4)]
            m3 = A.mark()
            wu_ = [A.alloc('wu%d' % i, [128, 8, 128], BF16) for i in range(2)]
            r_wu = [Res('wu0'), Res('wu1')]
            upad = [A.alloc('upad%d' % i, [128, L + 2], F32) for i in range(2)]
            r_up = [Res('upad0'), Res('upad1')]
            t0b = [A.alloc('t0b%d' % i, [128, L], F32) for i in range(2)]
            r_t0 = [Res('t0b0'), Res('t0b1')]
            for i in range(2):
                P.op('dve', lambda e, i=i: e.memset(upad[i][:, 0:1], 0.0), writes=[r_up[i]])
                P.op('dve', lambda e, i=i: e.memset(upad[i][:, L + 1:L + 2], 0.0), writes=[r_up[i]])
            dsts = [(g0T, r_g0), (g1T, r_g1), (zT, r_z)]
            for uc in range(12):
                b = uc % 2
                cast_load(wu_[b][:], w_in_v[:, :, 768 + uc * 128:768 + (uc + 1) * 128], r_wu[b], 'wu%d' % b)
                for tb in range(NT):
                    pst, r_ps = psrot.next()
                    sl = slice(tb * 512, (tb + 1) * 512)
                    for kc in range(8):
                        MM(pst[:], wu_[b][:, kc, :], hT[:, kc, sl], kc == 0, kc == 7, [r_wu[b], r_hT[tb]], [r_ps])
                    P.op('act', lambda e, pst=pst, b=b, tb=tb: e.copy(out=upad[b][:, 1 + tb * 512:1 + (tb + 1) * 512], in_=pst[:]),
                         reads=[r_ps], writes=[r_up[b]])
                dt_, rr = dsts[uc // 4]
                cc = uc % 4
                P.op('act', lambda e, b=b, uc=uc: e.activation(
                    out=t0b[b][:], in_=upad[b][:, 1:L + 1], func=AF.Identity, bias=V('short_b', uc), scale=V('sw1', uc)),
                    reads=[r_up[b], r_vec], writes=[r_t0[b]])
                P.op('dve', lambda e, b=b, uc=uc: e.scalar_tensor_tensor(
                    out=t0b[b][:], in0=upad[b][:, 0:L], scalar=V('sw0', uc), in1=t0b[b][:], op0=ALU.mult, op1=ALU.add),
                    reads=[r_up[b], r_t0[b], r_vec], writes=[r_t0[b]])
                P.op('dve', lambda e, b=b, uc=uc, dt_=dt_, cc=cc: e.scalar_tensor_tensor(
                    out=dt_[:, cc, :], in0=upad[b][:, 2:L + 2], scalar=V('sw2', uc), in1=t0b[b][:], op0=ALU.mult, op1=ALU.add),
                    reads=[r_up[b], r_t0[b], r_vec], writes=rr[cc])
            P.barrier()
            A.release(m_h)

            wo = A.alloc('wo', [128, 8, D], BF16)
            r_wo = Res('wo')
            for kc in range(8):
                cast_load(wo[:, kc, :], ev_w_out[kc * 128:(kc + 1) * 128, :], r_wo, 'wo')
            m_p4 = A.mark()
            ztok = A.alloc('ztok', [128, NB, 512], BF16)
            r_ztok = Res('ztok')
            Yb = A.alloc('Yb', [128, 16, 2, 512], BF16)
            r_Y = Res('Yb')
            for n in range(2):
                for blk in range(NB):
                    pst, r_ps = psrot.next()
                    pb = ps_bf(pst)
                    for cc in range(4):
                        TR(pb[:, cc * 128:(cc + 1) * 128], zT[:, cc, blk * 128:(blk + 1) * 128], identb[:],
                           [r_z[cc][blk // 4], r_const], [r_ps])
                    if blk % 2 == 0:
                        P.op('act', lambda e, pb=pb, blk=blk: e.copy(out=ztok[:, blk, :], in_=pb[:, 0:512]),
                             reads=[r_ps], writes=[r_ztok])
                    else:
                        P.op('dve', lambda e, pb=pb, blk=blk: e.tensor_copy(out=ztok[:, blk, :], in_=pb[:, 0:512]),
                             reads=[r_ps], writes=[r_ztok])
                m4 = A.mark()
                cqs = [A.alloc('cqf%d' % i, [128, 16, 128], BF16) for i in range(3)]
                sqs = [A.alloc('sqf%d' % i, [128, 16, 128], BF16) for i in range(3)]
                ksb = [A.alloc('ksb%d' % i, [128, 2, 512], BF16) for i in range(3)]
                r_m = [Res('mf0'), Res('mf1'), Res('mf2')]
                mt = [A.alloc('mt%d' % i, [128, 512], F32) for i in range(4)]
                r_mt = [Res('mt%d' % i) for i in range(4)]
                for j in range(16):
                    b = j % 3
                    P.dma('sp', cqs[b][:], c_cqf[j].rearrange('p (i q) -> p i q', q=128), writes=[r_m[b]], sem='mf%d' % b)
                    P.dma('sp', sqs[b][:], c_sqf[j].rearrange('p (i q) -> p i q', q=128), writes=[r_m[b]], sem='mf%d' % b)
                    P.dma('sp', ksb[b][:], kspec[j].rearrange('p (a b c) -> p a b c', a=2, b=2)[:, n, :, :],
                          writes=[r_m[b]], sem='mf%d' % b)
                    psa, r_psa = psrot.next()
                    psb, r_psb = psrot.next()
                    for i in range(16):
                        MM(psa[:], cqs[b][:, i, :], ztok[:, i, :], i == 0, i == 15, [r_m[b], r_ztok], [r_psa])
                    for i in range(16):
                        MM(psb[:], sqs[b][:, i, :], ztok[:, i, :], i == 0, i == 15, [r_m[b], r_ztok], [r_psb])
                    kr, ki = ksb[b][:, 0, :], ksb[b][:, 1, :]
                    P.op('dve', lambda e, psa=psa, kr=kr: e.tensor_tensor(out=mt[0][:], in0=psa[:], in1=kr, op=ALU.mult),
                         reads=[r_psa, r_m[b]], writes=[r_mt[0]])
                    P.op('dve', lambda e, psb=psb, ki=ki: e.tensor_tensor(out=mt[1][:], in0=psb[:], in1=ki, op=ALU.mult),
                         reads=[r_psb, r_m[b]], writes=[r_mt[1]])
                    P.op('pool', lambda e, j=j: e.tensor_tensor(out=Yb[:, j, 0, :], in0=mt[0][:], in1=mt[1][:], op=ALU.add),
                         reads=[r_mt[0], r_mt[1]], writes=[r_Y])
                    P.op('dve', lambda e, psb=psb, kr=kr: e.tensor_tensor(out=mt[2][:], in0=psb[:], in1=kr, op=ALU.mult),
                         reads=[r_psb, r_m[b]], writes=[r_mt[2]])
                    P.op('dve', lambda e, psa=psa, ki=ki: e.tensor_tensor(out=mt[3][:], in0=psa[:], in1=ki, op=ALU.mult),
                         reads=[r_psa, r_m[b]], writes=[r_mt[3]])
                    P.op('pool', lambda e, j=j: e.tensor_tensor(out=Yb[:, j, 1, :], in0=mt[2][:], in1=mt[3][:], op=ALU.subtract),
                         reads=[r_mt[2], r_mt[3]], writes=[r_Y])
                P.barrier()
                A.release(m4)
                m5 = A.mark()
                cqh = [A.alloc('cqh%d' % i, [128, 8, 512], BF16) for i in range(2)]
                sqh = [A.alloc('sqh%d' % i, [128, 8, 512], BF16) for i in range(2)]
                r_mh = [Res('mh0'), Res('mh1')]
                yt = [A.alloc('yt%d' % i, [128, 512], F32) for i in range(2)]
                r_yt = [Res('yt0'), Res('yt1')]
                dsn = 'ds%d' % n
                cq_v = c_cq.rearrange('(j p) t -> p j t', p=128)
                sq_v = c_sq.rearrange('(j p) t -> p j t', p=128)

                def load_half(tb, hf_):
                    sl = slice(tb * 512, (tb + 1) * 512)
                    P.dma('sp', cqh[hf_][:], cq_v[:, hf_ * 8:(hf_ + 1) * 8, sl], writes=[r_mh[hf_]], sem='mh%d' % hf_)
                    P.dma('sp', sqh[hf_][:], sq_v[:, hf_ * 8:(hf_ + 1) * 8, sl], writes=[r_mh[hf_]], sem='mh%d' % hf_)

                load_half(0, 0)
                load_half(0, 1)
                for tb in range(NT):
                    sl = slice(tb * 512, (tb + 1) * 512)
                    banks = PS[0:4] if tb % 2 == 0 else PS[4:8]
                    for hf_ in range(2):
                        for cc in range(4):
                            pst, r_ps = banks[cc]
                            for jj in range(8):
                                j = hf_ * 8 + jj
                                MM(pst[:], Yb[:, j, 0, cc * 128:(cc + 1) * 128], cqh[hf_][:, jj, :],
                                   hf_ == 0 and jj == 0, False, [r_Y, r_mh[hf_]], [r_ps])
                            for jj in range(8):
                                j = hf_ * 8 + jj
                                MM(pst[:], Yb[:, j, 1, cc * 128:(cc + 1) * 128], sqh[hf_][:, jj, :],
                                   False, hf_ == 1 and jj == 7, [r_Y, r_mh[hf_]], [r_ps])
                        if tb + 1 < NT:
                            load_half(tb + 1, hf_)
                    for cc in range(4):
                        pst, r_ps = banks[cc]
                        y_, r_y = yt[cc % 2], r_yt[cc % 2]
                        P.op('dve', lambda e, pst=pst, y_=y_, cc=cc, sl=sl, dsn=dsn: e.scalar_tensor_tensor(
                            out=y_[:], in0=zT[:, cc, sl], scalar=V(dsn, cc), in1=pst[:], op0=ALU.mult, op1=ALU.add),
                            reads=[r_ps, r_z[cc][tb], r_vec], writes=[r_y])
                        if n == 0:
                            P.op('pool', lambda e, y_=y_, cc=cc, sl=sl: e.tensor_tensor(
                                out=zT[:, cc, sl], in0=y_[:], in1=g0T[:, cc, sl], op=ALU.mult),
                                reads=[r_y, r_g0[cc][tb]], writes=[r_z[cc][tb]])
                        else:
                            P.op('pool', lambda e, y_=y_, cc=cc, sl=sl: e.tensor_tensor(
                                out=mixT[:, 4 + cc, sl], in0=y_[:], in1=g1T[:, cc, sl], op=ALU.mult),
                                reads=[r_y, r_g1[cc][tb]], writes=[r_mix[4 + cc][tb]])
                P.barrier()
                A.release(m5)
            if stop == 'mix':
                return mixT, r_mix

            A.release(m_p4)
            m6 = A.mark()
            xtk = [A.alloc('xtok%d' % i, [128, 4, D], F32) for i in range(2)]
            r_xtk = [Res('xtok0'), Res('xtok1')]
            xTbs = [A.alloc('xTb%d' % i, [128, 8, 512], F32) for i in range(2)]
            r_xTbs = [Res('xTb0'), Res('xTb1')]
            for tb in range(NT):
                b = tb % 2
                xTb, r_xTb = xTbs[b], r_xTbs[b]
                load_x_block(tb, xTb, r_xTb, stage=(xtk[b], r_xtk[b], 'xin%d' % b))
                sl = slice(tb * 512, (tb + 1) * 512)
                for oc in range(8):
                    pst, r_ps = psrot.next()
                    for kc in range(8):
                        MM(pst[:], wo[:, kc, oc * 128:(oc + 1) * 128], mixT[:, kc, sl], kc == 0, kc == 7,
                           [r_wo, r_mix[kc][tb]], [r_ps])
                    P.op('dve', lambda e, pst=pst, oc=oc, sl=sl, xTb=xTb: e.tensor_tensor(
                        out=xT[:, oc, sl], in0=pst[:], in1=xTb[:, oc, :], op=ALU.add),
                        reads=[r_ps, r_xTb], writes=[r_xT[oc][tb]])
            A.release(m0)
            return None, None

        def norm_all(xT, r_xT, gname, hT, r_hT, hf_cb=None):
            m = A.mark()
            sqs_ = [A.alloc('sq%d' % i, [128, 8, 512], BF16) for i in range(2)]
            rss_ = [A.alloc('rs%d' % i, [128, 512], F32) for i in range(2)]
            r_sqs = [Res('sq0'), Res('sq1')]
            r_rss = [Res('rs0'), Res('rs1')]
            hfs, r_hfs = None, None
            if hf_cb is not None:
                hfs = [A.alloc('hf%d' % i, [128, 8, 512], F32) for i in range(2)]
                r_hfs = [Res('hf0'), Res('hf1')]
            for tb in range(NT):
                sl = slice(tb * 512, (tb + 1) * 512)
                b = tb % 2
                sq, rs, r_sq, r_rs = sqs_[b], rss_[b], r_sqs[b], r_rss[b]
                rx_all = [r_xT[c][tb] for c in range(8)]
                P.op('act', lambda e, sl=sl, sq=sq: e.activation(out=sq[:], in_=xT[:, :, sl], func=AF.Square),
                     reads=rx_all, writes=[r_sq])
                pst, r_ps = psrot.next()
                for c in range(8):
                    MM(pst[:], onesb[:], sq[:, c, :], c == 0, c == 7, [r_sq, r_const], [r_ps])
                P.op('act', lambda e, pst=pst, rs=rs: e.activation(out=rs[:], in_=pst[:], func=AF.Sqrt, bias=epsD[:], scale=1.0),
                     reads=[r_ps, r_const], writes=[r_rs])
                P.op('dve', lambda e, rs=rs: e.reciprocal(out=rs[:], in_=rs[:]), reads=[r_rs], writes=[r_rs])
                for c in range(8):
                    P.op('dve', lambda e, c=c, sl=sl, rs=rs: e.scalar_tensor_tensor(
                        out=hT[:, c, sl], in0=xT[:, c, sl], scalar=V(gname, c), in1=rs[:],
                        op0=ALU.mult, op1=ALU.mult), reads=[r_xT[c][tb], r_rs, r_vec], writes=[r_hT[tb]])
                    if hfs is not None:
                        P.op('dve', lambda e, c=c, sl=sl, rs=rs, hf=hfs[b]: e.scalar_tensor_tensor(
                            out=hf[:, c, :], in0=xT[:, c, sl], scalar=V(gname, c), in1=rs[:],
                            op0=ALU.mult, op1=ALU.mult), reads=[r_xT[c][tb], r_rs, r_vec], writes=[r_hfs[b]])
                if hfs is not None and tb >= 1:
                    hf_cb(tb - 1, hfs[(tb - 1) % 2], r_hfs[(tb - 1) % 2])
            if hfs is not None:
                hf_cb(NT - 1, hfs[(NT - 1) % 2], r_hfs[(NT - 1) % 2])
            A.release(m)

        def swiglu_multi(xT, r_xT, hT, r_hT, experts, G, tag='f', cw_prep=None, before_compute=None):
            m = A.mark()
            NW = 2
            wgb = [A.alloc('wgb%d' % i, [128, 8, G * 128], BF16) for i in range(NW)]
            wub = [A.alloc('wub%d' % i, [128, 8, G * 128], BF16) for i in range(NW)]
            wdb = [A.alloc('wdb%d' % i, [128, G, D], BF16) for i in range(NW)]
            r_w = [Res('w%d' % i) for i in range(NW)]
            r_wd = [Res('wd%d' % i) for i in range(NW)]
            NA = 3
            sg = [A.alloc('sg%d' % i, [128, 512], F32) for i in range(NA)]
            r_sg = [Res('sg%d' % i) for i in range(NA)]
            a1 = [A.alloc('a1_%d' % i, [128, 512], F32) for i in range(NA)]
            r_a1 = [Res('a1_%d' % i) for i in range(NA)]
            NACT = 3
            actb = [A.alloc('actb%d' % i, [128, G, 512], BF16) for i in range(NACT)]
            r_act = [[Res('act%d_%d' % (i, f)) for f in range(G)] for i in range(NACT)]
            GU = Rot(PS[0:4])
            DN = Rot(PS[4:8])
            work = []
            for xi, (wg_d, wu_d, wd_d, nff) in enumerate(experts):
                nch = nff // 128
                assert nch % G == 0
                for g in range(nch // G):
                    work.append((xi, g))
            views = [(wg_d.rearrange('(kc p) f -> p kc f', p=128), wu_d.rearrange('(kc p) f -> p kc f', p=128),
                      wd_d.rearrange('(fc p) d -> p fc d', p=128)) for (wg_d, wu_d, wd_d, nff) in experts]

            def issue_gu(w):
                xi, g = work[w]
                b = w % NW
                wg_v, wu_v, wd_v = views[xi]
                fs = slice(g * G * 128, (g + 1) * G * 128)
                P.dma('pool', wgb[b][:], wg_v[:, :, fs], writes=[r_w[b]], sem='%sw%d' % (tag, b))
                P.dma('pool', wub[b][:], wu_v[:, :, fs], writes=[r_w[b]], sem='%sw%d' % (tag, b))

            def issue_d(w):
                xi, g = work[w]
                b = w % NW
                wg_v, wu_v, wd_v = views[xi]
                P.dma('pool', wdb[b][:], wd_v[:, g * G:(g + 1) * G, :], writes=[r_wd[b]], sem='%sd%d' % (tag, b))

            def issue(w):
                issue_gu(w)
                issue_d(w)

            steps = [(w, tb) for w in range(len(work)) for tb in range(NT)]
            cw_cur = {}
            kcnt = [0]

            def emit_gu(si):
                w, tb = steps[si]
                xi, g = work[w]
                b = w % NW
                if tb == 0:
                    if cw_prep is not None and g == 0:
                        cw_cur[xi] = cw_prep(xi)
                cwb, r_cwb = cw_cur[xi] if cw_prep is not None else (None, None)
                sl = slice(tb * 512, (tb + 1) * 512)
                ab = si % NACT
                for f in range(G):
                    psg, r_psg = GU.next()
                    psu, r_psu = GU.next()
                    for kc in range(8):
                        MM(psg[:], wgb[b][:, kc, f * 128:(f + 1) * 128], hT[:, kc, sl], kc == 0, kc == 7,
                           [r_w[b], r_hT[tb]], [r_psg])
                    for kc in range(8):
                        MM(psu[:], wub[b][:, kc, f * 128:(f + 1) * 128], hT[:, kc, sl], kc == 0, kc == 7,
                           [r_w[b], r_hT[tb]], [r_psu])
                    k = kcnt[0]
                    kcnt[0] += 1
                    s_, r_s = sg[k % NA], r_sg[k % NA]
                    a_, r_a = a1[k % NA], r_a1[k % NA]
                    P.op('act', lambda e, psg=psg, s_=s_: e.activation(out=s_[:], in_=psg[:], func=AF.Silu),
                         reads=[r_psg], writes=[r_s])
                    if cwb is None:
                        P.op('dve', lambda e, psu=psu, s_=s_, ab=ab, f=f: e.tensor_tensor(
                            out=actb[ab][:, f, :], in0=psu[:], in1=s_[:], op=ALU.mult),
                            reads=[r_psu, r_s], writes=[r_act[ab][f]])
                    else:
                        P.op('dve', lambda e, psu=psu, s_=s_, a_=a_: e.tensor_tensor(
                            out=a_[:], in0=psu[:], in1=s_[:], op=ALU.mult),
                            reads=[r_psu, r_s], writes=[r_a])
                        P.op('pool', lambda e, a_=a_, ab=ab, f=f, sl=sl, cwb=cwb: e.tensor_tensor(
                            out=actb[ab][:, f, :], in0=a_[:], in1=cwb[:, sl], op=ALU.mult),
                            reads=[r_a, r_cwb], writes=[r_act[ab][f]])

            def emit_dn(si):
                w, tb = steps[si]
                b = w % NW
                sl = slice(tb * 512, (tb + 1) * 512)
                ab = si % NACT
                for oc in range(8):
                    psd, r_psd = DN.next()
                    for f in range(G):
                        MM(psd[:], wdb[b][:, f, oc * 128:(oc + 1) * 128], actb[ab][:, f, :], f == 0, f == G - 1,
                           [r_wd[b], r_act[ab][f]], [r_psd])
                    P.op('dve', lambda e, psd=psd, oc=oc, sl=sl: e.tensor_tensor(
                        out=xT[:, oc, sl], in0=psd[:], in1=xT[:, oc, sl], op=ALU.add),
                        reads=[r_psd, r_xT[oc][tb]], writes=[r_xT[oc][tb]])

            issue(0)
            if len(work) > 1:
                issue(1)
            if before_compute is not None:
                before_compute()
            emit_gu(0)
            for si in range(len(steps)):
                if si + 1 < len(steps):
                    emit_gu(si + 1)
                    w1_, tb1_ = steps[si + 1]
                    if tb1_ == NT - 1 and w1_ + 2 < len(work):
                        issue_gu(w1_ + 2)
                emit_dn(si)
                w, tb = steps[si]
                if tb == NT - 1 and w + 2 < len(work):
                    issue_d(w + 2)
            P.barrier()
            A.release(m)

        def phase_conformer(xT, r_xT, hT, r_hT):
            m = A.mark()
            gluT = A.alloc('gluT', [128, 8, L + 30], BF16)
            r_glu = [Res('glu%d' % c) for c in range(8)]
            for c in range(8):
                P.op('pool', lambda e, c=c: e.memset(gluT[:, c, 0:15], 0.0), writes=[r_glu[c]])
                P.op('pool', lambda e, c=c: e.memset(gluT[:, c, L + 15:L + 30], 0.0), writes=[r_glu[c]])
            wa = [A.alloc('wa%d' % i, [128, 8, 128], BF16) for i in range(2)]
            wgt = [A.alloc('wgt%d' % i, [128, 8, 128], BF16) for i in range(2)]
            r_w = [Res('pw1_0'), Res('pw1_1')]
            w1v = od_pw1_w.rearrange('(kc p) n -> p kc n', p=128)

            def load_pw1(oc):
                b = oc % 2
                P.dma('pool', wa[b][:], w1v[:, :, oc * 128:(oc + 1) * 128], writes=[r_w[b]], sem='pw1_%d' % b)
                P.dma('pool', wgt[b][:], w1v[:, :, D + oc * 128:D + (oc + 1) * 128], writes=[r_w[b]], sem='pw1_%d' % b)

            load_pw1(0)
            load_pw1(1)
            norm_all(xT, r_xT, 'g_mix1', hT, r_hT)
            dwf = A.alloc('dwf', [128, 31, 8], F32)
            r_dwf = Res('dwf')
            P.dma('sp', dwf[:], od_dw_w.rearrange('j (c p) -> p j c', p=128), writes=[r_dwf], sem='dwf', **NCD)
            w2 = A.alloc('pw2', [128, 8, D], BF16)
            r_w2 = Res('pw2')
            for kc in range(8):
                P.dma('pool', w2[:, kc, :], od_pw2_w[kc * 128:(kc + 1) * 128, :], writes=[r_w2], sem='pw2')
            NDG = 4
            diag = [A.alloc('diag%d' % i, [128, 31, 128], BF16) for i in range(NDG)]
            r_diag = [[Res('diag%d_%d' % (i, j)) for j in range(31)] for i in range(NDG)]
            n_ = [0]

            def build_diag(c, db):
                for j in range(31):
                    n_[0] += 1
                    if n_[0] % 3 != 0:
                        P.op('dve', lambda e, c=c, j=j, db=db: e.tensor_scalar(
                            out=diag[db][:, j, :], in0=identb[:], scalar1=dwf[:, j, c:c + 1], scalar2=None, op0=ALU.mult),
                            reads=[r_dwf, r_const], writes=[r_diag[db][j]])
                    else:
                        P.op('act', lambda e, c=c, j=j, db=db: e.activation(
                            out=diag[db][:, j, :], in_=identb[:], func=AF.Copy, scale=dwf[:, j, c:c + 1]),
                            reads=[r_dwf, r_const], writes=[r_diag[db][j]])
            m1 = A.mark()
            sgb = [A.alloc('sgb%d' % i, [128, 512], F32) for i in range(2)]
            r_sgb = [Res('sgb0'), Res('sgb1')]
            k = 0
            for oc in range(8):
                b = oc % 2
                for tb in range(NT):
                    sl = slice(tb * 512, (tb + 1) * 512)
                    psa, r_psa = psrot.next()
                    psg, r_psg = psrot.next()
                    for kc in range(8):
                        MM(psa[:], wa[b][:, kc, :], hT[:, kc, sl], kc == 0, kc == 7, [r_w[b], r_hT[tb]], [r_psa])
                    for kc in range(8):
                        MM(psg[:], wgt[b][:, kc, :], hT[:, kc, sl], kc == 0, kc == 7, [r_w[b], r_hT[tb]], [r_psg])
                    s_, r_s = sgb[k % 2], r_sgb[k % 2]
                    k += 1
                    P.op('act', lambda e, psg=psg, s_=s_, oc=oc: e.activation(
                        out=s_[:], in_=psg[:], func=AF.Sigmoid, bias=V('pw1_b', 8 + oc), scale=1.0),
                        reads=[r_psg, r_vec], writes=[r_s])
                    P.op('dve', lambda e, psa=psa, s_=s_, oc=oc, tb=tb: e.scalar_tensor_tensor(
                        out=gluT[:, oc, 15 + tb * 512:15 + (tb + 1) * 512], in0=psa[:], scalar=V('pw1_b', oc),
                        in1=s_[:], op0=ALU.add, op1=ALU.mult), reads=[r_psa, r_s, r_vec], writes=[r_glu[oc]])
                if oc + 2 < 8:
                    load_pw1(oc + 2)
                if 4 <= oc < 4 + (NDG - 1):
                    build_diag(oc - 4, oc - 4)
            P.barrier()
            A.release(m1)
            m2 = A.mark()
            AH = Arena(nc, base=hT2_off, limit=hT2_off + 32768)
            dwv = AH.alloc('dwv', [128, 8, 512], F32)
            sqv = AH.alloc('sqv', [128, 8, 512], BF16)
            r_dwv = [Res('dwv%d' % c) for c in range(8)]
            r_sqv = [Res('sqv%d' % c) for c in range(8)]
            mean = A.alloc('mean', [128, 512], F32)
            rstd = A.alloc('rstd', [128, 512], F32)
            var = A.alloc('var', [128, 512], F32)
            r_stat = Res('stat')
            swT = AH.alloc('swT', [128, 8, 512], BF16)
            r_sw = [Res('sw%d' % c) for c in range(8)]
            dtmp = [A.alloc('dtmp%d' % i, [128, 512], F32) for i in range(2)]
            r_dt = [Res('dtmp0'), Res('dtmp1')]
            CV = Rot(PS[0:3])
            citems = [(tb, c) for tb in range(NT) for c in range(8)]

            def ln_chain(tb):
                ps_s, r_pss = PS[3]
                ps_q, r_psq = PS[4]
                for c in range(8):
                    MM(ps_s[:], onesf[:], dwv[:, c, :], c == 0, c == 7, [r_const, r_dwv[c]], [r_pss])
                for c in range(8):
                    MM(ps_q[:], onesb[:], sqv[:, c, :], c == 0, c == 7, [r_const, r_sqv[c]], [r_psq])
                P.op('dve', lambda e: e.tensor_scalar(out=mean[:], in0=ps_s[:], scalar1=1.0 / D, scalar2=None, op0=ALU.mult),
                     reads=[r_pss], writes=[r_stat])
                P.op('dve', lambda e: e.tensor_tensor(out=var[:], in0=mean[:], in1=mean[:], op=ALU.mult),
                     reads=[r_stat], writes=[r_stat])
                P.op('dve', lambda e: e.scalar_tensor_tensor(out=var[:], in0=ps_q[:], scalar=1.0 / D, in1=var[:],
                                                             op0=ALU.mult, op1=ALU.subtract),
                     reads=[r_psq, r_stat], writes=[r_stat])
                P.op('act', lambda e: e.activation(out=rstd[:], in_=var[:], func=AF.Sqrt, bias=epsT[:], scale=1.0),
                     reads=[r_stat, r_const], writes=[r_stat])
                P.op('dve', lambda e: e.reciprocal(out=rstd[:], in_=rstd[:]), reads=[r_stat], writes=[r_stat])
                for c in range(8):
                    d_, r_d = dtmp[c % 2], r_dt[c % 2]
                    P.op('dve', lambda e, c=c, d_=d_: e.tensor_tensor(out=d_[:], in0=dwv[:, c, :], in1=mean[:], op=ALU.subtract),
                         reads=[r_dwv[c], r_stat], writes=[r_d])
                    P.op('pool', lambda e, d_=d_: e.tensor_tensor(out=d_[:], in0=d_[:], in1=rstd[:], op=ALU.mult),
                         reads=[r_d, r_stat], writes=[r_d])
                    P.op('act', lambda e, c=c, d_=d_: e.activation(out=swT[:, c, :], in_=d_[:], func=AF.Silu,
                                                                   bias=V('ln_b', c), scale=V('ln_g', c)),
                         reads=[r_d, r_vec], writes=[r_sw[c]])

            def pw2_mm(tb):
                sl = slice(tb * 512, (tb + 1) * 512)
                for oc in range(8):
                    pst, r_ps = PS[5 + oc % 3]
                    for kc in range(8):
                        MM(pst[:], w2[:, kc, oc * 128:(oc + 1) * 128], swT[:, kc, :], kc == 0, kc == 7,
                           [r_w2, r_sw[kc]], [r_ps])
                    P.op('dve', lambda e, pst=pst, oc=oc, sl=sl: e.scalar_tensor_tensor(
                        out=xT[:, oc, sl], in0=pst[:], scalar=V('pw2_b', oc), in1=xT[:, oc, sl],
                        op0=ALU.add, op1=ALU.add), reads=[r_ps, r_vec, r_xT[oc][tb]], writes=[r_xT[oc][tb]])

            LA = NDG - 1
            pending_pw2 = None
            for i, (tb, c) in enumerate(citems):
                pst, r_ps = CV.next()
                db = i % NDG
                for j in range(31):
                    MM(pst[:], diag[db][:, j, :], gluT[:, c, tb * 512 + j:tb * 512 + j + 512], j == 0, j == 30,
                       [r_diag[db][j], r_glu[c]], [r_ps])
                if i + LA < len(citems):
                    build_diag(citems[i + LA][1], (i + LA) % NDG)
                if pending_pw2 is not None and c == 1:
                    pw2_mm(pending_pw2)
                    pending_pw2 = None
                P.op('act', lambda e, pst=pst, c=c: e.activation(out=dwv[:, c, :], in_=pst[:], func=AF.Identity,
                                                                 bias=V('dw_b', c), scale=1.0),
                     reads=[r_ps, r_vec], writes=[r_dwv[c]])
                P.op('act', lambda e, pst=pst, c=c: e.activation(out=sqv[:, c, :], in_=pst[:], func=AF.Square,
                                                                 bias=V('dw_b', c), scale=1.0),
                     reads=[r_ps, r_vec], writes=[r_sqv[c]])
                if c == 7:
                    ln_chain(tb)
                    pending_pw2 = tb
            pw2_mm(pending_pw2)
            P.barrier()
            A.release(m2)
            A.release(m)

        def phase_moe(xT, r_xT, hT, r_hT):
            m = A.mark()
            rt = A.alloc('router', [128, 8, NE], F32)
            r_rt = Res('router')
            P.dma('sp', rt[:], od_router.rearrange('(kc p) e -> p kc e', p=128), writes=[r_rt], sem='c')
            cw = A.alloc('cw', [128, NB, NE], F32)
            r_cw = Res('cw')
            cwT = A.alloc('cwT', [8, L], F32)
            r_cwT = Res('cwT')
            sel = A.alloc('sel', [8, 8 * 128], F32)
            P.dma('sp', sel[:], c_sel, writes=[r_rt], sem='c')
            lgall = A.alloc('lgall', [128, NB, NE], F32)
            r_lg = [Res('lg%d' % i) for i in range(NB)]
            rsc = A.alloc('rsc', [128, 8, NB], F32)
            r_rsc = Res('rsc')
            eq1 = A.alloc('eq1', [128, NB, NE], F32)
            eq2 = A.alloc('eq2', [128, NB, NE], F32)
            l2 = A.alloc('l2', [128, NB, NE], F32)
            r_eq = Res('eq')

            def route(tb, hf, r_hf):
                for j in range(4):
                    blk = tb * 4 + j
                    pst, r_ps = psrot.next()
                    for kc in range(8):
                        MM(pst[:, 0:NE], hf[:, kc, j * 128:(j + 1) * 128], rt[:, kc, :], kc == 0, kc == 7,
                           [r_hf, r_rt], [r_ps])
                    P.op('act', lambda e, pst=pst, blk=blk: e.copy(out=lgall[:, blk, :], in_=pst[:, 0:NE]),
                         reads=[r_ps], writes=[r_lg[blk]])

            def route_finish():
                M1, M2, DL, EX, DEN, G1, G2 = [rsc[:, i, :] for i in range(7)]
                P.op('dve', lambda e: e.tensor_reduce(out=M1, in_=lgall[:], axis=AX.X, op=ALU.max),
                     reads=r_lg, writes=[r_rsc])
                for blk in range(NB):
                    P.op('dve', lambda e, blk=blk: e.tensor_scalar(out=eq1[:, blk, :], in0=lgall[:, blk, :],
                                                                   scalar1=rsc[:, 0, blk:blk + 1], scalar2=None, op0=ALU.is_equal),
                         reads=[r_rsc, r_lg[blk]], writes=[r_eq])
                P.op('dve', lambda e: e.scalar_tensor_tensor(out=l2[:], in0=eq1[:], scalar=-1e30, in1=lgall[:],
                                                             op0=ALU.mult, op1=ALU.add), reads=[r_eq] + r_lg, writes=[r_eq])
                P.op('dve', lambda e: e.tensor_reduce(out=M2, in_=l2[:], axis=AX.X, op=ALU.max), reads=[r_eq], writes=[r_rsc])
                for blk in range(NB):
                    P.op('dve', lambda e, blk=blk: e.tensor_scalar(out=eq2[:, blk, :], in0=l2[:, blk, :],
                                                                   scalar1=rsc[:, 1, blk:blk + 1], scalar2=None, op0=ALU.is_equal),
                         reads=[r_rsc, r_eq], writes=[r_eq])
                P.op('dve', lambda e: e.tensor_tensor(out=DL, in0=M2, in1=M1, op=ALU.subtract), reads=[r_rsc], writes=[r_rsc])
                P.op('act', lambda e: e.activation(out=EX, in_=DL, func=AF.Exp), reads=[r_rsc], writes=[r_rsc])
                P.op('dve', lambda e: e.tensor_scalar(out=DEN, in0=EX, scalar1=1.0, scalar2=None, op0=ALU.add),
                     reads=[r_rsc], writes=[r_rsc])
                P.op('dve', lambda e: e.reciprocal(out=G1, in_=DEN), reads=[r_rsc], writes=[r_rsc])
                P.op('dve', lambda e: e.tensor_tensor(out=G2, in0=EX, in1=G1, op=ALU.mult), reads=[r_rsc], writes=[r_rsc])
                for blk in range(NB):
                    P.op('dve', lambda e, blk=blk: e.tensor_scalar(out=eq2[:, blk, :], in0=eq2[:, blk, :],
                                                                   scalar1=rsc[:, 6, blk:blk + 1], scalar2=None, op0=ALU.mult),
                         reads=[r_rsc, r_eq], writes=[r_eq])
                    P.op('dve', lambda e, blk=blk: e.scalar_tensor_tensor(
                        out=cw[:, blk, :], in0=eq1[:, blk, :], scalar=rsc[:, 5, blk:blk + 1], in1=eq2[:, blk, :],
                        op0=ALU.mult, op1=ALU.add), reads=[r_rsc, r_eq], writes=[r_cw])
                for q4 in range(4):
                    ps2, r_ps2 = psrot.next()
                    for j in range(4):
                        blk = q4 * 4 + j
                        TR(ps2[0:8, j * 128:(j + 1) * 128], cw[:, blk, :], ident[:], [r_cw, r_const], [r_ps2])
                    P.op('dve', lambda e, ps2=ps2, q4=q4: e.tensor_copy(out=cwT[:, q4 * 512:(q4 + 1) * 512], in_=ps2[0:8, :]),
                         reads=[r_ps2], writes=[r_cwT])

            norm_all(xT, r_xT, 'g_ffn1', hT, r_hT, hf_cb=route)
            route_finish()
            cwb = [A.alloc('cwb%d' % i, [128, L], F32) for i in range(2)]
            r_cwb = [Res('cwb0'), Res('cwb1')]

            def cw_prep(ex):
                b = ex % 2
                for tb in range(NT):
                    pst, r_ps = PS[4 + tb]
                    MM(pst[:], sel[:, ex * 128:(ex + 1) * 128], cwT[:, tb * 512:(tb + 1) * 512], True, True,
                       [r_rt, r_cwT], [r_ps])
                    P.op('act', lambda e, pst=pst, b=b, tb=tb: e.copy(out=cwb[b][:, tb * 512:(tb + 1) * 512], in_=pst[:]),
                         reads=[r_ps], writes=[r_cwb[b]])
                return cwb[b], r_cwb[b]

            swiglu_multi(xT, r_xT, hT, r_hT,
                         [(od_moe_wg[ex], od_moe_wu[ex], od_moe_wd[ex], FFE) for ex in range(NE)], 4,
                         tag='m', cw_prep=cw_prep)
            A.release(m)

        xT = A.alloc('xT', [128, 8, L], F32)
        xT_off = A.last_off
        r_xT = [[Res('xT%d_%d' % (c, tb)) for tb in range(NT)] for c in range(8)]

        AW = Arena(nc, base=xT_off, limit=xT_off + 65536)
        w_in_v0 = ev_w_in.rearrange('(kc p) n -> p kc n', p=128)
        wq_all = [AW.alloc('wqa%d' % i, [128, 8, 128], BF16) for i in range(5)]
        wr_all = [AW.alloc('wra%d' % i, [128, 8, 128], BF16) for i in range(5)]
        wv_pre = AW.alloc('wvp', [128, 8, 128], BF16)
        r_wpre = Res('wpre')
        if stop != 'in':
            for ci in range(5):
                if ci < 4:
                    runs_p = [(0, ci * 64, 64), (64, (ci + 4) * 64, 64)]
                    runs_r = []
                    for hi, h in enumerate((ci, ci + 4)):
                        runs_r.append((hi * 64, h * 64 + 32, 32))
                        runs_r.append((hi * 64 + 32, h * 64, 32))
                else:
                    runs_p = [(0, 512, 128)]
                    runs_r = [(0, 512 + 32, 32), (32, 512, 32), (64, 576 + 32, 32), (96, 576, 32)]
                for (o, s_, n) in runs_p:
                    P.dma('pool', wq_all[ci][:, :, o:o + n], w_in_v0[:, :, s_:s_ + n], writes=[r_wpre], sem='wpre')
                for (o, s_, n) in runs_r:
                    P.dma('pool', wr_all[ci][:, :, o:o + n], w_in_v0[:, :, s_:s_ + n], writes=[r_wpre], sem='wpre')
            P.dma('pool', wv_pre[:], w_in_v0[:, :, 640:768], writes=[r_wpre], sem='wpre')

        phase_filter_needed = stop not in ('attn', 'in')
        if phase_filter_needed:
            phase_filter()

        if stop == 'in':
            for tb in range(NT):
                r = Res('xTb')
                load_x_block(tb, xT[:, :, tb * 512:(tb + 1) * 512], r)
                for c in range(8):
                    r_xT[c][tb] = r
            finish(lambda tb: (xT[:, :, tb * 512:(tb + 1) * 512], r_xT[0][tb]))
            return nc

        mixT, r_mix = phase_layer0_mixer(xT, r_xT)
        if stop in ('attn', 'mix'):
            for tb in range(NT):
                sl = slice(tb * 512, (tb + 1) * 512)
                for c in range(8):
                    P.op('dve', lambda e, c=c, sl=sl: e.tensor_copy(out=xT[:, c, sl], in_=mixT[:, c, sl]),
                         reads=[r_mix[c][tb]], writes=[r_xT[c][tb]])
            P.barrier()

        def xblk(tb):
            r = Res('xall')
            return xT[:, :, tb * 512:(tb + 1) * 512], r

        if stop in ('attn', 'mix', 'l0mix'):
            P.barrier()
            finish(xblk)
            return nc

        hT = A.alloc('hT2', [128, 8, L], BF16)
        hT2_off = A.last_off
        r_hT = [Res('hT2_%d' % tb) for tb in range(NT)]
        swiglu_multi(xT, r_xT, hT, r_hT, [(ev_ffn_wg, ev_ffn_wu, ev_ffn_wd, FF)], 2, tag='f',
                     before_compute=lambda: norm_all(xT, r_xT, 'g_ffn0', hT, r_hT))
        if stop == 'l0':
            P.barrier()
            finish(xblk)
            return nc
        phase_conformer(xT, r_xT, hT, r_hT)
        if stop == 'conf':
            P.barrier()
            finish(xblk)
            return nc
        phase_moe(xT, r_xT, hT, r_hT)
        if stop == 'moe':
            P.barrier()
            finish(xblk)
            return nc
        P.barrier()
        AF2 = Arena(nc, base=hT2_off, limit=hT2_off + 32768)
        oTs = [AF2.alloc('oT%d' % i, [128, 8, 512], F32) for i in range(2)]
        r_oTs = [Res('oT0'), Res('oT1')]
        sqf = [A.alloc('sq%d' % i, [128, 8, 512], BF16) for i in range(2)]
        rsf = [A.alloc('rs%d' % i, [128, 512], F32) for i in range(2)]
        r_sqf = [Res('sq0'), Res('sq1')]
        r_rsf = [Res('rs0'), Res('rs1')]
        otk = [A.alloc('otok%d' % i, [128, 4, D], F32) for i in range(2)]
        r_otk = [Res('otok0'), Res('otok1')]
        for tb in range(NT):
            b = tb % 2
            sl = slice(tb * 512, (tb + 1) * 512)
            sq, rs, oT = sqf[b], rsf[b], oTs[b]
            r_sq, r_rs, r_o = r_sqf[b], r_rsf[b], r_oTs[b]
            P.op('act', lambda e, sl=sl, sq=sq: e.activation(out=sq[:], in_=xT[:, :, sl], func=AF.Square), writes=[r_sq])
            pst, r_ps = psrot.next()
            for c in range(8):
                MM(pst[:], onesb[:], sq[:, c, :], c == 0, c == 7, [r_sq, r_const], [r_ps])
            P.op('act', lambda e, pst=pst, rs=rs: e.activation(out=rs[:], in_=pst[:], func=AF.Sqrt, bias=epsD[:], scale=1.0),
                 reads=[r_ps, r_const], writes=[r_rs])
            P.op('dve', lambda e, rs=rs: e.reciprocal(out=rs[:], in_=rs[:]), reads=[r_rs], writes=[r_rs])
            for c in range(8):
                P.op('dve', lambda e, c=c, sl=sl, oT=oT, rs=rs: e.scalar_tensor_tensor(
                    out=oT[:, c, :], in0=xT[:, c, sl], scalar=V('g_fin', c), in1=rs[:],
                    op0=ALU.mult, op1=ALU.mult), reads=[r_rs, r_vec], writes=[r_o])
            store_block(tb, oT[:], r_o, stage=(otk[b], r_otk[b], 'xout%d' % b))
        P.barrier()
        P.emit()
    return nc


_CONST = {}


def _bf16(a):
    return np.ascontiguousarray(a.astype(ml_dtypes.bfloat16))


def host_constants():
    if _CONST:
        return _CONST
    c = {}
    c['c_ident'] = np.eye(128, dtype=np.float32)
    half = 32
    inv = 10000.0 ** (-np.arange(half, dtype=np.float32) / half)
    pos = np.arange(L, dtype=np.float32)
    ang = pos[None, :] * inv[:, None]
    cos = np.cos(ang).astype(np.float32)
    sin = np.sin(ang).astype(np.float32)
    cos64 = np.concatenate([cos, cos], 0)
    sin64 = np.concatenate([-sin, sin], 0)
    c['c_cos'] = np.ascontiguousarray(np.concatenate([cos64, cos64], 0))
    c['c_sin'] = np.ascontiguousarray(np.concatenate([sin64, sin64], 0))
    r = np.arange(128)[:, None]
    j = np.arange(384)[None, :]
    rel = j - 128 - r
    c['c_mask'] = np.where(np.abs(rel) <= 128, 0.0, -30000.0).astype(np.float32)
    bands = 16
    l = np.arange(L + 1, dtype=np.float64)
    t = (l / (L - 1))
    w = 2.0 * math.pi * l / L
    fb = np.linspace(1e-4, bands - 1, bands)
    zw = fb[None, :] * w[:, None]
    z = np.concatenate([t[:, None], np.cos(zw), -np.sin(zw)], axis=-1)
    c['c_zf'] = np.ascontiguousarray(z.T.astype(np.float32))
    min_decay = math.log(1e-2) / 1.5
    max_decay = math.log(1e-2) / 0.3
    deltas = np.abs(np.linspace(min_decay, max_decay, 512))
    tt = np.linspace(0.0, 1.0, L)
    dec = np.exp(-tt[:, None] * deltas[None, :])
    decsh = np.zeros_like(dec)
    decsh[:L - 1] = dec[1:]
    c['c_dec'] = dec.astype(np.float32)
    c['c_decsh'] = decsh.astype(np.float32)
    N = 2 * L
    a = np.arange(L, dtype=np.float64) + 0.5
    psi = 2.0 * math.pi * np.outer(a, a) / N
    cq = _bf16(np.cos(psi))
    sq = _bf16(np.sin(psi))
    c['c_cq'] = cq
    c['c_sq'] = sq

    def fwd_layout(m):
        return np.ascontiguousarray(m.reshape(16, 128, 16, 128).transpose(2, 1, 0, 3).reshape(16, 128, 2048))
    c['c_cqf'] = fwd_layout(cq)
    c['c_sqf'] = fwd_layout(sq)
    phi = 2.0 * math.pi * (np.arange(L) + 0.5) / N
    cf = (2.0 / N) * np.cos(phi / 2)
    sf = (2.0 / N) * np.sin(phi / 2)
    c['c_cfsf'] = np.ascontiguousarray(np.concatenate([cf.reshape(16, 128).T, sf.reshape(16, 128).T], 1).astype(np.float32))
    sel = np.zeros((8, 8, 128), np.float32)
    for e in range(8):
        sel[e, e, :] = 1.0
    c['c_sel'] = sel.reshape(8, 1024)
    _CONST.update(c)
    return _CONST


_NC_CACHE = {}


def kernel(**inputs):
    stop = inputs.pop('_stop', 'full')
    cores = inputs.pop('_cores', list(range(8)))
    if stop not in _NC_CACHE:
        _NC_CACHE[stop] = build_program(stop)
    nc = _NC_CACHE[stop]
    consts = host_constants()
    shared = {}
    for k, v in inputs.items():
        if k == 'x':
            continue
        a = np.asarray(v, dtype=np.float32)
        if k != 'final_norm':
            a = a.reshape(a.shape[1:])
        shared[k] = np.ascontiguousarray(a)
    shared.update(consts)
    x = np.asarray(inputs['x'], dtype=np.float32)
    in_maps = []
    for b in cores:
        d = dict(shared)
        d['x'] = np.ascontiguousarray(x[b])
        in_maps.append(d)
    res = run_bass_kernel_spmd(nc, in_maps, core_ids=list(range(len(cores))))
    out = np.stack([r['out'] for r in res.results], axis=0)
    return out.astype(np.float32)
```
